# Optimizing a Trainium2 kernel written in Bass

```python
import jax
import jax.numpy as jnp
from jax import lax
import numpy as np

D_MODEL = 1024
BATCH = 2
SEQ = 8192
DEPTH = 4

GRID_W = 64
CTX_LEN = 256
HEAD_DIM = D_MODEL // 16
FNET_GROUPS = 4
FNET_GROUP_DIM = D_MODEL // 8
FNET_WIDTH = FNET_GROUPS * FNET_GROUP_DIM
RET_HEADS = 4
RET_QK_DIM = HEAD_DIM
RET_V_DIM = 2 * HEAD_DIM
RET_CHUNK = 128
EVEN_SPLITS = (FNET_WIDTH,
               FNET_WIDTH + RET_HEADS * RET_QK_DIM,
               FNET_WIDTH + 2 * RET_HEADS * RET_QK_DIM,
               FNET_WIDTH + 2 * RET_HEADS * RET_QK_DIM + RET_HEADS * RET_V_DIM)
EVEN_IN_WIDTH = FNET_WIDTH + 2 * RET_HEADS * RET_QK_DIM + 2 * RET_HEADS * RET_V_DIM
EVEN_OUT_WIDTH = FNET_WIDTH + RET_HEADS * RET_V_DIM
DIFF_HEADS = 8
DIFF_V_DIM = 2 * HEAD_DIM
ODD_IN_WIDTH = 3 * DIFF_HEADS * 2 * HEAD_DIM
ODD_OUT_WIDTH = DIFF_HEADS * DIFF_V_DIM
Q_BLOCK = 128
ROPE_BASE = 10000.0
FFN_DIM = 256 * ((8 * D_MODEL // 3 + 255) // 256)
N_EXPERTS = 8
TOP_K = 2
EXPERT_DIM = 7 * D_MODEL // 2
N_MOD = 6
EPS = 1e-6

kernel_name = 'hybrid_fnet_retention_diffattn_moe_dit'


def rms_norm(x, gain=None):
    xf = x.astype(jnp.float32)
    y = xf * lax.rsqrt(jnp.mean(xf * xf, axis=-1, keepdims=True) + EPS)
    if gain is not None:
        y = y * gain.astype(jnp.float32)
    return y.astype(x.dtype)


def modulate(x, shift, scale):
    return rms_norm(x) * (1.0 + scale) + shift


def adaln_terms(cond, w_mod, b_mod):
    return jnp.split(jax.nn.silu(cond) @ w_mod + b_mod, N_MOD, axis=-1)


def axial_rope_tables(n_tokens, dim):
    rows = n_tokens // GRID_W
    row = jnp.repeat(jnp.arange(rows, dtype=jnp.float32), GRID_W)
    col = jnp.tile(jnp.arange(GRID_W, dtype=jnp.float32), rows)
    quarter = dim // 4
    inv_freq = ROPE_BASE ** (-jnp.arange(quarter, dtype=jnp.float32) / quarter)
    ang = jnp.concatenate([row[:, None] * inv_freq, col[:, None] * inv_freq], axis=-1)
    return jnp.cos(ang), jnp.sin(ang)


def apply_rope(x, cos, sin):
    half = x.shape[-1] // 2
    xf = x.astype(jnp.float32)
    x1, x2 = xf[..., :half], xf[..., half:]
    return jnp.concatenate([x1 * cos - x2 * sin, x2 * cos + x1 * sin], axis=-1).astype(x.dtype)


def swiglu(h, w1, w3, w2):
    return (jax.nn.silu(h @ w1) * (h @ w3)) @ w2


def moe_swiglu(h, w_router, w1, w3, w2):
    logits = jnp.einsum('bnd,de->bne', h, w_router).astype(jnp.float32)
    top_vals, top_idx = lax.top_k(logits, TOP_K)
    gates = jax.nn.softmax(top_vals, axis=-1)
    combine = jnp.einsum('bnk,bnke->bne', gates, jax.nn.one_hot(top_idx, N_EXPERTS, dtype=jnp.float32))
    out = jnp.zeros_like(h)
    for e in range(N_EXPERTS):
        out = out + combine[..., e:e + 1].astype(h.dtype) * swiglu(h, w1[e], w3[e], w2[e])
    return out


def fourier_mix(f):
    b, n, _ = f.shape
    u = f.reshape(b, n, FNET_GROUPS, FNET_GROUP_DIM).astype(jnp.float32)
    y = jnp.fft.fft2(u, axes=(1, 3), norm='ortho').real
    return y.reshape(b, n, FNET_WIDTH).astype(f.dtype)


def retention_chunkwise(q, k, v, log_gamma, state0, include_diag):
    b, h, n, _ = q.shape
    dv = v.shape[-1]
    nc = n // RET_CHUNK
    pos = jnp.arange(RET_CHUNK, dtype=jnp.float32)
    rel = pos[:, None] - pos[None, :]
    mask = rel >= 0 if include_diag else rel > 0
    lg = log_gamma[:, None, None]
    decay_intra = jnp.where(mask, jnp.exp(lg * jnp.where(mask, rel, 0.0)), 0.0)
    decay_q = jnp.exp(log_gamma[:, None] * (pos + 1.0))[:, :, None]
    decay_k = jnp.exp(log_gamma[:, None] * (RET_CHUNK - 1.0 - pos))[:, :, None]
    decay_chunk = jnp.exp(log_gamma * RET_CHUNK)[:, None, None]

    def to_chunks(t):
        return t.reshape(b, h, nc, RET_CHUNK, t.shape[-1]).transpose(2, 0, 1, 3, 4)

    def step(state, qkv):
        qc, kc, vc = qkv
        scores = jnp.einsum('bhid,bhjd->bhij', qc, kc) * decay_intra
        out = (jnp.einsum('bhij,bhje->bhie', scores, vc)
               + jnp.einsum('bhid,bhde->bhie', qc * decay_q, state))
        state = decay_chunk * state + jnp.einsum('bhjd,bhje->bhde', kc * decay_k, vc)
        return state, out

    state, out = lax.scan(step, state0, (to_chunks(q), to_chunks(k), to_chunks(v)))
    return out.transpose(1, 2, 0, 3, 4).reshape(b, h, n, dv), state


def bidir_retention(qc, kc, vc, ql, kl, vl, lg_f, lg_b):
    zero = jnp.zeros((qc.shape[0], RET_HEADS, RET_QK_DIM, RET_V_DIM), jnp.float32)
    flip = lambda t: jnp.flip(t, axis=2)
    oc_f, s_f = retention_chunkwise(qc, kc, vc, lg_f, zero, True)
    ol_f, _ = retention_chunkwise(ql, kl, vl, lg_f, s_f, True)
    oc_b, s_b = retention_chunkwise(flip(qc), flip(kc), flip(vc), lg_b, zero, False)
    ol_b, _ = retention_chunkwise(flip(ql), flip(kl), flip(vl), lg_b, s_b, False)
    return oc_f + flip(oc_b), ol_f + flip(ol_b)


def even_mixer(h_ctx, h_lat, w_in, w_out, dec_f, dec_b, cos, sin):
    lg_f = jax.nn.log_sigmoid(dec_f.astype(jnp.float32))
    lg_b = jax.nn.log_sigmoid(dec_b.astype(jnp.float32))

    def project(h, rope):
        b, n, _ = h.shape
        f, q, k, v, g = jnp.split(h @ w_in, EVEN_SPLITS, axis=-1)
        heads = lambda t, d: t.reshape(b, n, RET_HEADS, d).transpose(0, 2, 1, 3).astype(jnp.float32)
        q, k, v = heads(q, RET_QK_DIM), heads(k, RET_QK_DIM), heads(v, RET_V_DIM)
        if rope:
            q, k = apply_rope(q, cos, sin), apply_rope(k, cos, sin)
        return f, q, k * RET_QK_DIM ** -0.5, v, g

    fc, qc, kc, vc, gc = project(h_ctx, False)
    fl, ql, kl, vl, gl = project(h_lat, True)
    oc, ol = bidir_retention(qc, kc, vc, ql, kl, vl, lg_f, lg_b)

    def merge(f, o, g):
        b, n, _ = f.shape
        o = rms_norm(o).transpose(0, 2, 1, 3).reshape(b, n, RET_HEADS * RET_V_DIM).astype(g.dtype)
        return jnp.concatenate([fourier_mix(f), o * jax.nn.silu(g)], axis=-1) @ w_out

    return merge(fc, oc, gc), merge(fl, ol, gl)


def diff_attend(q, k, v, lam):
    s = jnp.einsum('bhmqd,bhmkd->bhmqk', q, k).astype(jnp.float32) * HEAD_DIM ** -0.5
    p = jax.nn.softmax(s, axis=-1)
    a = p[:, :, 0] - lam * p[:, :, 1]
    return jnp.einsum('bhqk,bhkv->bhqv', a.astype(v.dtype), v)


def odd_mixer(h_ctx, h_lat, w_in, w_out, q_gain, k_gain, lq1, lk1, lq2, lk2, sub_gain,
              lam_init, cos, sin, with_ctx_out):
    f32 = jnp.float32
    lam = (jnp.exp(jnp.sum(lq1.astype(f32) * lk1.astype(f32)))
           - jnp.exp(jnp.sum(lq2.astype(f32) * lk2.astype(f32))) + lam_init)

    def project(h, rope):
        b, n, _ = h.shape
        q, k, v = jnp.split(h @ w_in, 3, axis=-1)
        q = rms_norm(q.reshape(b, n, DIFF_HEADS, 2, HEAD_DIM).transpose(0, 2, 3, 1, 4), q_gain)
        k = rms_norm(k.reshape(b, n, DIFF_HEADS, 2, HEAD_DIM).transpose(0, 2, 3, 1, 4), k_gain)
        if rope:
            q, k = apply_rope(q, cos, sin), apply_rope(k, cos, sin)
        v = v.reshape(b, n, DIFF_HEADS, DIFF_V_DIM).transpose(0, 2, 1, 3)
        return q, k, v

    qc, kc, vc = project(h_ctx, False)
    ql, kl, vl = project(h_lat, True)
    k_all = jnp.concatenate([kc, kl], axis=3)
    v_all = jnp.concatenate([vc, vl], axis=2)

    def finish(o):
        b, _, n, _ = o.shape
        o = rms_norm(o, sub_gain) * (1.0 - lam_init)
        return o.transpose(0, 2, 1, 3).reshape(b, n, ODD_OUT_WIDTH) @ w_out

    b, _, _, n, _ = ql.shape
    nb = n // Q_BLOCK
    q_blocks = ql.reshape(b, DIFF_HEADS, 2, nb, Q_BLOCK, HEAD_DIM).transpose(3, 0, 1, 2, 4, 5)
    o_lat = lax.map(lambda qb: diff_attend(qb, k_all, v_all, lam), q_blocks)
    o_lat = o_lat.transpose(1, 2, 0, 3, 4).reshape(b, DIFF_HEADS, n, DIFF_V_DIM)
    out_ctx = finish(diff_attend(qc, kc, vc, lam)) if with_ctx_out else None
    return out_ctx, finish(o_lat)


def setup_inputs(seed: int = 0) -> dict:
    key = jax.random.key(seed)
    ks = jax.random.split(key, 26)
    n_even = (DEPTH + 1) // 2
    n_odd = DEPTH // 2
    f32 = jnp.float32
    d = D_MODEL

    def nrm(k, shape, scale):
        return scale * jax.random.normal(k, shape, f32)

    decay_logit = jnp.log(2.0 ** (5.0 + jnp.arange(RET_HEADS, dtype=f32)) - 1.0)
    return {
        'x': nrm(ks[0], (BATCH, SEQ, d), 1.0),
        'c': nrm(ks[1], (BATCH, d), 1.0),
        'ctx': nrm(ks[2], (BATCH, CTX_LEN, d), 1.0),
        'c_ctx': nrm(ks[3], (d,), 1.0),
        'w_mod': nrm(ks[4], (DEPTH, d, N_MOD * d), 0.5 * d ** -0.5),
        'b_mod': nrm(ks[5], (DEPTH, N_MOD * d), 0.02),
        'w_in_even': nrm(ks[6], (n_even, d, EVEN_IN_WIDTH), d ** -0.5),
        'w_out_even': nrm(ks[7], (n_even, EVEN_OUT_WIDTH, d), EVEN_OUT_WIDTH ** -0.5),
        'ret_decay_fwd': decay_logit + nrm(ks[8], (n_even, RET_HEADS), 0.1),
        'ret_decay_bwd': decay_logit + nrm(ks[9], (n_even, RET_HEADS), 0.1),
        'ffn_w1': nrm(ks[10], (n_even, d, FFN_DIM), d ** -0.5),
        'ffn_w3': nrm(ks[11], (n_even, d, FFN_DIM), d ** -0.5),
        'ffn_w2': nrm(ks[12], (n_even, FFN_DIM, d), FFN_DIM ** -0.5),
        'w_in_odd': nrm(ks[13], (n_odd, d, ODD_IN_WIDTH), d ** -0.5),
        'w_out_odd': nrm(ks[14], (n_odd, ODD_OUT_WIDTH, d), ODD_OUT_WIDTH ** -0.5),
        'q_norm_gain': 1.0 + nrm(ks[15], (n_odd, HEAD_DIM), 0.02),
        'k_norm_gain': 1.0 + nrm(ks[16], (n_odd, HEAD_DIM), 0.02),
        'lambda_q1': nrm(ks[17], (n_odd, HEAD_DIM), 0.1),
        'lambda_k1': nrm(ks[18], (n_odd, HEAD_DIM), 0.1),
        'lambda_q2': nrm(ks[19], (n_odd, HEAD_DIM), 0.1),
        'lambda_k2': nrm(ks[20], (n_odd, HEAD_DIM), 0.1),
        'subln_gain': 1.0 + nrm(ks[21], (n_odd, DIFF_V_DIM), 0.02),
        'w_router': nrm(ks[22], (n_odd, d, N_EXPERTS), d ** -0.5),
        'moe_w1': nrm(ks[23], (n_odd, N_EXPERTS, d, EXPERT_DIM), d ** -0.5),
        'moe_w3': nrm(ks[24], (n_odd, N_EXPERTS, d, EXPERT_DIM), d ** -0.5),
        'moe_w2': nrm(ks[25], (n_odd, N_EXPERTS, EXPERT_DIM, d), EXPERT_DIM ** -0.5),
    }


def reference(x, c, ctx, c_ctx, w_mod, b_mod, w_in_even, w_out_even, ret_decay_fwd, ret_decay_bwd,
              ffn_w1, ffn_w3, ffn_w2, w_in_odd, w_out_odd, q_norm_gain, k_norm_gain,
              lambda_q1, lambda_k1, lambda_q2, lambda_k2, subln_gain, w_router, moe_w1, moe_w3, moe_w2):
    cos, sin = axial_rope_tables(x.shape[1], HEAD_DIM)
    x_lat, x_ctx = x, ctx
    n_ctx = ctx.shape[1]
    for layer in range(DEPTH):
        i = layer // 2
        last = layer == DEPTH - 1
        mod_lat = [m[:, None, :] for m in adaln_terms(c, w_mod[layer], b_mod[layer])]
        mod_ctx = adaln_terms(c_ctx, w_mod[layer], b_mod[layer])
        h_lat = modulate(x_lat, mod_lat[0], mod_lat[1])
        h_ctx = modulate(x_ctx, mod_ctx[0], mod_ctx[1])
        if layer % 2 == 0:
            o_ctx, o_lat = even_mixer(h_ctx, h_lat, w_in_even[i], w_out_even[i],
                                      ret_decay_fwd[i], ret_decay_bwd[i], cos, sin)
        else:
            lam_init = 0.8 - 0.6 * float(np.exp(-0.3 * layer))
            o_ctx, o_lat = odd_mixer(h_ctx, h_lat, w_in_odd[i], w_out_odd[i], q_norm_gain[i], k_norm_gain[i],
                                     lambda_q1[i], lambda_k1[i], lambda_q2[i], lambda_k2[i], subln_gain[i],
                                     lam_init, cos, sin, not last)
        x_lat = x_lat + mod_lat[2] * o_lat
        h_lat = modulate(x_lat, mod_lat[3], mod_lat[4])
        if last:
            h_all = h_lat
        else:
            x_ctx = x_ctx + mod_ctx[2] * o_ctx
            h_all = jnp.concatenate([modulate(x_ctx, mod_ctx[3], mod_ctx[4]), h_lat], axis=1)
        if layer % 2 == 0:
            y = swiglu(h_all, ffn_w1[i], ffn_w3[i], ffn_w2[i])
        else:
            y = moe_swiglu(h_all, w_router[i], moe_w1[i], moe_w3[i], moe_w2[i])
        if last:
            x_lat = x_lat + mod_lat[5] * y
        else:
            x_ctx = x_ctx + mod_ctx[5] * y[:, :n_ctx]
            x_lat = x_lat + mod_lat[5] * y[:, n_ctx:]
    return x_lat
```

```python
import math
import numpy as np
import ml_dtypes
import concourse.bass as bass
import concourse.mybir as mybir
from concourse.bass_utils import run_bass_kernel_spmd

F32 = mybir.dt.float32
BF16 = mybir.dt.bfloat16
AF = mybir.ActivationFunctionType
ALU = mybir.AluOpType
AX = mybir.AxisListType

NCORES = 8
D = 1024
KC = 8
NCTX = 64
NLAT = 2048
NT = NCTX + NLAT
TT = [(0, 64), (64, 512), (576, 512), (1088, 512), (1600, 512)]
TM = [(0, 64)] + [(64 + 128 * i, 128) for i in range(16)]
EPS = 1e-6
FFN = 2816
EXP = 3584
NEXP = 8
DEPTH = 4
ENGS = ("pe", "act", "dve", "pool", "sp")


PHASE_BUFS = []


class Buf:
    def __init__(self, name, keep=False):
        self.name = name
        if not keep:
            PHASE_BUFS.append(self)
        self.lw = None
        self.readers = {}
        self.dma_readers = []
        self.dsem = None
        self.dcount = 0


class Ins:
    __slots__ = ("eng", "fn", "deps", "kind", "dsem", "dcount", "sig", "need", "waits", "idx")

    def __init__(self, eng, fn, kind):
        self.eng = eng
        self.fn = fn
        self.kind = kind
        self.deps = set()
        self.dsem = None
        self.dcount = 0
        self.sig = None
        self.need = False
        self.waits = None


class Sched:
    def __init__(self, nc):
        self.nc = nc
        self.q = {e: [] for e in ENGS}
        self.last = {e: None for e in ENGS}
        self.dmas_since_bar = []
        self.nsem = 0
        self.enabled = True
        self.free_dsems = []
        self.phase = 0
        self.phase_limit = None

    def newsem(self):
        self.nsem += 1
        return self.nc.alloc_semaphore(name=f"s{self.nsem}")

    def op(self, eng, fn, R=(), W=(), kind="c"):
        if not self.enabled:
            return None
        ins = Ins(eng, fn, kind)
        for b in R:
            if b.lw is not None:
                ins.deps.add(b.lw)
        for b in W:
            if b.lw is not None:
                ins.deps.add(b.lw)
            for r in b.readers.values():
                ins.deps.add(r)
            for r in b.dma_readers:
                ins.deps.add(r)
        ins.deps.discard(ins)
        for b in R:
            if kind == "d":
                b.dma_readers.append(ins)
            else:
                b.readers[eng] = ins
        for b in W:
            b.lw = ins
            b.readers = {}
            b.dma_readers = []
        if kind == "d":
            tgt = W[0]
            if tgt.dsem is None:
                if self.free_dsems:
                    tgt.dsem, tgt.dcount = self.free_dsems.pop()
                else:
                    tgt.dsem = self.newsem()
            tgt.dcount += 16
            ins.dsem = tgt.dsem
            ins.dcount = tgt.dcount
            self.dmas_since_bar.append(ins)
        elif kind == "cc":
            tgt = W[0]
            if tgt.dsem is None:
                tgt.dsem = self.newsem()
            tgt.dcount += 1
            ins.dsem = tgt.dsem
            ins.dcount = tgt.dcount
            self.dmas_since_bar.append(ins)
        self.q[eng].append(ins)
        if kind == "c":
            self.last[eng] = ins
        return ins

    def barrier(self):
        if not self.enabled:
            return
        deps = set(x for x in self.last.values() if x is not None) | set(self.dmas_since_bar)
        self.dmas_since_bar = []
        for e in ENGS:
            ins = Ins(e, None, "c")
            ins.deps = set(deps)
            self.q[e].append(ins)

    def finalize(self):
        for e in ENGS:
            for ins in self.q[e]:
                for d in ins.deps:
                    if d.kind == "c":
                        if d.eng == "pe" and ins.eng == "pe" and ins.kind == "c":
                            continue
                        d.need = True
        self.engsem = {}
        for e in ENGS:
            cur = None
            cnt = 0
            for ins in self.q[e]:
                if ins.kind == "c" and ins.need and ins.fn is not None:
                    if cur is None or cnt >= 20000:
                        cur = self.newsem()
                        cnt = 0
                    cnt += 1
                    ins.sig = (cur, cnt)
        for e in ENGS:
            waited = {}
            for ins in self.q[e]:
                w = {}
                for d in ins.deps:
                    if d.kind == "c":
                        if d.eng == "pe" and ins.eng == "pe" and ins.kind == "c":
                            continue
                        if d.sig is None:
                            continue
                        sem, cnt = d.sig
                    else:
                        sem, cnt = d.dsem, d.dcount
                    k = id(sem)
                    if waited.get(k, (None, 0))[1] >= cnt:
                        continue
                    if k not in w or w[k][1] < cnt:
                        w[k] = (sem, cnt)
                for k, v in w.items():
                    waited[k] = v
                ins.waits = list(w.values())

    def emit(self, e, eng):
        for ins in self.q[eng]:
            for sem, cnt in ins.waits:
                e.wait_ge(sem, cnt)
            if ins.fn is None:
                continue
            r = ins.fn(e)
            if ins.kind == "d":
                r.then_inc(ins.dsem, 16)
            elif ins.kind == "cc":
                r.then_inc(ins.dsem)
            elif ins.sig is not None:
                r.then_inc(ins.sig[0], 1)


def build_program(n_layers=DEPTH, stop_after=None):
    del PHASE_BUFS[:]
    nc = bass.Bass("TRN2", target_bir_lowering=False)
    S = Sched(nc)
    S.phase_limit = stop_after

    ODD = ("w_in_odd", "w_out_odd", "w_router", "moe_w1", "moe_w3", "moe_w2")

    def din(name, shape, dt=F32):
        if name in ODD and n_layers < 2:
            return None
        return nc.dram_tensor(name, list(shape), dt, kind="ExternalInput").ap()

    xT_d = din("xT", [D, NT])
    cT_d = din("cT", [128, KC, 2])
    n_even = (n_layers + 1) // 2
    n_odd = max(n_layers // 2, 1)
    wmod_d = din("w_mod", [n_layers, D, 6 * D])
    bmod_d = din("b_modT", [128, 4, 48])
    w_in_even_d = din("w_in_even", [n_even, D, 2048])
    w_out_even_d = din("w_out_even", [n_even, D, D])
    ffn_w1_d = din("ffn_w1", [n_even, D, FFN])
    ffn_w3_d = din("ffn_w3", [n_even, D, FFN])
    ffn_w2_d = din("ffn_w2", [n_even, FFN, D])
    w_in_odd_d = din("w_in_odd", [n_odd, D, 3072])
    w_out_odd_d = din("w_out_odd", [n_odd, D, D])
    w_router_d = din("w_router", [n_odd, D, NEXP])
    moe_w1_d = din("moe_w1", [n_odd, NEXP, D, EXP])
    moe_w3_d = din("moe_w3", [n_odd, NEXP, D, EXP])
    moe_w2_d = din("moe_w2", [n_odd, NEXP, EXP, D])
    smallp_d = din("smallp", [128, 1024])
    cosT_d = din("cosT", [128, NT])
    sinT_d = din("sinT", [128, NT])
    cmat_d = din("cmat", [128, 6, 128])
    cs128_d = din("cs128", [128, 256])
    small_dft = stop_after is not None and stop_after < 4
    dftc_d = din("dftc", [1024 if small_dft else 8192, 2048], BF16)
    dfts_d = din("dfts", [1024 if small_dft else 8192, 2048], BF16)
    dftctx_d = din("dftctx", [64, 4, 2, 64])
    rtab_d = din("rtab", [128, 4, 128])
    rpos_d = din("rpos", [128, 4, 128])
    rdist_d = din("rdist", [128, 4, 68])
    rctx_d = din("rctx", [64, 4, 4, 64])
    yT_d = nc.dram_tensor("yT", [D, NLAT], F32, kind="ExternalOutput").ap()

    class XT:
        def __init__(self, name, rblocks, cblocks):
            self.rb = rblocks
            self.cb = cblocks
            self.ch = {}
            self.xb = Buf(name + "xb", keep=True)
            self.gb = Buf(name + "gb", keep=True)
            for i, (r0, rn) in enumerate(rblocks):
                for j, (c0, cw) in enumerate(cblocks):
                    xt_ = nc.dram_tensor(f"{name}x{i}_{j}", [rn, cw], BF16)
                    gt_ = nc.dram_tensor(f"{name}g{i}_{j}", [4 * rn, cw], BF16)
                    self.ch[(i, j)] = (xt_, gt_, self.xb, self.gb)

        def pieces(self, r0, n, c0, w):
            out = []
            for i, (b0, bn) in enumerate(self.rb):
                lo, hi = max(r0, b0), min(r0 + n, b0 + bn)
                if lo >= hi:
                    continue
                for j, (d0, dw) in enumerate(self.cb):
                    cl, chh = max(c0, d0), min(c0 + w, d0 + dw)
                    if cl >= chh:
                        continue
                    out.append((i, j, lo - b0, hi - lo, cl - d0, chh - cl, lo - r0, cl - c0))
            return out

        def wr(self, r0, n, c0, w):
            res = []
            for (i, j, br, nn, bc, ww, dr, dc) in self.pieces(r0, n, c0, w):
                xt_, gt_, xb_, gb_ = self.ch[(i, j)]
                res.append((xt_.ap()[br:br + nn, bc:bc + ww], xb_, dr, nn, dc, ww))
            return res

        def rd(self, rank, r0, n, c0, w, own=False):
            res = []
            for (i, j, br, nn, bc, ww, dr, dc) in self.pieces(r0, n, c0, w):
                xt_, gt_, xb_, gb_ = self.ch[(i, j)]
                if own:
                    res.append((xt_.ap()[br:br + nn, bc:bc + ww], xb_, dr, nn, dc, ww))
                else:
                    rn = self.rb[i][1]
                    res.append((gt_.ap()[rank * rn + br:rank * rn + br + nn, bc:bc + ww], gb_, dr, nn, dc, ww))
            return res

    TTB = [(t0, n) for (t0, n) in [(0, 64), (64, 512), (576, 512), (1088, 512), (1600, 512)]]
    ex = {}
    for L in range(n_layers):
        if L % 2 == 1:
            ex[L] = dict(KT=XT(f"KT{L}", [(i * 256, 256) for i in range(4)], TTB),
                         V=XT(f"V{L}", TTB, [(i * 256, 256) for i in range(4)]))
        else:
            ex[L] = dict(E=XT(f"E{L}", TTB, [(i * 256, 256) for i in range(7)]),
                         Kc=XT(f"Kc{L}", [(0, 256)], [(0, 64)]))

    def sb(name, shape, dt):
        return nc.alloc_sbuf_tensor(name, list(shape), dt)

    xT = sb("xT_s", [128, KC, NT], F32)
    bufA = sb("bufA", [128, KC, NT], BF16)
    bufB = sb("bufB", [128, KC, NT], BF16)
    xB = [Buf(f"x{t}", keep=True) for t in range(5)]
    aB = [Buf(f"a{t}", keep=True) for t in range(5)]
    bB = [Buf(f"b{t}", keep=True) for t in range(5)]
    modt = sb("modt", [128, 4, 48, 2], F32)
    modB = Buf("mod", keep=True)
    smallp = sb("smallp_s", [128, 1024], F32)
    smB = Buf("smallp", keep=True)
    cmat_f = sb("cmat_f", [128, 6, 128], F32)
    cmat_b = sb("cmat_b", [128, 6, 128], BF16)
    cmB = Buf("cmat", keep=True)
    ONES_B = cmat_b[:, 0, :]
    BD64_B = cmat_b[:, 1, :]
    ID_B = cmat_b[:, 2, :]
    PERM_B = cmat_b[:, 3, :]
    ONES_F = cmat_f[:, 0, :]
    ID_F = cmat_f[:, 2, :]
    ARENA = 66000
    comb = sb("comb_s", [128, 17, 8], F32)
    combB = Buf("comb", keep=True)
    arena = sb("arena", [128, ARENA // 2], BF16)

    class Arena:
        def __init__(self, backing=None, cap=None):
            self.off = 0
            self.backing = backing
            self.cap = cap

        def reset(self):
            self.off = 0

        def take(self, shape, dt):
            n = int(np.prod(shape[1:]))
            nb = n * (2 if dt == BF16 else 4)
            nb_al = (nb + 63) // 64 * 64
            bk = arena if self.backing is None else self.backing
            cap = ARENA if self.cap is None else self.cap
            assert self.off + nb_al <= cap, (self.off, nb_al, cap)
            v = bk[0:shape[0], self.off // 2:(self.off + nb) // 2]
            if dt == F32:
                v = v.bitcast(F32)
            self.off += nb_al
            if len(shape) == 3:
                v = v.rearrange("p (a b) -> p a b", a=shape[1])
            elif len(shape) == 4:
                v = v.rearrange("p (a b c) -> p a b c", a=shape[1], b=shape[2])
            return v

    AR = Arena()
    bufA_flat = bufA[:, :, :].rearrange("p a b -> p (a b)")
    bufB_flat = bufB[:, :, :].rearrange("p a b -> p (a b)")
    AR2 = Arena(bufA_flat, KC * NT * 2)
    AR3 = Arena(bufB_flat, 4 * NT * 2)
    rope_state = {}

    def load_rope():
        c_ = AR.take([128, NT], F32)
        s_ = AR.take([128, NT], F32)
        b_ = Buf("rope")
        dma("sp", c_, cosT_d[:, :], R=[], W=[b_])
        dma("sp", s_, sinT_d[:, :], R=[], W=[b_])
        rope_state["c"], rope_state["s"], rope_state["b"] = c_, s_, b_
    psum = [nc.alloc_psum_tensor(f"ps{i}", [128, 512], F32) for i in range(8)]
    psB = [Buf(f"ps{i}", keep=True) for i in range(8)]

    class Ring:
        def __init__(self, n, mk):
            self.items = [mk(i) for i in range(n)]
            self.i = 0

        def next(self):
            it = self.items[self.i % len(self.items)]
            self.i += 1
            return it

    def new_phase():
        S.phase += 1
        if S.phase_limit is not None and S.phase > S.phase_limit:
            S.enabled = False
        S.barrier()
        for b_ in PHASE_BUFS:
            if b_.dsem is not None:
                S.free_dsems.append((b_.dsem, b_.dcount))
        del PHASE_BUFS[:]
        AR.reset()

    def mm(out, lhsT, rhs, start, stop, R, W):
        S.op("pe", lambda e: e.matmul(out, lhsT, rhs, start=start, stop=stop), R=R, W=W)

    def act(out, in_, func, R, W, bias=None, scale=None):
        kw = {}
        if bias is not None:
            kw["bias"] = bias
        if scale is not None:
            kw["scale"] = scale
        S.op("act", lambda e: e.activation(out=out, in_=in_, func=func, **kw), R=R, W=W)

    def tt(eng, out, in0, in1, op, R, W):
        S.op(eng, lambda e: e.tensor_tensor(out=out, in0=in0, in1=in1, op=op), R=R, W=W)

    def ts(eng, out, in0, s1, s2, op0, op1, R, W):
        if s2 is None:
            S.op(eng, lambda e: e.tensor_scalar(out=out, in0=in0, scalar1=s1, scalar2=None, op0=op0), R=R, W=W)
        else:
            S.op(eng, lambda e: e.tensor_scalar(out=out, in0=in0, scalar1=s1, scalar2=s2, op0=op0, op1=op1), R=R, W=W)

    def stt(eng, out, in0, scalar, in1, op0, op1, R, W):
        S.op(eng, lambda e: e.scalar_tensor_tensor(out=out, in0=in0, scalar=scalar, in1=in1, op0=op0, op1=op1), R=R, W=W)

    def recip(out, in_, R, W):
        S.op("dve", lambda e: e.reciprocal(out=out, in_=in_), R=R, W=W)

    def copy(eng, out, in_, R, W):
        if eng == "act":
            S.op("act", lambda e: e.copy(out=out, in_=in_), R=R, W=W)
        else:
            S.op(eng, lambda e: e.tensor_copy(out=out, in_=in_), R=R, W=W)

    def dma(q, out, in_, R, W):
        S.op(q, lambda e: e.dma_start(out=out, in_=in_), R=R, W=W, kind="d")

    def rstd_from_ps(ps_ap, psbuf, out_ap, outbuf, n_inv):
        act(out_ap, ps_ap, AF.Sqrt, R=[psbuf], W=[outbuf], bias=epsb[:, 0:1], scale=n_inv)
        recip(out_ap, out_ap, R=[outbuf], W=[outbuf])

    epsb = sb("epsb", [128, 2], F32)
    epsB = Buf("eps", keep=True)
    S.op("dve", lambda e: e.memset(epsb[:, 0:1], EPS), W=[epsB])
    S.op("dve", lambda e: e.memset(epsb[:, 1:2], 1.0), W=[epsB])
    for kc in range(KC):
        dma("sp", xT[:, kc, :], xT_d[kc * 128:(kc + 1) * 128, :], R=[], W=xB)
    dma("sp", smallp[:], smallp_d[:, :], R=[], W=[smB])
    dma("sp", cmat_f[:], cmat_d[:, :, :], R=[], W=[cmB])
    dma("pool", cmat_b[:], cmat_d[:, :, :], R=[], W=[cmB])

    SP_DEC = 0
    SP_QG = 16
    SP_KG = 18
    SP_SUB = 20
    SP_LAM = 32
    SP_LG = 600
    SP_LAMNEG = 620
    SP_SUBG = 624
    SP_G128 = 630
    SP_KG8 = 650
    SP_TMP = 700

    act(smallp[:, SP_LG:SP_LG + 16], smallp[:, SP_DEC:SP_DEC + 16], AF.Exp, R=[smB], W=[smB], scale=-1.0)
    act(smallp[:, SP_LG:SP_LG + 16], smallp[:, SP_LG:SP_LG + 16], AF.Ln, R=[smB, epsB], W=[smB], bias=epsb[:, 1:2])
    ts("dve", smallp[:, SP_LG:SP_LG + 16], smallp[:, SP_LG:SP_LG + 16], -1.0, None, ALU.mult, None, R=[smB], W=[smB])
    act(smallp[:, SP_G128:SP_G128 + 16], smallp[:, SP_LG:SP_LG + 16], AF.Exp, R=[smB], W=[smB], scale=128.0)
    for i in range(2):
        layer = 2 * i + 1
        lam_init = 0.8 - 0.6 * float(np.exp(-0.3 * layer))
        base = SP_LAM + i * 256
        for j in range(2):
            tt("dve", smallp[:, SP_TMP:SP_TMP + 64], smallp[:, base + j * 128:base + j * 128 + 64],
               smallp[:, base + j * 128 + 64:base + j * 128 + 128], ALU.mult, R=[smB], W=[smB])
            S.op("dve", lambda e, j=j: e.reduce_sum(out=smallp[:, SP_TMP + 64 + j:SP_TMP + 65 + j],
                                                   in_=smallp[:, SP_TMP:SP_TMP + 64], axis=AX.X), R=[smB], W=[smB])
        act(smallp[:, SP_TMP + 64:SP_TMP + 66], smallp[:, SP_TMP + 64:SP_TMP + 66], AF.Exp, R=[smB], W=[smB])
        tt("dve", smallp[:, SP_LAMNEG + i:SP_LAMNEG + i + 1], smallp[:, SP_TMP + 65:SP_TMP + 66],
           smallp[:, SP_TMP + 64:SP_TMP + 65], ALU.subtract, R=[smB], W=[smB])
        ts("dve", smallp[:, SP_LAMNEG + i:SP_LAMNEG + i + 1], smallp[:, SP_LAMNEG + i:SP_LAMNEG + i + 1],
           -lam_init, None, ALU.add, None, R=[smB], W=[smB])
        ts("dve", smallp[:, SP_SUBG + i:SP_SUBG + i + 1], smallp[:, SP_SUB + i:SP_SUB + i + 1],
           1.0 - lam_init, None, ALU.mult, None, R=[smB], W=[smB])

    AR.reset()
    csil_f = AR.take([128, KC, 2], F32)
    csil = AR.take([128, KC, 2], BF16)
    bmod_s = AR.take([128, 4, 48], F32)
    cB = Buf("csil")
    dma("sp", csil_f, cT_d[:, :, :], R=[], W=[cB])
    dma("sp", bmod_s, bmod_d[:, :, :], R=[], W=[cB])
    act(csil, csil_f, AF.Silu, R=[cB], W=[cB])
    wring = Ring(3, lambda i: (AR.take([128, KC, 1024], BF16), Buf(f"wm{i}")))
    pr = Ring(2, lambda i: i)
    for L in range(n_layers):
        for blk in range(6):
            wt, wb = wring.next()
            dma("pool", wt, wmod_d[L, :, blk * 1024:(blk + 1) * 1024].rearrange("(k p) n -> p k n", p=128), R=[], W=[wb])
            pi = pr.next()
            for m in range(8):
                for kc in range(KC):
                    mm(psum[pi][:, 2 * m:2 * m + 2], wt[:, kc, m * 128:(m + 1) * 128], csil[:, kc, :],
                       kc == 0, kc == KC - 1, R=[wb, cB], W=[psB[pi]])
            for col in range(2):
                tt("dve", modt[:, L, blk * 8:(blk + 1) * 8, col],
                   psum[pi][:, 0:16].rearrange("p (m c) -> p m c", c=2)[:, :, col],
                   bmod_s[:, L, blk * 8:(blk + 1) * 8], ALU.add, R=[psB[pi], cB], W=[modB])
        for which in (1, 4):
            ts("dve", modt[:, L, which * 8:(which + 1) * 8, :], modt[:, L, which * 8:(which + 1) * 8, :],
               1.0, None, ALU.add, None, R=[modB], W=[modB])

    def modap(L, which, kc, col):
        return modt[:, L, which * 8 + kc, col:col + 1]

    def modulate(L, w_shift, w_scale, tiles, hook=None):
        sq = Ring(2, lambda i: (AR.take([128, 512], BF16), Buf(f"msq{i}")))
        rs = Ring(2, lambda i: (AR.take([128, 512], F32), Buf(f"mrs{i}")))
        tmp = Ring(2, lambda i: (AR.take([128, 512], F32), Buf(f"mtmp{i}")))
        pring = Ring(2, lambda i: i)
        for ti in tiles:
            t0, n = TT[ti]
            col = 1 if ti == 0 else 0
            pi = pring.next()
            for kc in range(KC):
                sqt, sqb = sq.next()
                act(sqt[:, :n], xT[:, kc, t0:t0 + n], AF.Square, R=[xB[ti]], W=[sqb])
                mm(psum[pi][:, :n], ONES_B, sqt[:, :n], kc == 0, kc == KC - 1, R=[sqb, cmB], W=[psB[pi]])
            rst, rsb = rs.next()
            rstd_from_ps(psum[pi][:, :n], psB[pi], rst[:, :n], rsb, 1.0 / D)
            for kc in range(KC):
                tmt, tmb = tmp.next()
                tt("dve", tmt[:, :n], xT[:, kc, t0:t0 + n], rst[:, :n], ALU.mult, R=[xB[ti], rsb], W=[tmb])
                if hook is not None:
                    hook(ti, kc, tmt, tmb, n, col)
                act(bufA[:, kc, t0:t0 + n], tmt[:, :n], AF.Identity, R=[tmb, modB], W=[aB[ti]],
                    bias=modap(L, w_shift, kc, col), scale=modap(L, w_scale, kc, col))

    def wload(dst, dstb, src2d, k0, ncols_total, c0, ncols):
        nk = dst.shape[1]
        dma("pool", dst, src2d[k0 * 128:(k0 + nk) * 128, c0:c0 + ncols].rearrange("(k p) n -> p k n", p=128), R=[], W=[dstb])

    def rope(src, srcb, n, t0, out_ap, outb, ps_i, tmpring):
        mm(psum[ps_i][:, :n], PERM_B, src[:, :n], True, True, R=[srcb, cmB], W=[psB[ps_i]])
        t1, t1b = tmpring.next()
        t2, t2b = tmpring.next()
        cosT, sinT, ropeB = rope_state["c"], rope_state["s"], rope_state["b"]
        tt("pool", t1[:, :n], src[:, :n], cosT[:, t0:t0 + n], ALU.mult, R=[srcb, ropeB], W=[t1b])
        tt("dve", t2[:, :n], psum[ps_i][:, :n], sinT[:, t0:t0 + n], ALU.mult, R=[psB[ps_i], ropeB], W=[t2b])
        tt("dve", out_ap, t1[:, :n], t2[:, :n], ALU.add, R=[t1b, t2b], W=[outb])

    def out_proj_residual(L, wsrc, tiles):
        wr = Ring(2, lambda i: (AR.take([128, KC, 512], BF16), Buf(f"wo{i}")))
        pring = Ring(4, lambda i: i)
        slots = []
        for blk in range(2):
            wt, wb = wr.next()
            wload(wt, wb, wsrc, 0, D, blk * 512, 512)
            slots.append((wt, wb))
        for blk in range(2):
            wt, wb = slots[blk]
            for ti in tiles:
                t0, n = TT[ti]
                col = 1 if ti == 0 else 0
                for m in range(4):
                    o = blk * 4 + m
                    pi = pring.next()
                    for kc in range(KC):
                        mm(psum[pi][:, :n], wt[:, kc, m * 128:(m + 1) * 128], bufB[:, kc, t0:t0 + n],
                           kc == 0, kc == KC - 1, R=[wb, bB[ti]], W=[psB[pi]])
                    stt("dve", xT[:, o, t0:t0 + n], psum[pi][:, :n], modap(L, 2, o, col), xT[:, o, t0:t0 + n],
                        ALU.mult, ALU.add, R=[psB[pi], modB, xB[ti]], W=[xB[ti]])

    def swiglu_jobs(L, jobs, blkw, tiles, rings):
        wr, gr, sr = rings
        nm = blkw // 128

        def load(job):
            w1src, w3src, w2src, j, cw, cwb = job[:6]
            if len(job) > 6 and job[6] is not None:
                job[6]()
            (w1t, w3t, w2t), wb = wr.next()
            wload(w1t, wb, w1src, 0, 0, j * blkw, blkw)
            wload(w3t, wb, w3src, 0, 0, j * blkw, blkw)
            dma("pool", w2t, w2src[j * blkw:(j + 1) * blkw, :].rearrange("(k p) n -> p k n", p=128), R=[], W=[wb])
            return (w1t, w3t, w2t, wb)

        def stage_ab(w, ti, cw, cwb):
            w1t, w3t, w2t, wb = w
            t0, n = TT[ti]
            gt, gb = gr.next()
            for m in range(nm):
                pa = m % 2
                pb = 2 + m % 2
                for kc in range(KC):
                    mm(psum[pa][:, :n], w1t[:, kc, m * 128:(m + 1) * 128], bufA[:, kc, t0:t0 + n],
                       kc == 0, kc == KC - 1, R=[wb, aB[ti]], W=[psB[pa]])
                for kc in range(KC):
                    mm(psum[pb][:, :n], w3t[:, kc, m * 128:(m + 1) * 128], bufA[:, kc, t0:t0 + n],
                       kc == 0, kc == KC - 1, R=[wb, aB[ti]], W=[psB[pb]])
                st, sbf = sr.next()
                act(st[:, :n], psum[pa][:, :n], AF.Silu, R=[psB[pa]], W=[sbf])
                if cw is None:
                    tt("dve", gt[:, m, :n], st[:, :n], psum[pb][:, :n], ALU.mult, R=[sbf, psB[pb]], W=[gb])
                else:
                    tt("dve", st[:, :n], st[:, :n], psum[pb][:, :n], ALU.mult, R=[sbf, psB[pb]], W=[sbf])
                    tt("dve", gt[:, m, :n], st[:, :n], cw[:, t0:t0 + n], ALU.mult, R=[sbf, cwb], W=[gb])
            return (gt, gb)

        def stage_w2(w, ti, g):
            w1t, w3t, w2t, wb = w
            gt, gb = g
            t0, n = TT[ti]
            col = 1 if ti == 0 else 0
            for o in range(KC):
                py = 4 + o % 4
                for m in range(nm):
                    mm(psum[py][:, :n], w2t[:, m, o * 128:(o + 1) * 128], gt[:, m, :n],
                       m == 0, m == nm - 1, R=[wb, gb], W=[psB[py]])
                stt("dve", xT[:, o, t0:t0 + n], psum[py][:, :n], modap(L, 5, o, col), xT[:, o, t0:t0 + n],
                    ALU.mult, ALU.add, R=[psB[py], modB, xB[ti]], W=[xB[ti]])

        nxt = load(jobs[0])
        pend = None
        for ji, job in enumerate(jobs):
            w = nxt
            if pend is not None:
                stage_w2(*pend)
                pend = None
            if ji + 1 < len(jobs):
                nxt = load(jobs[ji + 1])
            for ti in tiles:
                g = stage_ab(w, ti, job[4], job[5])
                if pend is not None:
                    stage_w2(*pend)
                pend = (w, ti, g)
        if pend is not None:
            stage_w2(*pend)

    def ffn_rings(blkw, nslots):
        nm = blkw // 128
        wr = Ring(nslots, lambda i: ((AR.take([128, KC, blkw], BF16), AR.take([128, KC, blkw], BF16),
                                      AR.take([128, nm, D], BF16)), Buf(f"fw{i}")))
        gr = Ring(2, lambda i: (AR.take([128, nm, 512], BF16), Buf(f"g{i}")))
        sr = Ring(3, lambda i: (AR.take([128, 512], F32), Buf(f"s{i}")))
        return wr, gr, sr

    AG = [[0, 1, 2, 3], [4, 5, 6, 7]]

    def allgather(xt):
        for key, (xt_, gt_, xb_, gb_) in xt.ch.items():
            S.op("pool", lambda e, xt_=xt_, gt_=gt_: e.collective_compute(
                "AllGather", ALU.bypass, replica_groups=AG, ins=[xt_.ap().opt()], outs=[gt_.ap().opt()]),
                R=[xb_], W=[gb_], kind="cc")

    def xstore(xt, r0, n, c0, w, src, srcb):
        for (ap_, buf_, dr, nn, dc, ww) in xt.wr(r0, n, c0, w):
            dma("sp", ap_, src[dr:dr + nn, dc:dc + ww], R=[srcb], W=[buf_])

    def xload_rows(xt, rank, r0, n, c0, w, dst, dstb, own=False):
        for (ap_, buf_, dr, nn, dc, ww) in xt.rd(rank, r0, n, c0, w, own):
            dma("sp", dst[dr:dr + nn, dc:dc + ww], ap_, R=[buf_], W=[dstb])

    def xload_tiles(xt, rank, r0, n, c0, w, dst, dstb, own=False):
        for (ap_, buf_, dr, nn, dc, ww) in xt.rd(rank, r0, n, c0, w, own):
            assert dr % 128 == 0 and nn % 128 == 0
            dma("sp", dst[:, dr // 128:(dr + nn) // 128, dc:dc + ww], ap_.rearrange("(t p) c -> p t c", p=128),
                R=[buf_], W=[dstb])

    def v_project(wt, wb, dst, c0, stg):
        for (t0, n) in TM:
            ti = 0 if t0 == 0 else 1 + (t0 - 64) // 512
            p0 = 6 + (t0 // 128) % 2
            for kc in range(KC):
                mm(psum[p0][:n, :], bufA[:, kc, t0:t0 + n], wt[:, kc, :], kc == 0, kc == KC - 1,
                   R=[wb, aB[ti]], W=[psB[p0]])
            vt, vb2 = stg.next()
            copy("act", vt[:n, :], psum[p0][:n, :], R=[psB[p0]], W=[vb2])
            xstore(dst, t0, n, c0, 512, vt, vb2)

    for L in range(n_layers):
        i2 = L // 2
        last = L == DEPTH - 1
        all_tiles = [0, 1, 2, 3, 4]
        lat_tiles = [1, 2, 3, 4]
        if L % 2 == 1:
            X = ex[L]
            XKT, XV = X["KT"], X["V"]
            new_phase()
            modulate(L, 0, 1, all_tiles)
            new_phase()
            load_rope()
            wr = Ring(2, lambda i: (AR.take([128, KC, 512], BF16), Buf(f"wq{i}")))
            raw = Ring(2, lambda i: (AR.take([128, 512], F32), Buf(f"raw{i}")))
            sqr = Ring(2, lambda i: (AR.take([128, 512], BF16), Buf(f"sq{i}")))
            rsr = Ring(2, lambda i: (AR.take([128, 512], F32), Buf(f"rs{i}")))
            qnr = Ring(2, lambda i: (AR.take([128, 512], BF16), Buf(f"qn{i}")))
            tmpr = Ring(4, lambda i: (AR.take([128, 512], F32), Buf(f"rt{i}")))
            kst = Ring(3, lambda i: (AR.take([128, 512], BF16), Buf(f"kst{i}")))

            def ld_odd(blk):
                wt, wb = wr.next()
                wload(wt, wb, w_in_odd_d[i2], 0, 3072, blk * 512, 512)
                return wt, wb

            nxt = ld_odd(0)
            for blk in range(6):
                wt, wb = nxt
                if blk + 1 < 6:
                    nxt = ld_odd(blk + 1)
                if blk < 4:
                    isq = blk < 2
                    gcol = (SP_QG if isq else SP_KG) + i2
                    for ti in all_tiles:
                        t0, n = TT[ti]
                        for m in range(4):
                            ch = (blk % 2) * 4 + m
                            p0 = m % 2
                            for kc in range(KC):
                                mm(psum[p0][:, :n], wt[:, kc, m * 128:(m + 1) * 128], bufA[:, kc, t0:t0 + n],
                                   kc == 0, kc == KC - 1, R=[wb, aB[ti]], W=[psB[p0]])
                            rt, rb = raw.next()
                            copy("act", rt[:, :n], psum[p0][:, :n], R=[psB[p0]], W=[rb])
                            st, sbf = sqr.next()
                            act(st[:, :n], rt[:, :n], AF.Square, R=[rb], W=[sbf])
                            p1 = 2 + m % 2
                            mm(psum[p1][:, :n], BD64_B, st[:, :n], True, True, R=[sbf, cmB], W=[psB[p1]])
                            rst, rsb = rsr.next()
                            rstd_from_ps(psum[p1][:, :n], psB[p1], rst[:, :n], rsb, 1.0 / 64)
                            qt, qb = qnr.next()
                            stt("dve", qt[:, :n], rt[:, :n], smallp[:, gcol:gcol + 1], rst[:, :n], ALU.mult, ALU.mult,
                                R=[rb, rsb, smB], W=[qb])
                            p2 = 4 + m % 2
                            if isq:
                                rope(qt, qb, n, t0, bufB[:, ch, t0:t0 + n], bB[ti], p2, tmpr)
                            else:
                                kt, kb = kst.next()
                                rope(qt, qb, n, t0, kt[:, :n], kb, p2, tmpr)
                                xstore(XKT, ch * 128, 128, t0, n, kt, kb)
                else:
                    v_project(wt, wb, XV, (blk - 4) * 512, kst)
            allgather(XKT)
            allgather(XV)
            new_phase()
            kslot = AR.take([128, 4, NT], BF16)
            ksB = Buf("kslot")
            vslot = AR.take([128, 4, 17, 128], BF16)
            vsB = Buf("vslot")
            ering = Ring(6, lambda i: (AR.take([128, 512], BF16), Buf(f"e{i}")))
            ftmp = Ring(4, lambda i: (AR.take([128, 512], F32), Buf(f"ft{i}")))
            fsq = Ring(2, lambda i: (AR.take([128, 512], BF16), Buf(f"fsq{i}")))
            sring = Ring(4, lambda i: i)
            qtiles = lat_tiles if last else all_tiles
            LA = 2
            for h in range(8):
                for r in range(4):
                    xload_rows(XKT, r, h * 128, 128, 0, NT, kslot[:, r, :], ksB)
                    xload_rows(XV, r, 0, 64, h * 128, 128, vslot[:, r, 0, :], vsB)
                    xload_tiles(XV, r, 64, NLAT, h * 128, 128, vslot[:, r, 1:17, :], vsB)
                for ti in qtiles:
                    t0, n = TT[ti]
                    if ti == 0:
                        ktl = [(r, 0, 0, 64) for r in range(4)]
                    else:
                        ktl = []
                        for r in range(4):
                            ktl.append((r, 0, 0, 64))
                            for j in range(16):
                                ktl.append((r, 1 + j, 64 + 128 * j, 128))
                    steps = [(ki, m) for ki in range(len(ktl)) for m in range(2)]
                    nk = len(ktl)
                    ets = {}
                    for s_ in range(len(steps) + LA):
                        if s_ < len(steps):
                            ki, m = steps[s_]
                            r, vt_i, k0, kn = ktl[ki]
                            si = sring.next()
                            mm(psum[si][:kn, :n], kslot[m * 64:(m + 1) * 64, r, k0:k0 + kn],
                               bufB[m * 64:(m + 1) * 64, h, t0:t0 + n], True, True, R=[ksB, bB[ti]], W=[psB[si]])
                            et, eb = ering.next()
                            act(et[:kn, :n], psum[si][:kn, :n], AF.Exp, R=[psB[si]], W=[eb], scale=0.125)
                            ets[s_] = (et, eb)
                        if s_ >= LA:
                            ki, m = steps[s_ - LA]
                            r, vt_i, k0, kn = ktl[ki]
                            et, eb = ets.pop(s_ - LA)
                            mm(psum[4 + m][:, :n], vslot[:kn, r, vt_i, :], et[:kn, :n], ki == 0, ki == nk - 1,
                               R=[vsB, eb], W=[psB[4 + m]])
                            mm(psum[6 + m][:, :n], ONES_B[:kn, :], et[:kn, :n], ki == 0, ki == nk - 1,
                               R=[cmB, eb], W=[psB[6 + m]])
                    r1, r1b = ftmp.next()
                    r2, r2b = ftmp.next()
                    recip(r1[:, :n], psum[6][:, :n], R=[psB[6]], W=[r1b])
                    recip(r2[:, :n], psum[7][:, :n], R=[psB[7]], W=[r2b])
                    tt("dve", r1[:, :n], psum[4][:, :n], r1[:, :n], ALU.mult, R=[psB[4], r1b], W=[r1b])
                    tt("dve", r2[:, :n], psum[5][:, :n], r2[:, :n], ALU.mult, R=[psB[5], r2b], W=[r2b])
                    o_, ob = ftmp.next()
                    stt("dve", o_[:, :n], r2[:, :n], smallp[:, SP_LAMNEG + i2:SP_LAMNEG + i2 + 1], r1[:, :n],
                        ALU.mult, ALU.add, R=[r1b, r2b, smB], W=[ob])
                    sq_, sqb_ = fsq.next()
                    act(sq_[:, :n], o_[:, :n], AF.Square, R=[ob], W=[sqb_])
                    si = sring.next()
                    mm(psum[si][:, :n], ONES_B, sq_[:, :n], True, True, R=[sqb_, cmB], W=[psB[si]])
                    rs_, rsb_ = ftmp.next()
                    rstd_from_ps(psum[si][:, :n], psB[si], rs_[:, :n], rsb_, 1.0 / 128)
                    tt("dve", o_[:, :n], o_[:, :n], rs_[:, :n], ALU.mult, R=[ob, rsb_], W=[ob])
                    ts("dve", bufB[:, h, t0:t0 + n], o_[:, :n], smallp[:, SP_SUBG + i2:SP_SUBG + i2 + 1], None,
                       ALU.mult, None, R=[ob, smB], W=[bB[ti]])
            new_phase()
            out_proj_residual(L, w_out_odd_d[i2], qtiles)
            new_phase()
            wrt = AR.take([128, KC, 8], F32)
            wrtB = Buf("wrt")
            dma("sp", wrt, w_router_d[i2].rearrange("(k p) n -> p k n", p=128), R=[], W=[wrtB])
            hf32 = AR.take([128, KC, 512], F32)
            hfB = Buf("hf32")
            rl = AR.take([128, 17, 8], F32)
            rlB = Buf("rl")
            mx8 = AR.take([128, 8], F32)
            ex8 = AR.take([128, 8], F32)
            nb1 = AR.take([128, 2], F32)
            ffn_tiles = lat_tiles if last else all_tiles
            sqm = Ring(2, lambda i: (AR.take([128, 512], BF16), Buf(f"msq{i}")))
            rsm = Ring(2, lambda i: (AR.take([128, 512], F32), Buf(f"mrs{i}")))
            tmpm = Ring(2, lambda i: (AR.take([128, 512], F32), Buf(f"mtmp{i}")))
            for ti in ffn_tiles:
                t0, n = TT[ti]
                col = 1 if ti == 0 else 0
                pi = 0
                for kc in range(KC):
                    sqt, sqb = sqm.next()
                    act(sqt[:, :n], xT[:, kc, t0:t0 + n], AF.Square, R=[xB[ti]], W=[sqb])
                    mm(psum[pi][:, :n], ONES_B, sqt[:, :n], kc == 0, kc == KC - 1, R=[sqb, cmB], W=[psB[pi]])
                rst, rsb = rsm.next()
                rstd_from_ps(psum[pi][:, :n], psB[pi], rst[:, :n], rsb, 1.0 / D)
                for kc in range(KC):
                    tmt, tmb = tmpm.next()
                    tt("dve", tmt[:, :n], xT[:, kc, t0:t0 + n], rst[:, :n], ALU.mult, R=[xB[ti], rsb], W=[tmb])
                    act(hf32[:, kc, :n], tmt[:, :n], AF.Identity, R=[tmb, modB], W=[hfB],
                        bias=modap(L, 3, kc, col), scale=modap(L, 4, kc, col))
                    copy("pool", bufA[:, kc, t0:t0 + n], hf32[:, kc, :n], R=[hfB], W=[aB[ti]])
                for s0 in range(0, n, 128):
                    sn = min(128, n - s0)
                    tmi = 0 if ti == 0 else 1 + (t0 + s0 - 64) // 128
                    for kc in range(KC):
                        mm(psum[1][:sn, 0:8], hf32[:, kc, s0:s0 + sn], wrt[:, kc, :], kc == 0, kc == KC - 1,
                           R=[hfB, wrtB], W=[psB[1]])
                    copy("dve", rl[:sn, tmi, :], psum[1][:sn, 0:8], R=[psB[1]], W=[rlB])
                    S.op("dve", lambda e, sn=sn, tmi=tmi: e.max(out=mx8[:sn, :], in_=rl[:sn, tmi, :]), R=[rlB], W=[rlB])
                    ts("dve", nb1[:sn, 0:1], mx8[:sn, 0:1], -1.0, None, ALU.mult, None, R=[rlB], W=[rlB])
                    act(ex8[:sn, :], rl[:sn, tmi, :], AF.Exp, R=[rlB], W=[rlB], bias=nb1[:sn, 0:1])
                    stt("dve", ex8[:sn, :], rl[:sn, tmi, :], mx8[:sn, 1:2], ex8[:sn, :], ALU.is_ge, ALU.mult,
                        R=[rlB], W=[rlB])
                    S.op("dve", lambda e, sn=sn: e.reduce_sum(out=nb1[:sn, 1:2], in_=ex8[:sn, :], axis=AX.X), R=[rlB], W=[rlB])
                    recip(nb1[:sn, 1:2], nb1[:sn, 1:2], R=[rlB], W=[rlB])
                    ts("dve", comb[:sn, tmi, :], ex8[:sn, :], nb1[:sn, 1:2], None, ALU.mult, None, R=[rlB], W=[combB])
            new_phase()
            cw_all = bufB_flat.bitcast(F32).rearrange("p (a b) -> p a b", a=4)
            cwr = Ring(4, lambda i: (cw_all[:, i, :], Buf(f"cw{i}")))
            dgr = Ring(2, lambda i: (AR.take([128, 128], F32), Buf(f"dg{i}")))
            rings = ffn_rings(512, 2)
            jobs = []
            for ex_i in range(NEXP):
                cw, cwb = cwr.next()

                def pre(ex_i=ex_i, cw=cw, cwb=cwb):
                    for (t0, n) in TM:
                        if last and t0 == 0:
                            continue
                        tmi = 0 if t0 == 0 else 1 + (t0 - 64) // 128
                        dg, dgb = dgr.next()
                        ts("pool", dg[:n, :n], ID_F[:n, :n], comb[:n, tmi, ex_i:ex_i + 1], None, ALU.mult, None,
                           R=[cmB, combB], W=[dgb])
                        mm(psum[6][:, :n], ONES_F[:n, :], dg[:n, :n], True, True, R=[dgb, cmB], W=[psB[6]])
                        copy("act", cw[:, t0:t0 + n], psum[6][:, :n], R=[psB[6]], W=[cwb])

                for j in range(EXP // 512):
                    jobs.append((moe_w1_d[i2, ex_i], moe_w3_d[i2, ex_i], moe_w2_d[i2, ex_i], j, cw, cwb,
                                 pre if j == 0 else None))
            swiglu_jobs(L, jobs, 512, ffn_tiles, rings)
        else:
            X = ex[L]
            XE, XKc = X["E"], X["Kc"]
            lgc = lambda d_, hd, i2=i2: smallp[:, SP_LG + i2 * 8 + d_ * 4 + hd:SP_LG + i2 * 8 + d_ * 4 + hd + 1]
            g128c = lambda d_, hd, i2=i2: smallp[:, SP_G128 + i2 * 8 + d_ * 4 + hd:SP_G128 + i2 * 8 + d_ * 4 + hd + 1]
            new_phase()
            modulate(L, 0, 1, all_tiles)
            new_phase()
            QrT = AR.take([128, 2, NT], BF16)
            KrT = AR.take([128, 2, NT], BF16)
            qrB = [Buf(f"qr{t}") for t in range(5)]
            krB = [Buf(f"kr{t}") for t in range(5)]
            keep_off = AR.off
            load_rope()
            wr = Ring(2, lambda i: (AR.take([128, KC, 512], BF16), Buf(f"we{i}")))
            cs128 = AR.take([128, 256], BF16)
            csB = Buf("cs128")
            dma("pool", cs128, cs128_d[:, :], R=[], W=[csB])
            qnr = Ring(2, lambda i: (AR.take([128, 512], BF16), Buf(f"qn{i}")))
            tmpr = Ring(4, lambda i: (AR.take([128, 512], F32), Buf(f"rt{i}")))
            stg = Ring(3, lambda i: (AR.take([128, 512], BF16), Buf(f"stg{i}")))
            fT = bufB

            def ld_even(blk):
                wt, wb = wr.next()
                wload(wt, wb, w_in_even_d[i2], 0, 2048, blk * 512, 512)
                return wt, wb

            nxt = ld_even(0)
            for blk in range(4):
                wt, wb = nxt
                if blk + 1 < 4:
                    nxt = ld_even(blk + 1)
                if blk == 2:
                    v_project(wt, wb, XE, 1280, stg)
                    continue
                for ti in all_tiles:
                    t0, n = TT[ti]
                    for m in range(4):
                        p0 = m % 2
                        for kc in range(KC):
                            mm(psum[p0][:, :n], wt[:, kc, m * 128:(m + 1) * 128], bufA[:, kc, t0:t0 + n],
                               kc == 0, kc == KC - 1, R=[wb, aB[ti]], W=[psB[p0]])
                        if blk == 0:
                            copy("act", fT[:, m, t0:t0 + n], psum[p0][:, :n], R=[psB[p0]], W=[bB[ti]])
                        elif blk == 3:
                            act(bufB[:, 4 + m, t0:t0 + n], psum[p0][:, :n], AF.Silu, R=[psB[p0]], W=[bB[ti]])
                        else:
                            qt, qb = qnr.next()
                            if m < 2:
                                copy("act", qt[:, :n], psum[p0][:, :n], R=[psB[p0]], W=[qb])
                                rope(qt, qb, n, t0, QrT[:, m, t0:t0 + n], qrB[ti], 4 + m % 2, tmpr)
                            else:
                                S.op("act", lambda e, qt=qt, p0=p0, n=n: e.mul(out=qt[:, :n], in_=psum[p0][:, :n], mul=0.125),
                                     R=[psB[p0]], W=[qb])
                                rope(qt, qb, n, t0, KrT[:, m - 2, t0:t0 + n], krB[ti], 4 + m % 2, tmpr)
            for (t0, n) in TM:
                ti = 0 if t0 == 0 else 1 + (t0 - 64) // 512
                for gp in range(2):
                    p0 = 6 + gp
                    for g2 in range(2):
                        g = gp * 2 + g2
                        mm(psum[p0][:n, g2 * 256:(g2 + 1) * 256], fT[:, g, t0:t0 + n], cs128, True, True,
                           R=[bB[ti], csB], W=[psB[p0]])
                    at, ab = stg.next()
                    copy("act" if gp == 0 else "dve", at[:n, :], psum[p0][:n, :], R=[psB[p0]], W=[ab])
                    xstore(XE, t0, n, gp * 512, 512, at, ab)
                p0 = 5
                for c2 in range(2):
                    mm(psum[p0][:n, c2 * 128:(c2 + 1) * 128], KrT[:, c2, t0:t0 + n], ID_B, True, True,
                       R=[krB[ti], cmB], W=[psB[p0]])
                kt, kb = stg.next()
                copy("dve", kt[:n, 0:256], psum[p0][:n, 0:256], R=[psB[p0]], W=[kb])
                xstore(XE, t0, n, 1024, 256, kt, kb)
            for c2 in range(2):
                xstore(XKc, c2 * 128, 128, 0, 64, KrT[:, c2, :], krB[0])
            allgather(XE)
            allgather(XKc)
            S.phase += 1
            if S.phase_limit is not None and S.phase > S.phase_limit:
                S.enabled = False
            S.barrier()
            AR.off = keep_off
            AR2.reset()
            AR3.reset()
            rtab = AR.take([128, 4, 128], F32)
            rpos = AR.take([128, 4, 128], F32)
            rdist = AR.take([128, 4, 68], F32)
            rctx = AR.take([64, 4, 4, 64], F32)
            rcB = Buf("rconst")
            dma("sp", rtab, rtab_d[:, :, :], R=[], W=[rcB])
            dma("sp", rpos, rpos_d[:, :, :], R=[], W=[rcB])
            dma("sp", rdist, rdist_d[:, :, :], R=[], W=[rcB])
            dma("sp", rctx, rctx_d[:, :, :, :], R=[], W=[rcB])
            Dc = AR.take([128, 4, 128], F32)
            dq = AR.take([128, 2, 4, 128], BF16)
            dk = AR.take([128, 2, 4], F32)
            wk = AR.take([128, 2, 4, 68], F32)
            Dx = AR.take([64, 4, 4, 64], F32)
            dcB = Buf("dconst")
            t1 = AR.take([128, 128], F32)
            for hd in range(4):
                act(Dc[:, hd, :], rtab[:, 0, :], AF.Exp, R=[rcB, smB], W=[dcB], scale=lgc(0, hd))
                tt("dve", Dc[:, hd, :], Dc[:, hd, :], rtab[:, 1, :], ALU.mult, R=[dcB, rcB], W=[dcB])
                act(t1[:, :], rtab[:, 2, :], AF.Exp, R=[rcB, smB, dcB], W=[dcB], scale=lgc(1, hd))
                tt("dve", t1[:, :], t1[:, :], rtab[:, 3, :], ALU.mult, R=[dcB, rcB], W=[dcB])
                tt("dve", Dc[:, hd, :], Dc[:, hd, :], t1[:, :], ALU.add, R=[dcB], W=[dcB])
                for d_ in range(2):
                    act(dq[:, d_, hd, :], rpos[:, d_, :], AF.Exp, R=[rcB, smB], W=[dcB], scale=lgc(d_, hd))
                    act(dk[:, d_, hd:hd + 1], rpos[:, 2, d_:d_ + 1], AF.Exp, R=[rcB, smB], W=[dcB], scale=lgc(d_, hd))
                    act(wk[:, d_, hd, :], rdist[:, 2 * d_, :], AF.Exp, R=[rcB, smB], W=[dcB], scale=lgc(d_, hd))
                    tt("dve", wk[:, d_, hd, :], wk[:, d_, hd, :], rdist[:, 2 * d_ + 1, :], ALU.mult, R=[dcB, rcB], W=[dcB])
                for r in range(4):
                    act(Dx[:, hd, r, :], rctx[:, 0, r, :], AF.Exp, R=[rcB, smB], W=[dcB], scale=lgc(0, hd)[0:64, :])
                    tt("dve", Dx[:, hd, r, :], Dx[:, hd, r, :], rctx[:, 1, r, :], ALU.mult, R=[dcB, rcB], W=[dcB])
                    act(t1[0:64, 0:64], rctx[:, 2, r, :], AF.Exp, R=[rcB, smB, dcB], W=[dcB], scale=lgc(1, hd)[0:64, :])
                    tt("dve", t1[0:64, 0:64], t1[0:64, 0:64], rctx[:, 3, r, :], ALU.mult, R=[dcB, rcB], W=[dcB])
                    tt("dve", Dx[:, hd, r, :], Dx[:, hd, r, :], t1[0:64, 0:64], ALU.add, R=[dcB], W=[dcB])
            kvr = Ring(2, lambda i: (AR.take([128, 4, 768], BF16), Buf(f"kv{i}")))
            k2r = Ring(4, lambda i: (AR.take([128, 128], BF16), Buf(f"k2{i}")))

            def scaled_pair(src_ap, pr_, scal, R_, eng0="dve", eng1="pool"):
                k2, k2b = k2r.next()
                n_ = src_ap.shape[0]
                for hh in range(2):
                    hd = pr_ * 2 + hh
                    ts(eng0 if hh == 0 else eng1, k2[:n_, hh * 64:(hh + 1) * 64], src_ap[:, hd * 64:(hd + 1) * 64],
                       scal(hd), None, ALU.mult, None, R=R_, W=[k2b])
                return k2, k2b

            gi = 0
            for r in range(4):
                groups = [(0, [0])] + [(1 + 4 * q, [1 + 4 * q + u for u in range(4)]) for q in range(4)]
                for (tl0, tls) in groups:
                    kv, kvb = kvr.next()
                    if tl0 == 0:
                        xload_rows(XE, r, 0, 64, 1024, 768, kv[:, 0, :], kvb)
                    else:
                        xload_tiles(XE, r, 64 + (tl0 - 1) * 128, 512, 1024, 768, kv[:, 0:4, :], kvb)
                    for u, tl in enumerate(tls):
                        n = 64 if tl == 0 else 128
                        gi = r * 17 + tl
                        for d_ in range(2):
                            for pr_ in range(2):
                                k2, k2b = scaled_pair(kv[:n, u, :], pr_, lambda hd, d_=d_, gi=gi, n=n: wk[:n, d_, hd, gi:gi + 1], [kvb, dcB])
                                for hh in range(2):
                                    hd = pr_ * 2 + hh
                                    mm(psum[4 + d_][:, hd * 128:(hd + 1) * 128], k2[:n, :], kv[:n, u, 256 + hd * 128:256 + (hd + 1) * 128],
                                       gi == 0, gi == 67, R=[k2b, kvb], W=[psB[4 + d_]])
            Sf = AR2.take([128, 4, 128], F32)
            Sfb = AR2.take([128, 4, 128], BF16)
            Sbf = AR2.take([128, 4, 128], F32)
            okv = AR2.take([128, 16, 768], BF16)
            Sb_all = AR3.take([128, 16, 4, 128], BF16)
            stB = Buf("states")
            ps4v = psum[4][:, :].rearrange("p (h e) -> p h e", h=4)
            ps5v = psum[5][:, :].rearrange("p (h e) -> p h e", h=4)
            copy("dve", Sf[:, :, :], ps4v, R=[psB[4]], W=[stB])
            copy("act", Sfb[:, :, :], ps4v, R=[psB[4]], W=[stB])
            copy("dve", Sbf[:, :, :], ps5v, R=[psB[5]], W=[stB])
            okB = Buf("okv")
            xload_tiles(XE, 0, 64, NLAT, 1024, 768, okv, okB, own=True)
            for c in range(15, -1, -1):
                copy("act", Sb_all[:, c, :, :], Sbf[:, :, :], R=[stB], W=[stB])
                if c == 0:
                    break
                for pr_ in range(2):
                    k2, k2b = scaled_pair(okv[:, c, :], pr_, lambda hd: dk[:, 1, hd:hd + 1], [okB, dcB], "pool", "pool")
                    for hh in range(2):
                        hd = pr_ * 2 + hh
                        mm(psum[5][:, hd * 128:(hd + 1) * 128], k2[:, :], okv[:, c, 256 + hd * 128:256 + (hd + 1) * 128],
                           True, True, R=[k2b, okB], W=[psB[5]])
                for hd in range(4):
                    stt("dve", Sbf[:, hd, :], Sbf[:, hd, :], g128c(1, hd), ps5v[:, hd, :],
                        ALU.mult, ALU.add, R=[stB, psB[5], smB], W=[stB])
            qd = Ring(4, lambda i: (AR.take([128, 128], BF16), Buf(f"qd{i}")))
            sdr = Ring(3, lambda i: (AR.take([128, 128], BF16), Buf(f"sd{i}")))
            osq = Ring(2, lambda i: (AR.take([128, 512], BF16), Buf(f"osq{i}")))
            ors = Ring(2, lambda i: (AR.take([128, 512], F32), Buf(f"ors{i}")))
            oo = Ring(2, lambda i: (AR.take([128, 512], F32), Buf(f"oo{i}")))
            scr = Ring(2, lambda i: i)

            def finish_out(pso, hd, t0, n, ti):
                o_, ob = oo.next()
                copy("act", o_[:, :n], psum[pso][:, :n], R=[psB[pso]], W=[ob])
                sq_, sqb_ = osq.next()
                act(sq_[:, :n], psum[pso][:, :n], AF.Square, R=[psB[pso]], W=[sqb_])
                mm(psum[6][:, :n], ONES_B, sq_[:, :n], True, True, R=[sqb_, cmB], W=[psB[6]])
                rs_, rsb_ = ors.next()
                rstd_from_ps(psum[6][:, :n], psB[6], rs_[:, :n], rsb_, 1.0 / 128)
                tt("dve", o_[:, :n], o_[:, :n], rs_[:, :n], ALU.mult, R=[ob, rsb_], W=[ob])
                tt("dve", bufB[:, 4 + hd, t0:t0 + n], o_[:, :n], bufB[:, 4 + hd, t0:t0 + n], ALU.mult, R=[ob, bB[ti]], W=[bB[ti]])

            for c4 in range(4):
                ti = 1 + c4
                t0t, _ = TT[ti]
                for pr_ in range(2):
                    for cc in range(4):
                        c = c4 * 4 + cc
                        t0 = 64 + c * 128
                        k2, k2b = scaled_pair(okv[:, c, :], pr_, lambda hd: dk[:, 0, hd:hd + 1], [okB, dcB], "pool", "pool")
                        for hh in range(2):
                            hd = pr_ * 2 + hh
                            pso = 2 + hh
                            hp = hh * 64
                            cq = pr_
                            si = scr.next()
                            mm(psum[si][:, 0:128], KrT[hp:hp + 64, cq, t0:t0 + 128], QrT[hp:hp + 64, cq, t0:t0 + 128], True, True,
                               R=[krB[ti], qrB[ti]], W=[psB[si]])
                            sd, sdb = sdr.next()
                            tt("dve", sd[:, :], psum[si][:, 0:128], Dc[:, hd, :], ALU.mult, R=[psB[si], dcB], W=[sdb])
                            qf, qfb = qd.next()
                            qbk, qbb = qd.next()
                            tt("pool", qf[hp:hp + 64, :], QrT[hp:hp + 64, cq, t0:t0 + 128], dq[hp:hp + 64, 0, hd, :], ALU.mult,
                               R=[qrB[ti], dcB], W=[qfb])
                            tt("pool", qbk[hp:hp + 64, :], QrT[hp:hp + 64, cq, t0:t0 + 128], dq[hp:hp + 64, 1, hd, :], ALU.mult,
                               R=[qrB[ti], dcB], W=[qbb])
                            oc = psum[pso][:, cc * 128:(cc + 1) * 128]
                            mm(oc, okv[:, c, 256 + hd * 128:256 + (hd + 1) * 128], sd[:, :], True, False, R=[okB, sdb], W=[psB[pso]])
                            mm(oc, Sfb[hp:hp + 64, hd, :], qf[hp:hp + 64, :], False, False, R=[stB, qfb], W=[psB[pso]])
                            mm(oc, Sb_all[hp:hp + 64, c, hd, :], qbk[hp:hp + 64, :], False, True, R=[stB, qbb], W=[psB[pso]])
                            mm(psum[4][:, hd * 128:(hd + 1) * 128], k2[:, :], okv[:, c, 256 + hd * 128:256 + (hd + 1) * 128],
                               True, True, R=[k2b, okB], W=[psB[4]])
                            stt("dve", Sf[:, hd, :], Sf[:, hd, :], g128c(0, hd), ps4v[:, hd, :],
                                ALU.mult, ALU.add, R=[stB, psB[4], smB], W=[stB])
                            copy("act", Sfb[:, hd, :], Sf[:, hd, :], R=[stB], W=[stB])
                    for hh in range(2):
                        finish_out(2 + hh, pr_ * 2 + hh, t0t, 512, ti)
            kcs = AR.take([128, 2, 4, 64], BF16)
            kcB = Buf("kcs")
            for r in range(4):
                for c2 in range(2):
                    xload_rows(XKc, r, c2 * 128, 128, 0, 64, kcs[:, c2, r, :], kcB)
            vcs = AR2.take([64, 4, 512], BF16)
            vcB = Buf("vcs")
            for r in range(4):
                xload_rows(XE, r, 0, 64, 1280, 512, vcs[:, r, :], vcB)
            for hd in range(4):
                pso = 2 + hd % 2
                hp = (hd % 2) * 64
                cq = hd // 2
                for r in range(4):
                    si = scr.next()
                    mm(psum[si][0:64, 0:64], kcs[hp:hp + 64, cq, r, :], QrT[hp:hp + 64, cq, 0:64], True, True,
                       R=[kcB, qrB[0]], W=[psB[si]])
                    sd, sdb = sdr.next()
                    tt("dve", sd[0:64, 0:64], psum[si][0:64, 0:64], Dx[:, hd, r, :], ALU.mult, R=[psB[si], dcB], W=[sdb])
                    mm(psum[pso][:, 0:64], vcs[:, r, hd * 128:(hd + 1) * 128], sd[0:64, 0:64], r == 0, r == 3,
                       R=[vcB, sdb], W=[psB[pso]])
                finish_out(pso, hd, 0, 64, 0)
            new_phase()
            AR2.reset()
            Ag = AR2.take([128, 64, 256], BF16)
            agB = Buf("Ag")
            tbr = Ring(3, lambda i: (AR.take([128, 2, 8, 512], BF16), Buf(f"tb{i}")))
            dctx = AR.take([64, 4, 2, 64], BF16)
            dcxB = Buf("dctx")
            dma("pool", dctx, dftctx_d[:, :, :, :], R=[], W=[dcxB])
            actx = AR.take([64, 4, 1024], BF16)
            acxB = Buf("actx")
            for r in range(4):
                xload_rows(XE, r, 0, 64, 0, 1024, actx[:, r, :], acxB)

            def ld_tab(kt, tc):
                if small_dft:
                    tc = 0
                tb, tbb = tbr.next()
                dma("sp", tb[:, 0, :, :], dftc_d[tc * 1024:(tc + 1) * 1024, kt * 512:(kt + 1) * 512].rearrange("(t p) k -> p t k", p=128),
                    R=[], W=[tbb])
                dma("sp", tb[:, 1, :, :], dfts_d[tc * 1024:(tc + 1) * 1024, kt * 512:(kt + 1) * 512].rearrange("(t p) k -> p t k", p=128),
                    R=[], W=[tbb])
                return tb, tbb

            for g in range(4):
                for r in range(4):
                    xload_tiles(XE, r, 64, NLAT, g * 256, 256, Ag[:, r * 16:(r + 1) * 16, :], agB)
                seq = [(kt, tc) for kt in range(4) for tc in range(8)]
                nxt = ld_tab(*seq[0])
                for qi, (kt, tc) in enumerate(seq):
                    tb, tbb = nxt
                    if qi + 1 < len(seq):
                        nxt = ld_tab(*seq[qi + 1])
                    pi = kt % 2
                    for j in range(8):
                        tl = tc * 8 + j
                        for cs_ in range(2):
                            mm(psum[pi][:, :], Ag[:, tl, cs_ * 128:(cs_ + 1) * 128], tb[:, cs_, j, :],
                               tc == 0 and j == 0 and cs_ == 0, tc == 7 and j == 7 and cs_ == 1,
                               R=[agB, tbb], W=[psB[pi]])
                    if tc == 7:
                        t0 = 64 + kt * 512
                        copy("act", bufB[:, g, t0:t0 + 512], psum[pi][:, :], R=[psB[pi]], W=[bB[1 + kt]])
                for r in range(4):
                    for cs_ in range(2):
                        mm(psum[2][:, 0:64], actx[:, r, g * 256 + cs_ * 128:g * 256 + (cs_ + 1) * 128], dctx[:, r, cs_, :],
                           r == 0 and cs_ == 0, r == 3 and cs_ == 1, R=[acxB, dcxB], W=[psB[2]])
                copy("act", bufB[:, g, 0:64], psum[2][:, 0:64], R=[psB[2]], W=[bB[0]])
            new_phase()
            out_proj_residual(L, w_out_even_d[i2], all_tiles)
            new_phase()
            modulate(L, 3, 4, all_tiles)
            rings = ffn_rings(256, 3)
            jobs = [(ffn_w1_d[i2], ffn_w3_d[i2], ffn_w2_d[i2], j, None, None) for j in range(FFN // 256)]
            swiglu_jobs(L, jobs, 256, all_tiles, rings)

    S.enabled = True
    S.phase_limit = None
    new_phase()
    yB = Buf("yT", keep=True)
    for kc in range(KC):
        dma("sp", yT_d[kc * 128:(kc + 1) * 128, :], xT[:, kc, 64:NT], R=xB, W=[yB])
    fin = Ins("sp", None, "c")
    fin.deps = set([yB.lw])
    S.q["sp"].append(fin)
    S.finalize()
    with nc.Block() as block:
        @block.tensor
        def _(e):
            S.emit(e, "pe")

        @block.scalar
        def _(e):
            S.emit(e, "act")

        @block.vector
        def _(e):
            S.emit(e, "dve")

        @block.gpsimd
        def _(e):
            S.emit(e, "pool")

        @block.sync
        def _(e):
            S.emit(e, "sp")
    return nc


def _rope_tables(core):
    r = core % 4
    quarter = 16
    inv_freq = 10000.0 ** (-np.arange(quarter, dtype=np.float32) / quarter)
    cos = np.ones((128, NT), np.float32)
    sin = np.zeros((128, NT), np.float32)
    tok = np.arange(r * NLAT, (r + 1) * NLAT)
    row = (tok // 64).astype(np.float32)
    colv = (tok % 64).astype(np.float32)
    ang = np.concatenate([row[:, None] * inv_freq, colv[:, None] * inv_freq], axis=-1).astype(np.float32)
    c = np.cos(ang).T
    s = np.sin(ang).T
    for p in range(128):
        d = p % 64
        fi = d % 32
        cos[p, 64:] = c[fi]
        sin[p, 64:] = -s[fi] if d < 32 else s[fi]
    return cos, sin


def _const_mats():
    cm = np.zeros((128, 6, 128), np.float32)
    cm[:, 0, :] = 1.0
    cm[0:64, 1, 0:64] = 1.0
    cm[64:128, 1, 64:128] = 1.0
    cm[:, 2, :] = np.eye(128, dtype=np.float32)
    for p in range(128):
        d = p % 64
        partner = (p - d) + ((d + 32) % 64)
        cm[p, 3, partner] = 1.0
    return cm


def _dft_tables(core):
    r = core % 4
    n = 8192
    t = np.arange(n, dtype=np.int64)[:, None]
    k = (np.arange(NLAT, dtype=np.int64) + r * NLAT)[None, :]
    ph = ((t * k) % n).astype(np.float64) * (2.0 * np.pi / n)
    sc = 1.0 / np.sqrt(n)
    dc = (np.cos(ph) * sc).astype(np.float32).astype(ml_dtypes.bfloat16)
    ds = (-np.sin(ph) * sc).astype(np.float32).astype(ml_dtypes.bfloat16)
    n2 = 256
    dctx = np.zeros((64, 4, 2, 64), np.float32)
    for rr in range(4):
        tt_ = (np.arange(64) + rr * 64)[:, None]
        kk = (np.arange(64) + r * 64)[None, :]
        ph2 = ((tt_ * kk) % n2).astype(np.float64) * (2.0 * np.pi / n2)
        dctx[:, rr, 0, :] = np.cos(ph2) / np.sqrt(n2)
        dctx[:, rr, 1, :] = -np.sin(ph2) / np.sqrt(n2)
    return dc, ds, dctx


def _cs128():
    c = np.arange(128)[:, None] * np.arange(128)[None, :]
    ph = (c % 128).astype(np.float64) * (2.0 * np.pi / 128)
    out = np.zeros((128, 256), np.float32)
    out[:, :128] = np.cos(ph) / np.sqrt(128.0)
    out[:, 128:] = np.sin(ph) / np.sqrt(128.0)
    return out


def _ret_tables(core):
    r = core % 4
    j = np.arange(128)[:, None].astype(np.float32)
    i = np.arange(128)[None, :].astype(np.float32)
    rtab = np.zeros((128, 4, 128), np.float32)
    rtab[:, 0, :] = np.maximum(i - j, 0)
    rtab[:, 1, :] = (j <= i)
    rtab[:, 2, :] = np.maximum(j - i, 0)
    rtab[:, 3, :] = (j > i)
    rpos = np.zeros((128, 4, 128), np.float32)
    rpos[:, 0, :] = np.arange(128)[None, :] + 1.0
    rpos[:, 1, :] = 128.0 - np.arange(128)[None, :]
    rpos[:, 2, 0] = 127.0 - np.arange(128)
    rpos[:, 2, 1] = np.arange(128)
    I0 = r * NLAT
    P0f = 256 + I0
    i_last = I0 + NLAT - 1
    P0b = 256 + 8191 - i_last
    rdist = np.zeros((128, 4, 68), np.float32)
    for rr in range(4):
        for tl in range(17):
            gi = rr * 17 + tl
            for p in range(128):
                if tl == 0:
                    if p >= 64:
                        continue
                    m = rr * 64 + p
                    pf = m
                    pb = 255 - m
                else:
                    li = rr * NLAT + (tl - 1) * 128 + p
                    pf = 256 + li
                    pb = 256 + 8191 - li
                if pf < P0f:
                    rdist[p, 0, gi] = P0f - 1 - pf
                    rdist[p, 1, gi] = 1.0
                if pb < P0b:
                    rdist[p, 2, gi] = P0b - 1 - pb
                    rdist[p, 3, gi] = 1.0
    rctx = np.zeros((64, 4, 4, 64), np.float32)
    for rr in range(4):
        mj = (rr * 64 + np.arange(64))[:, None].astype(np.float32)
        mi = (r * 64 + np.arange(64))[None, :].astype(np.float32)
        rctx[:, 0, rr, :] = np.maximum(mi - mj, 0)
        rctx[:, 1, rr, :] = (mj <= mi)
        rctx[:, 2, rr, :] = np.maximum(mj - mi, 0)
        rctx[:, 3, rr, :] = (mj > mi)
    return rtab, rpos, rdist, rctx


_CACHE = {}


def _get_program(n_layers=DEPTH):
    key = ("nc", n_layers)
    if key not in _CACHE:
        _CACHE[key] = build_program(n_layers)
    return _CACHE[key]


def _host_inputs(inp, n_layers=DEPTH):
    f32 = np.float32
    x = np.asarray(inp["x"], f32)
    ctx = np.asarray(inp["ctx"], f32)
    c = np.asarray(inp["c"], f32)
    c_ctx = np.asarray(inp["c_ctx"], f32)
    shared = {}
    n_even = (n_layers + 1) // 2
    n_odd = max(n_layers // 2, 1)
    shared["w_mod"] = np.ascontiguousarray(np.asarray(inp["w_mod"], f32)[:n_layers])
    for k in ("w_in_even", "w_out_even", "ffn_w1", "ffn_w3", "ffn_w2"):
        shared[k] = np.ascontiguousarray(np.asarray(inp[k], f32)[:n_even])
    for k in ("w_in_odd", "w_out_odd", "w_router", "moe_w1", "moe_w3", "moe_w2"):
        if n_layers >= 2:
            shared[k] = np.ascontiguousarray(np.asarray(inp[k], f32)[:n_odd])
    b_mod = np.asarray(inp["b_mod"], f32)
    shared["b_modT"] = np.ascontiguousarray(b_mod.reshape(4, 48, 128).transpose(2, 0, 1))
    sp = np.zeros((128, 1024), f32)
    dec = np.stack([np.asarray(inp["ret_decay_fwd"], f32), np.asarray(inp["ret_decay_bwd"], f32)], axis=1)
    sp[:, 0:16] = dec.reshape(1, 16)
    p64 = np.arange(128) % 64
    for i in range(2):
        sp[:, 16 + i] = np.asarray(inp["q_norm_gain"], f32)[i][p64]
        sp[:, 18 + i] = np.asarray(inp["k_norm_gain"], f32)[i][p64]
        sp[:, 20 + i] = np.asarray(inp["subln_gain"], f32)[i]
        lam = np.concatenate([np.asarray(inp[k], f32)[i] for k in ("lambda_q1", "lambda_k1", "lambda_q2", "lambda_k2")])
        sp[:, 32 + i * 256:32 + (i + 1) * 256] = lam[None, :]
    shared["smallp"] = sp
    shared["cmat"] = _const_mats()
    shared["cs128"] = _cs128()
    in_maps = []
    for core in range(NCORES):
        b, r = core // 4, core % 4
        m = dict(shared)
        xt = np.concatenate([ctx[b, r * 64:(r + 1) * 64, :], x[b, r * NLAT:(r + 1) * NLAT, :]], axis=0)
        m["xT"] = np.ascontiguousarray(xt.T)
        cT = np.stack([c[b], c_ctx], axis=-1)
        m["cT"] = np.ascontiguousarray(cT.reshape(8, 128, 2).transpose(1, 0, 2))
        key = ("tabs", r)
        if key not in _CACHE:
            cos, sin = _rope_tables(core)
            dc, ds, dctx = _dft_tables(core)
            rtab, rpos, rdist, rctx = _ret_tables(core)
            _CACHE[key] = dict(cosT=cos, sinT=sin, dftc=dc, dfts=ds, dftctx=dctx, rtab=rtab, rpos=rpos, rdist=rdist, rctx=rctx)
        m.update(_CACHE[key])
        in_maps.append(m)
    return in_maps


def kernel(**inputs):
    nc = _get_program(DEPTH)
    in_maps = _host_inputs(inputs)
    res = run_bass_kernel_spmd(nc, in_maps, core_ids=list(range(NCORES)))
    out = np.zeros((2, 8192, D), np.float32)
    for core in range(NCORES):
        b, r = core // 4, core % 4
        out[b, r * NLAT:(r + 1) * NLAT, :] = np.asarray(res.results[core]["yT"], np.float32).T
    return out
```

```python
import math
import numpy as np
import ml_dtypes
import concourse.bass as bass
import concourse.mybir as mybir
from concourse.bass_utils import run_bass_kernel_spmd

F32 = mybir.dt.float32
BF16 = mybir.dt.bfloat16
AF = mybir.ActivationFunctionType
ALU = mybir.AluOpType
AX = mybir.AxisListType

NCORES = 8
D = 1024
KC = 8
NCTX = 64
NLAT = 2048
NT = NCTX + NLAT
TT = [(0, 64), (64, 512), (576, 512), (1088, 512), (1600, 512)]
TM = [(0, 64)] + [(64 + 128 * i, 128) for i in range(16)]
EPS = 1e-6
FFN = 2816
EXP = 3584
NEXP = 8
DEPTH = 4
ENGS = ("pe", "act", "dve", "pool", "sp")


PHASE_BUFS = []


class Buf:
    def __init__(self, name, keep=False):
        self.name = name
        if not keep:
            PHASE_BUFS.append(self)
        self.lw = None
        self.readers = {}
        self.dma_readers = []
        self.dsem = None
        self.dcount = 0


class Ins:
    __slots__ = ("eng", "fn", "deps", "kind", "dsem", "dcount", "sig", "need", "waits", "idx")

    def __init__(self, eng, fn, kind):
        self.eng = eng
        self.fn = fn
        self.kind = kind
        self.deps = set()
        self.dsem = None
        self.dcount = 0
        self.sig = None
        self.need = False
        self.waits = None


class Sched:
    def __init__(self, nc):
        self.nc = nc
        self.q = {e: [] for e in ENGS}
        self.last = {e: None for e in ENGS}
        self.dmas_since_bar = []
        self.nsem = 0
        self.enabled = True
        self.free_dsems = {}
        self.phase = 0
        self.phase_limit = None

    def newsem(self):
        self.nsem += 1
        return self.nc.alloc_semaphore(name=f"s{self.nsem}")

    def op(self, eng, fn, R=(), W=(), kind="c"):
        if not self.enabled:
            return None
        ins = Ins(eng, fn, kind)
        for b in R:
            if b.lw is not None:
                ins.deps.add(b.lw)
        for b in W:
            if b.lw is not None:
                ins.deps.add(b.lw)
            for r in b.readers.values():
                ins.deps.add(r)
            for r in b.dma_readers:
                ins.deps.add(r)
        ins.deps.discard(ins)
        for b in R:
            if kind == "d":
                b.dma_readers.append(ins)
            else:
                b.readers[eng] = ins
        for b in W:
            b.lw = ins
            b.readers = {}
            b.dma_readers = []
        if kind == "d":
            tgt = W[0]
            if tgt.dsem is None:
                tgt.dq = eng
                fl = self.free_dsems.setdefault(eng, [])
                if fl:
                    tgt.dsem, tgt.dcount = fl.pop()
                else:
                    tgt.dsem = self.newsem()
            assert tgt.dq == eng, (tgt.name, tgt.dq, eng)
            tgt.dcount += 16
            ins.dsem = tgt.dsem
            ins.dcount = tgt.dcount
            self.dmas_since_bar.append(ins)
        elif kind == "cc":
            tgt = W[0]
            if tgt.dsem is None:
                tgt.dsem = self.newsem()
            tgt.dcount += 1
            ins.dsem = tgt.dsem
            ins.dcount = tgt.dcount
            self.dmas_since_bar.append(ins)
        self.q[eng].append(ins)
        if kind == "c":
            self.last[eng] = ins
        return ins

    def barrier(self):
        if not self.enabled:
            return
        deps = set(x for x in self.last.values() if x is not None) | set(self.dmas_since_bar)
        self.dmas_since_bar = []
        for e in ENGS:
            ins = Ins(e, None, "c")
            ins.deps = set(deps)
            self.q[e].append(ins)

    def finalize(self):
        for e in ENGS:
            for ins in self.q[e]:
                for d in ins.deps:
                    if d.kind == "c":
                        if d.eng == "pe" and ins.eng == "pe" and ins.kind == "c":
                            continue
                        d.need = True
        self.engsem = {}
        for e in ENGS:
            cur = None
            cnt = 0
            for ins in self.q[e]:
                if ins.kind == "c" and ins.need and ins.fn is not None:
                    if cur is None or cnt >= 20000:
                        cur = self.newsem()
                        cnt = 0
                    cnt += 1
                    ins.sig = (cur, cnt)
        for e in ENGS:
            waited = {}
            for ins in self.q[e]:
                w = {}
                for d in ins.deps:
                    if d.kind == "c":
                        if d.eng == "pe" and ins.eng == "pe" and ins.kind == "c":
                            continue
                        if d.sig is None:
                            continue
                        sem, cnt = d.sig
                    else:
                        sem, cnt = d.dsem, d.dcount
                    k = id(sem)
                    if waited.get(k, (None, 0))[1] >= cnt:
                        continue
                    if k not in w or w[k][1] < cnt:
                        w[k] = (sem, cnt)
                for k, v in w.items():
                    waited[k] = v
                ins.waits = list(w.values())

    def emit(self, e, eng):
        for ins in self.q[eng]:
            for sem, cnt in ins.waits:
                e.wait_ge(sem, cnt)
            if ins.fn is None:
                continue
            r = ins.fn(e)
            if ins.kind == "d":
                r.then_inc(ins.dsem, 16)
            elif ins.kind == "cc":
                r.then_inc(ins.dsem)
            elif ins.sig is not None:
                r.then_inc(ins.sig[0], 1)


def build_program(n_layers=DEPTH, stop_after=None):
    del PHASE_BUFS[:]
    nc = bass.Bass("TRN2", target_bir_lowering=False)
    S = Sched(nc)
    S.phase_limit = stop_after

    ODD = ("w_in_odd", "w_out_odd", "w_router", "moe_w1", "moe_w3", "moe_w2")

    def din(name, shape, dt=F32):
        if name in ODD and n_layers < 2:
            return None
        return nc.dram_tensor(name, list(shape), dt, kind="ExternalInput").ap()

    xT_d = din("xT", [D, NT])
    cT_d = din("cT", [128, KC, 2])
    n_even = (n_layers + 1) // 2
    n_odd = max(n_layers // 2, 1)
    wmod_d = din("w_mod", [n_layers, D, 6 * D])
    bmod_d = din("b_modT", [128, 4, 48])
    w_in_even_d = din("w_in_even", [n_even, D, 2048])
    w_out_even_d = din("w_out_even", [n_even, D, D])
    ffn_w1_d = din("ffn_w1", [n_even, D, FFN])
    ffn_w3_d = din("ffn_w3", [n_even, D, FFN])
    ffn_w2_d = din("ffn_w2", [n_even, FFN, D])
    w_in_odd_d = din("w_in_odd", [n_odd, D, 3072])
    w_out_odd_d = din("w_out_odd", [n_odd, D, D])
    w_router_d = din("w_router", [n_odd, D, NEXP])
    moe_w1_d = din("moe_w1", [n_odd, NEXP, D, EXP])
    moe_w3_d = din("moe_w3", [n_odd, NEXP, D, EXP])
    moe_w2_d = din("moe_w2", [n_odd, NEXP, EXP, D])
    smallp_d = din("smallp", [128, 1024])
    cosT_d = din("cosT", [128, NT])
    sinT_d = din("sinT", [128, NT])
    cmat_d = din("cmat", [128, 6, 128])
    cs128_d = din("cs128", [128, 256])
    dft_d = din("dft", [8192, 4096], BF16)
    dftctx_d = din("dftctx", [64, 4, 2, 64])
    rtab_d = din("rtab", [128, 4, 128])
    rpos_d = din("rpos", [128, 4, 128])
    rdist_d = din("rdist", [128, 4, 68])
    rctx_d = din("rctx", [64, 4, 4, 64])
    yT_d = nc.dram_tensor("yT", [D, NLAT], F32, kind="ExternalOutput").ap()

    class XT:
        def __init__(self, name, rblocks, cblocks):
            self.rb = rblocks
            self.cb = cblocks
            self.ch = {}
            self.xb = Buf(name + "xb", keep=True)
            self.gb = Buf(name + "gb", keep=True)
            for i, (r0, rn) in enumerate(rblocks):
                for j, (c0, cw) in enumerate(cblocks):
                    xt_ = nc.dram_tensor(f"{name}x{i}_{j}", [rn, cw], BF16)
                    gt_ = nc.dram_tensor(f"{name}g{i}_{j}", [4 * rn, cw], BF16)
                    self.ch[(i, j)] = (xt_, gt_, self.xb, self.gb)

        def pieces(self, r0, n, c0, w):
            out = []
            for i, (b0, bn) in enumerate(self.rb):
                lo, hi = max(r0, b0), min(r0 + n, b0 + bn)
                if lo >= hi:
                    continue
                for j, (d0, dw) in enumerate(self.cb):
                    cl, chh = max(c0, d0), min(c0 + w, d0 + dw)
                    if cl >= chh:
                        continue
                    out.append((i, j, lo - b0, hi - lo, cl - d0, chh - cl, lo - r0, cl - c0))
            return out

        def wr(self, r0, n, c0, w):
            res = []
            for (i, j, br, nn, bc, ww, dr, dc) in self.pieces(r0, n, c0, w):
                xt_, gt_, xb_, gb_ = self.ch[(i, j)]
                res.append((xt_.ap()[br:br + nn, bc:bc + ww], xb_, dr, nn, dc, ww))
            return res

        def rd(self, rank, r0, n, c0, w, own=False):
            res = []
            for (i, j, br, nn, bc, ww, dr, dc) in self.pieces(r0, n, c0, w):
                xt_, gt_, xb_, gb_ = self.ch[(i, j)]
                if own:
                    res.append((xt_.ap()[br:br + nn, bc:bc + ww], xb_, dr, nn, dc, ww))
                else:
                    rn = self.rb[i][1]
                    res.append((gt_.ap()[rank * rn + br:rank * rn + br + nn, bc:bc + ww], gb_, dr, nn, dc, ww))
            return res

    TTB = [(t0, n) for (t0, n) in [(0, 64), (64, 512), (576, 512), (1088, 512), (1600, 512)]]
    ex = {}
    for L in range(n_layers):
        if L % 2 == 1:
            ex[L] = dict(KT=XT(f"KT{L}", [(i * 256, 256) for i in range(4)], TTB),
                         V=XT(f"V{L}", TTB, [(i * 256, 256) for i in range(4)]))
        else:
            ex[L] = dict(E=XT(f"E{L}", TTB, [(i * 256, 256) for i in range(7)]),
                         Kc=XT(f"Kc{L}", [(0, 256)], [(0, 64)]))

    def sb(name, shape, dt):
        return nc.alloc_sbuf_tensor(name, list(shape), dt)

    xT = sb("xT_s", [128, KC, NT], F32)
    bufA = sb("bufA", [128, KC, NT], BF16)
    bufB = sb("bufB", [128, KC, NT], BF16)
    xB = [Buf(f"x{t}", keep=True) for t in range(5)]
    aB = [Buf(f"a{t}", keep=True) for t in range(5)]
    bB = [Buf(f"b{t}", keep=True) for t in range(5)]
    modt = sb("modt", [128, 4, 48, 2], F32)
    modB = Buf("mod", keep=True)
    smallp = sb("smallp_s", [128, 1024], F32)
    smB = Buf("smallp", keep=True)
    cmat_f = sb("cmat_f", [128, 6, 128], F32)
    cmat_b = sb("cmat_b", [128, 6, 128], BF16)
    cmB = Buf("cmat", keep=True)
    ONES_B = cmat_b[:, 0, :]
    BD64_B = cmat_b[:, 1, :]
    ID_B = cmat_b[:, 2, :]
    PERM_B = cmat_b[:, 3, :]
    ONES_F = cmat_f[:, 0, :]
    ID_F = cmat_f[:, 2, :]
    ARENA = 66000
    comb = sb("comb_s", [128, 17, 8], F32)
    combB = Buf("comb", keep=True)
    arena = sb("arena", [128, ARENA // 2], BF16)

    class Arena:
        def __init__(self, backing=None, cap=None):
            self.off = 0
            self.backing = backing
            self.cap = cap

        def reset(self):
            self.off = 0

        def take(self, shape, dt):
            n = int(np.prod(shape[1:]))
            nb = n * (2 if dt == BF16 else 4)
            nb_al = (nb + 63) // 64 * 64
            bk = arena if self.backing is None else self.backing
            cap = ARENA if self.cap is None else self.cap
            assert self.off + nb_al <= cap, (self.off, nb_al, cap)
            v = bk[0:shape[0], self.off // 2:(self.off + nb) // 2]
            if dt == F32:
                v = v.bitcast(F32)
            self.off += nb_al
            if len(shape) == 3:
                v = v.rearrange("p (a b) -> p a b", a=shape[1])
            elif len(shape) == 4:
                v = v.rearrange("p (a b c) -> p a b c", a=shape[1], b=shape[2])
            return v

    AR = Arena()
    bufA_flat = bufA[:, :, :].rearrange("p a b -> p (a b)")
    bufB_flat = bufB[:, :, :].rearrange("p a b -> p (a b)")
    AR2 = Arena(bufA_flat, KC * NT * 2)
    AR3 = Arena(bufB_flat, 4 * NT * 2)
    rope_state = {}

    def load_rope():
        c_ = AR.take([128, NT], F32)
        s_ = AR.take([128, NT], F32)
        b_ = Buf("rope")
        dma("sp", c_, cosT_d[:, :], R=[], W=[b_])
        dma("sp", s_, sinT_d[:, :], R=[], W=[b_])
        rope_state["c"], rope_state["s"], rope_state["b"] = c_, s_, b_
    psum = [nc.alloc_psum_tensor(f"ps{i}", [128, 512], F32) for i in range(8)]
    psB = [Buf(f"ps{i}", keep=True) for i in range(8)]

    class Ring:
        def __init__(self, n, mk):
            self.items = [mk(i) for i in range(n)]
            self.i = 0

        def next(self):
            it = self.items[self.i % len(self.items)]
            self.i += 1
            return it

    def new_phase():
        S.phase += 1
        if S.phase_limit is not None and S.phase > S.phase_limit:
            S.enabled = False
        S.barrier()
        for b_ in PHASE_BUFS:
            if b_.dsem is not None and getattr(b_, "dq", None) is not None:
                S.free_dsems.setdefault(b_.dq, []).append((b_.dsem, b_.dcount))
        del PHASE_BUFS[:]
        AR.reset()

    def mm(out, lhsT, rhs, start, stop, R, W):
        S.op("pe", lambda e: e.matmul(out, lhsT, rhs, start=start, stop=stop), R=R, W=W)

    def act(out, in_, func, R, W, bias=None, scale=None):
        kw = {}
        if bias is not None:
            kw["bias"] = bias
        if scale is not None:
            kw["scale"] = scale
        S.op("act", lambda e: e.activation(out=out, in_=in_, func=func, **kw), R=R, W=W)

    def tt(eng, out, in0, in1, op, R, W):
        S.op(eng, lambda e: e.tensor_tensor(out=out, in0=in0, in1=in1, op=op), R=R, W=W)

    def ts(eng, out, in0, s1, s2, op0, op1, R, W):
        if s2 is None:
            S.op(eng, lambda e: e.tensor_scalar(out=out, in0=in0, scalar1=s1, scalar2=None, op0=op0), R=R, W=W)
        else:
            S.op(eng, lambda e: e.tensor_scalar(out=out, in0=in0, scalar1=s1, scalar2=s2, op0=op0, op1=op1), R=R, W=W)

    def stt(eng, out, in0, scalar, in1, op0, op1, R, W):
        S.op(eng, lambda e: e.scalar_tensor_tensor(out=out, in0=in0, scalar=scalar, in1=in1, op0=op0, op1=op1), R=R, W=W)

    def recip(out, in_, R, W):
        S.op("dve", lambda e: e.reciprocal(out=out, in_=in_), R=R, W=W)

    def copy(eng, out, in_, R, W):
        if eng == "act":
            S.op("act", lambda e: e.copy(out=out, in_=in_), R=R, W=W)
        else:
            S.op(eng, lambda e: e.tensor_copy(out=out, in_=in_), R=R, W=W)

    def dma(q, out, in_, R, W):
        S.op(q, lambda e: e.dma_start(out=out, in_=in_), R=R, W=W, kind="d")

    def rstd_from_ps(ps_ap, psbuf, out_ap, outbuf, n_inv):
        act(out_ap, ps_ap, AF.Ln, R=[psbuf], W=[outbuf], bias=epsb[:, 0:1], scale=n_inv)
        act(out_ap, out_ap, AF.Exp, R=[outbuf], W=[outbuf], scale=-0.5)

    epsb = sb("epsb", [128, 2], F32)
    epsB = Buf("eps", keep=True)
    S.op("dve", lambda e: e.memset(epsb[:, 0:1], EPS), W=[epsB])
    S.op("dve", lambda e: e.memset(epsb[:, 1:2], 1.0), W=[epsB])
    for kc in range(KC):
        dma("sp", xT[:, kc, :], xT_d[kc * 128:(kc + 1) * 128, :], R=[], W=xB)
    dma("sp", smallp[:], smallp_d[:, :], R=[], W=[smB])
    dma("sp", cmat_f[:], cmat_d[:, :, :], R=[], W=[cmB])
    cmB2 = Buf("cmat_b", keep=True)
    dma("pool", cmat_b[:], cmat_d[:, :, :], R=[], W=[cmB2])
    S.op("dve", lambda e: e.memset(epsb[:, 1:2], 1.0), R=[cmB2], W=[epsB])

    SP_DEC = 0
    SP_QG = 16
    SP_KG = 18
    SP_SUB = 20
    SP_LAM = 32
    SP_LG = 600
    SP_LAMNEG = 620
    SP_SUBG = 624
    SP_G128 = 630
    SP_KG8 = 650
    SP_TMP = 700

    act(smallp[:, SP_LG:SP_LG + 16], smallp[:, SP_DEC:SP_DEC + 16], AF.Exp, R=[smB], W=[smB], scale=-1.0)
    act(smallp[:, SP_LG:SP_LG + 16], smallp[:, SP_LG:SP_LG + 16], AF.Ln, R=[smB, epsB], W=[smB], bias=epsb[:, 1:2])
    ts("dve", smallp[:, SP_LG:SP_LG + 16], smallp[:, SP_LG:SP_LG + 16], -1.0, None, ALU.mult, None, R=[smB], W=[smB])
    act(smallp[:, SP_G128:SP_G128 + 16], smallp[:, SP_LG:SP_LG + 16], AF.Exp, R=[smB], W=[smB], scale=128.0)
    for i in range(2):
        layer = 2 * i + 1
        lam_init = 0.8 - 0.6 * float(np.exp(-0.3 * layer))
        base = SP_LAM + i * 256
        for j in range(2):
            tt("dve", smallp[:, SP_TMP:SP_TMP + 64], smallp[:, base + j * 128:base + j * 128 + 64],
               smallp[:, base + j * 128 + 64:base + j * 128 + 128], ALU.mult, R=[smB], W=[smB])
            S.op("dve", lambda e, j=j: e.reduce_sum(out=smallp[:, SP_TMP + 64 + j:SP_TMP + 65 + j],
                                                   in_=smallp[:, SP_TMP:SP_TMP + 64], axis=AX.X), R=[smB], W=[smB])
        act(smallp[:, SP_TMP + 64:SP_TMP + 66], smallp[:, SP_TMP + 64:SP_TMP + 66], AF.Exp, R=[smB], W=[smB])
        tt("dve", smallp[:, SP_LAMNEG + i:SP_LAMNEG + i + 1], smallp[:, SP_TMP + 65:SP_TMP + 66],
           smallp[:, SP_TMP + 64:SP_TMP + 65], ALU.subtract, R=[smB], W=[smB])
        ts("dve", smallp[:, SP_LAMNEG + i:SP_LAMNEG + i + 1], smallp[:, SP_LAMNEG + i:SP_LAMNEG + i + 1],
           -lam_init, None, ALU.add, None, R=[smB], W=[smB])
        ts("dve", smallp[:, SP_SUBG + i:SP_SUBG + i + 1], smallp[:, SP_SUB + i:SP_SUB + i + 1],
           1.0 - lam_init, None, ALU.mult, None, R=[smB], W=[smB])

    AR.reset()
    csil_f = AR.take([128, KC, 2], F32)
    csil = AR.take([128, KC, 2], BF16)
    bmod_s = AR.take([128, 4, 48], F32)
    cB = Buf("csil")
    dma("sp", csil_f, cT_d[:, :, :], R=[], W=[cB])
    dma("sp", bmod_s, bmod_d[:, :, :], R=[], W=[cB])
    act(csil, csil_f, AF.Silu, R=[cB], W=[cB])
    wring = Ring(3, lambda i: (AR.take([128, KC, 1024], BF16), Buf(f"wm{i}")))
    pr = Ring(2, lambda i: i)
    for L in range(n_layers):
        for blk in range(6):
            wt, wb = wring.next()
            dma("pool", wt, wmod_d[L, :, blk * 1024:(blk + 1) * 1024].rearrange("(k p) n -> p k n", p=128), R=[], W=[wb])
            pi = pr.next()
            for m in range(8):
                for kc in range(KC):
                    mm(psum[pi][:, 2 * m:2 * m + 2], wt[:, kc, m * 128:(m + 1) * 128], csil[:, kc, :],
                       kc == 0, kc == KC - 1, R=[wb, cB], W=[psB[pi]])
            for col in range(2):
                tt("dve", modt[:, L, blk * 8:(blk + 1) * 8, col],
                   psum[pi][:, 0:16].rearrange("p (m c) -> p m c", c=2)[:, :, col],
                   bmod_s[:, L, blk * 8:(blk + 1) * 8], ALU.add, R=[psB[pi], cB], W=[modB])
        for which in (1, 4):
            ts("dve", modt[:, L, which * 8:(which + 1) * 8, :], modt[:, L, which * 8:(which + 1) * 8, :],
               1.0, None, ALU.add, None, R=[modB], W=[modB])

    def modap(L, which, kc, col):
        return modt[:, L, which * 8 + kc, col:col + 1]

    def modulate(L, w_shift, w_scale, tiles, hook=None):
        sq = Ring(2, lambda i: (AR.take([128, 512], BF16), Buf(f"msq{i}")))
        rs = Ring(2, lambda i: (AR.take([128, 512], F32), Buf(f"mrs{i}")))
        tmp = Ring(2, lambda i: (AR.take([128, 512], F32), Buf(f"mtmp{i}")))
        pring = Ring(2, lambda i: i)
        for ti in tiles:
            t0, n = TT[ti]
            col = 1 if ti == 0 else 0
            pi = pring.next()
            for kc in range(KC):
                sqt, sqb = sq.next()
                act(sqt[:, :n], xT[:, kc, t0:t0 + n], AF.Square, R=[xB[ti]], W=[sqb])
                mm(psum[pi][:, :n], ONES_B, sqt[:, :n], kc == 0, kc == KC - 1, R=[sqb, cmB], W=[psB[pi]])
            rst, rsb = rs.next()
            rstd_from_ps(psum[pi][:, :n], psB[pi], rst[:, :n], rsb, 1.0 / D)
            for kc in range(KC):
                tmt, tmb = tmp.next()
                tt("dve", tmt[:, :n], xT[:, kc, t0:t0 + n], rst[:, :n], ALU.mult, R=[xB[ti], rsb], W=[tmb])
                if hook is not None:
                    hook(ti, kc, tmt, tmb, n, col)
                act(bufA[:, kc, t0:t0 + n], tmt[:, :n], AF.Identity, R=[tmb, modB], W=[aB[ti]],
                    bias=modap(L, w_shift, kc, col), scale=modap(L, w_scale, kc, col))

    def wload(dst, dstb, src2d, k0, ncols_total, c0, ncols):
        nk = dst.shape[1]
        dma("pool", dst, src2d[k0 * 128:(k0 + nk) * 128, c0:c0 + ncols].rearrange("(k p) n -> p k n", p=128), R=[], W=[dstb])

    def rope(src, srcb, n, t0, out_ap, outb, ps_i, tmpring):
        mm(psum[ps_i][:, :n], PERM_B, src[:, :n], True, True, R=[srcb, cmB], W=[psB[ps_i]])
        t1, t1b = tmpring.next()
        t2, t2b = tmpring.next()
        cosT, sinT, ropeB = rope_state["c"], rope_state["s"], rope_state["b"]
        tt("pool", t1[:, :n], src[:, :n], cosT[:, t0:t0 + n], ALU.mult, R=[srcb, ropeB], W=[t1b])
        tt("dve", t2[:, :n], psum[ps_i][:, :n], sinT[:, t0:t0 + n], ALU.mult, R=[psB[ps_i], ropeB], W=[t2b])
        tt("dve", out_ap, t1[:, :n], t2[:, :n], ALU.add, R=[t1b, t2b], W=[outb])

    def out_proj_residual(L, wsrc, tiles):
        wr = Ring(2, lambda i: (AR.take([128, KC, 512], BF16), Buf(f"wo{i}")))
        pring = Ring(4, lambda i: i)
        slots = []
        for blk in range(2):
            wt, wb = wr.next()
            wload(wt, wb, wsrc, 0, D, blk * 512, 512)
            slots.append((wt, wb))
        for blk in range(2):
            wt, wb = slots[blk]
            for ti in tiles:
                t0, n = TT[ti]
                col = 1 if ti == 0 else 0
                for m in range(4):
                    o = blk * 4 + m
                    pi = pring.next()
                    for kc in range(KC):
                        mm(psum[pi][:, :n], wt[:, kc, m * 128:(m + 1) * 128], bufB[:, kc, t0:t0 + n],
                           kc == 0, kc == KC - 1, R=[wb, bB[ti]], W=[psB[pi]])
                    stt("dve", xT[:, o, t0:t0 + n], psum[pi][:, :n], modap(L, 2, o, col), xT[:, o, t0:t0 + n],
                        ALU.mult, ALU.add, R=[psB[pi], modB, xB[ti]], W=[xB[ti]])

    def swiglu_jobs(L, jobs, blkw, tiles, rings):
        wr, gr, sr = rings
        nm = blkw // 128

        def load(job):
            w1src, w3src, w2src, j, cw, cwb = job[:6]
            if len(job) > 6 and job[6] is not None:
                job[6]()
            (w1t, w3t, w2t), wb = wr.next()
            wload(w1t, wb, w1src, 0, 0, j * blkw, blkw)
            wload(w3t, wb, w3src, 0, 0, j * blkw, blkw)
            dma("pool", w2t, w2src[j * blkw:(j + 1) * blkw, :].rearrange("(k p) n -> p k n", p=128), R=[], W=[wb])
            return (w1t, w3t, w2t, wb)

        def stage_ab(w, ti, cw, cwb):
            w1t, w3t, w2t, wb = w
            t0, n = TT[ti]
            gt, gb = gr.next()
            for m in range(nm):
                pa = m % 2
                pb = 2 + m % 2
                for kc in range(KC):
                    mm(psum[pa][:, :n], w1t[:, kc, m * 128:(m + 1) * 128], bufA[:, kc, t0:t0 + n],
                       kc == 0, kc == KC - 1, R=[wb, aB[ti]], W=[psB[pa]])
                for kc in range(KC):
                    mm(psum[pb][:, :n], w3t[:, kc, m * 128:(m + 1) * 128], bufA[:, kc, t0:t0 + n],
                       kc == 0, kc == KC - 1, R=[wb, aB[ti]], W=[psB[pb]])
                st, sbf = sr.next()
                act(st[:, :n], psum[pa][:, :n], AF.Silu, R=[psB[pa]], W=[sbf])
                if cw is None:
                    tt("dve", gt[:, m, :n], st[:, :n], psum[pb][:, :n], ALU.mult, R=[sbf, psB[pb]], W=[gb])
                else:
                    tt("dve", st[:, :n], st[:, :n], psum[pb][:, :n], ALU.mult, R=[sbf, psB[pb]], W=[sbf])
                    tt("dve", gt[:, m, :n], st[:, :n], cw[:, t0:t0 + n], ALU.mult, R=[sbf, cwb], W=[gb])
            return (gt, gb)

        def stage_w2(w, ti, g):
            w1t, w3t, w2t, wb = w
            gt, gb = g
            t0, n = TT[ti]
            col = 1 if ti == 0 else 0
            for o in range(KC):
                py = 4 + o % 4
                for m in range(nm):
                    mm(psum[py][:, :n], w2t[:, m, o * 128:(o + 1) * 128], gt[:, m, :n],
                       m == 0, m == nm - 1, R=[wb, gb], W=[psB[py]])
                stt("dve", xT[:, o, t0:t0 + n], psum[py][:, :n], modap(L, 5, o, col), xT[:, o, t0:t0 + n],
                    ALU.mult, ALU.add, R=[psB[py], modB, xB[ti]], W=[xB[ti]])

        nxt = load(jobs[0])
        pend = None
        for ji, job in enumerate(jobs):
            w = nxt
            if pend is not None:
                stage_w2(*pend)
                pend = None
            if ji + 1 < len(jobs):
                nxt = load(jobs[ji + 1])
            for ti in tiles:
                g = stage_ab(w, ti, job[4], job[5])
                if pend is not None:
                    stage_w2(*pend)
                pend = (w, ti, g)
        if pend is not None:
            stage_w2(*pend)

    def ffn_rings(blkw, nslots):
        nm = blkw // 128
        wr = Ring(nslots, lambda i: ((AR.take([128, KC, blkw], BF16), AR.take([128, KC, blkw], BF16),
                                      AR.take([128, nm, D], BF16)), Buf(f"fw{i}")))
        gr = Ring(2, lambda i: (AR.take([128, nm, 512], BF16), Buf(f"g{i}")))
        sr = Ring(3, lambda i: (AR.take([128, 512], F32), Buf(f"s{i}")))
        return wr, gr, sr

    AG = [[0, 1, 2, 3], [4, 5, 6, 7]]

    def allgather(xt):
        for key, (xt_, gt_, xb_, gb_) in xt.ch.items():
            S.op("pool", lambda e, xt_=xt_, gt_=gt_: e.collective_compute(
                "AllGather", ALU.bypass, replica_groups=AG, ins=[xt_.ap().opt()], outs=[gt_.ap().opt()]),
                R=[xb_], W=[gb_], kind="cc")

    def xstore(xt, r0, n, c0, w, src, srcb):
        for (ap_, buf_, dr, nn, dc, ww) in xt.wr(r0, n, c0, w):
            dma("sp", ap_, src[dr:dr + nn, dc:dc + ww], R=[srcb], W=[buf_])

    def xload_rows(xt, rank, r0, n, c0, w, dst, dstb, own=False):
        for (ap_, buf_, dr, nn, dc, ww) in xt.rd(rank, r0, n, c0, w, own):
            dma("sp", dst[dr:dr + nn, dc:dc + ww], ap_, R=[buf_], W=[dstb])

    def xload_tiles(xt, rank, r0, n, c0, w, dst, dstb, own=False):
        for (ap_, buf_, dr, nn, dc, ww) in xt.rd(rank, r0, n, c0, w, own):
            assert dr % 128 == 0 and nn % 128 == 0
            dma("sp", dst[:, dr // 128:(dr + nn) // 128, dc:dc + ww], ap_.rearrange("(t p) c -> p t c", p=128),
                R=[buf_], W=[dstb])

    def v_project(wt, wb, dst, c0, stg):
        for (t0, n) in TM:
            ti = 0 if t0 == 0 else 1 + (t0 - 64) // 512
            p0 = 6 + (t0 // 128) % 2
            for kc in range(KC):
                mm(psum[p0][:n, :], bufA[:, kc, t0:t0 + n], wt[:, kc, :], kc == 0, kc == KC - 1,
                   R=[wb, aB[ti]], W=[psB[p0]])
            vt, vb2 = stg.next()
            copy("act", vt[:n, :], psum[p0][:n, :], R=[psB[p0]], W=[vb2])
            xstore(dst, t0, n, c0, 512, vt, vb2)

    for L in range(n_layers):
        i2 = L // 2
        last = L == DEPTH - 1
        all_tiles = [0, 1, 2, 3, 4]
        lat_tiles = [1, 2, 3, 4]
        if L % 2 == 1:
            X = ex[L]
            XKT, XV = X["KT"], X["V"]
            new_phase()
            modulate(L, 0, 1, all_tiles)
            new_phase()
            load_rope()
            wr = Ring(2, lambda i: (AR.take([128, KC, 512], BF16), Buf(f"wq{i}")))
            raw = Ring(2, lambda i: (AR.take([128, 512], F32), Buf(f"raw{i}")))
            sqr = Ring(2, lambda i: (AR.take([128, 512], BF16), Buf(f"sq{i}")))
            rsr = Ring(2, lambda i: (AR.take([128, 512], F32), Buf(f"rs{i}")))
            qnr = Ring(2, lambda i: (AR.take([128, 512], BF16), Buf(f"qn{i}")))
            tmpr = Ring(4, lambda i: (AR.take([128, 512], F32), Buf(f"rt{i}")))
            kst = Ring(3, lambda i: (AR.take([128, 512], BF16), Buf(f"kst{i}")))

            def ld_odd(blk):
                wt, wb = wr.next()
                wload(wt, wb, w_in_odd_d[i2], 0, 3072, blk * 512, 512)
                return wt, wb

            nxt = ld_odd(0)
            for blk in range(6):
                wt, wb = nxt
                if blk + 1 < 6:
                    nxt = ld_odd(blk + 1)
                if blk < 4:
                    isq = blk < 2
                    gcol = (SP_QG if isq else SP_KG) + i2
                    for ti in all_tiles:
                        t0, n = TT[ti]
                        for m in range(4):
                            ch = (blk % 2) * 4 + m
                            p0 = m % 2
                            for kc in range(KC):
                                mm(psum[p0][:, :n], wt[:, kc, m * 128:(m + 1) * 128], bufA[:, kc, t0:t0 + n],
                                   kc == 0, kc == KC - 1, R=[wb, aB[ti]], W=[psB[p0]])
                            rt, rb = raw.next()
                            copy("act", rt[:, :n], psum[p0][:, :n], R=[psB[p0]], W=[rb])
                            st, sbf = sqr.next()
                            act(st[:, :n], rt[:, :n], AF.Square, R=[rb], W=[sbf])
                            p1 = 2 + m % 2
                            mm(psum[p1][:, :n], BD64_B, st[:, :n], True, True, R=[sbf, cmB], W=[psB[p1]])
                            rst, rsb = rsr.next()
                            rstd_from_ps(psum[p1][:, :n], psB[p1], rst[:, :n], rsb, 1.0 / 64)
                            qt, qb = qnr.next()
                            stt("dve", qt[:, :n], rt[:, :n], smallp[:, gcol:gcol + 1], rst[:, :n], ALU.mult, ALU.mult,
                                R=[rb, rsb, smB], W=[qb])
                            p2 = 4 + m % 2
                            if isq:
                                rope(qt, qb, n, t0, bufB[:, ch, t0:t0 + n], bB[ti], p2, tmpr)
                            else:
                                kt, kb = kst.next()
                                rope(qt, qb, n, t0, kt[:, :n], kb, p2, tmpr)
                                xstore(XKT, ch * 128, 128, t0, n, kt, kb)
                else:
                    v_project(wt, wb, XV, (blk - 4) * 512, kst)
            allgather(XKT)
            allgather(XV)
            new_phase()
            kslot = AR.take([128, 4, NT], BF16)
            ksB = Buf("kslot")
            vslot = AR.take([128, 4, 17, 128], BF16)
            vsB = Buf("vslot")
            ering = Ring(9, lambda i: (AR.take([128, 512], BF16), Buf(f"e{i}")))
            ftmp = Ring(4, lambda i: (AR.take([128, 512], F32), Buf(f"ft{i}")))
            fsq = Ring(2, lambda i: (AR.take([128, 512], BF16), Buf(f"fsq{i}")))
            accs = [(AR.take([128, 512], F32), Buf(f"acc{i}")) for i in range(3)]
            qtiles = lat_tiles if last else all_tiles
            G = 3
            for h in range(8):
                for r in range(4):
                    xload_rows(XKT, r, h * 128, 128, 0, NT, kslot[:, r, :], ksB)
                    xload_rows(XV, r, 0, 64, h * 128, 128, vslot[:, r, 0, :], vsB)
                    xload_tiles(XV, r, 64, NLAT, h * 128, 128, vslot[:, r, 1:17, :], vsB)
                for ti in qtiles:
                    t0, n = TT[ti]
                    if ti == 0:
                        ktl = [(r, 0, 0, 64) for r in range(4)]
                    else:
                        ktl = []
                        for r in range(4):
                            ktl.append((r, 0, 0, 64))
                            for j in range(16):
                                ktl.append((r, 1 + j, 64 + 128 * j, 128))
                    steps = [(ki, m) for ki in range(len(ktl)) for m in range(2)]
                    groups = [steps[i:i + G] for i in range(0, len(steps), G)]
                    av_order = []
                    for gi_, grp in enumerate(groups):
                        fwd = (gi_ // 2) % 2 == 0
                        order = list(range(len(grp))) if fwd else list(range(len(grp) - 1, -1, -1))
                        av_order += [grp[i] for i in reversed(order)]
                    first_av = {}
                    last_av = {}
                    for idx, (ki, m) in enumerate(av_order):
                        first_av.setdefault(m, idx)
                        last_av[m] = idx
                    av_idx = 0
                    acc_used = [False, False, False]
                    for (at_, ab_), eng_ in zip(accs, ("dve", "dve", "pool")):
                        S.op(eng_, lambda e, at_=at_: e.memset(at_[:, :], 0.0), W=[ab_])
                    prev = None
                    for gi_ in range(len(groups) + 1):
                        cur = None
                        if gi_ < len(groups):
                            grp = groups[gi_]
                            fwd = (gi_ // 2) % 2 == 0
                            order = list(range(len(grp))) if fwd else list(range(len(grp) - 1, -1, -1))
                            cur = []
                            for i in order:
                                ki, m = grp[i]
                                r, vt_i, k0, kn = ktl[ki]
                                si = (gi_ % 2) * 3 + i
                                mm(psum[si][:kn, :n], kslot[m * 64:(m + 1) * 64, r, k0:k0 + kn],
                                   bufB[m * 64:(m + 1) * 64, h, t0:t0 + n], True, True, R=[ksB, bB[ti]], W=[psB[si]])
                                et, eb = ering.next()
                                act(et[:kn, :n], psum[si][:kn, :n], AF.Exp, R=[psB[si]], W=[eb], scale=0.125)
                                ai = 0 if m == 0 else (1 if ki % 2 == 0 else 2)
                                at_, ab_ = accs[ai]
                                tt("pool" if ai == 2 else "dve", at_[:kn, :n], at_[:kn, :n], et[:kn, :n], ALU.add,
                                   R=[ab_, eb], W=[ab_])
                                cur.append((ki, m, et, eb))
                        if prev is not None:
                            for (ki, m, et, eb) in reversed(prev):
                                r, vt_i, k0, kn = ktl[ki]
                                mm(psum[6 + m][:, :n], vslot[:kn, r, vt_i, :], et[:kn, :n],
                                   av_idx == first_av[m], av_idx == last_av[m], R=[vsB, eb], W=[psB[6 + m]])
                                av_idx += 1
                        prev = cur
                    mm(psum[0][:, :n], ONES_F, accs[0][0][:, :n], True, True, R=[cmB, accs[0][1]], W=[psB[0]])
                    mm(psum[1][:, :n], ONES_F, accs[1][0][:, :n], True, False, R=[cmB, accs[1][1]], W=[psB[1]])
                    mm(psum[1][:, :n], ONES_F, accs[2][0][:, :n], False, True, R=[cmB, accs[2][1]], W=[psB[1]])
                    r1, r1b = ftmp.next()
                    r2, r2b = ftmp.next()
                    recip(r1[:, :n], psum[0][:, :n], R=[psB[0]], W=[r1b])
                    recip(r2[:, :n], psum[1][:, :n], R=[psB[1]], W=[r2b])
                    tt("dve", r1[:, :n], psum[6][:, :n], r1[:, :n], ALU.mult, R=[psB[6], r1b], W=[r1b])
                    tt("dve", r2[:, :n], psum[7][:, :n], r2[:, :n], ALU.mult, R=[psB[7], r2b], W=[r2b])
                    o_, ob = ftmp.next()
                    stt("dve", o_[:, :n], r2[:, :n], smallp[:, SP_LAMNEG + i2:SP_LAMNEG + i2 + 1], r1[:, :n],
                        ALU.mult, ALU.add, R=[r1b, r2b, smB], W=[ob])
                    sq_, sqb_ = fsq.next()
                    act(sq_[:, :n], o_[:, :n], AF.Square, R=[ob], W=[sqb_])
                    mm(psum[2][:, :n], ONES_B, sq_[:, :n], True, True, R=[sqb_, cmB], W=[psB[2]])
                    rs_, rsb_ = ftmp.next()
                    rstd_from_ps(psum[2][:, :n], psB[2], rs_[:, :n], rsb_, 1.0 / 128)
                    tt("dve", o_[:, :n], o_[:, :n], rs_[:, :n], ALU.mult, R=[ob, rsb_], W=[ob])
                    ts("dve", bufB[:, h, t0:t0 + n], o_[:, :n], smallp[:, SP_SUBG + i2:SP_SUBG + i2 + 1], None,
                       ALU.mult, None, R=[ob, smB], W=[bB[ti]])
            new_phase()
            out_proj_residual(L, w_out_odd_d[i2], qtiles)
            new_phase()
            wrt = AR.take([128, KC, 8], F32)
            wrtB = Buf("wrt")
            dma("sp", wrt, w_router_d[i2].rearrange("(k p) n -> p k n", p=128), R=[], W=[wrtB])
            hf32 = AR.take([128, KC, 512], F32)
            hfB = Buf("hf32")
            rl = AR.take([128, 17, 8], F32)
            rlB = Buf("rl")
            mx8 = AR.take([128, 8], F32)
            ex8 = AR.take([128, 8], F32)
            nb1 = AR.take([128, 2], F32)
            ffn_tiles = lat_tiles if last else all_tiles
            sqm = Ring(2, lambda i: (AR.take([128, 512], BF16), Buf(f"msq{i}")))
            rsm = Ring(2, lambda i: (AR.take([128, 512], F32), Buf(f"mrs{i}")))
            tmpm = Ring(2, lambda i: (AR.take([128, 512], F32), Buf(f"mtmp{i}")))
            for ti in ffn_tiles:
                t0, n = TT[ti]
                col = 1 if ti == 0 else 0
                pi = 0
                for kc in range(KC):
                    sqt, sqb = sqm.next()
                    act(sqt[:, :n], xT[:, kc, t0:t0 + n], AF.Square, R=[xB[ti]], W=[sqb])
                    mm(psum[pi][:, :n], ONES_B, sqt[:, :n], kc == 0, kc == KC - 1, R=[sqb, cmB], W=[psB[pi]])
                rst, rsb = rsm.next()
                rstd_from_ps(psum[pi][:, :n], psB[pi], rst[:, :n], rsb, 1.0 / D)
                for kc in range(KC):
                    tmt, tmb = tmpm.next()
                    tt("dve", tmt[:, :n], xT[:, kc, t0:t0 + n], rst[:, :n], ALU.mult, R=[xB[ti], rsb], W=[tmb])
                    act(hf32[:, kc, :n], tmt[:, :n], AF.Identity, R=[tmb, modB], W=[hfB],
                        bias=modap(L, 3, kc, col), scale=modap(L, 4, kc, col))
                    copy("pool", bufA[:, kc, t0:t0 + n], hf32[:, kc, :n], R=[hfB], W=[aB[ti]])
                for s0 in range(0, n, 128):
                    sn = min(128, n - s0)
                    tmi = 0 if ti == 0 else 1 + (t0 + s0 - 64) // 128
                    for kc in range(KC):
                        mm(psum[1][:sn, 0:8], hf32[:, kc, s0:s0 + sn], wrt[:, kc, :], kc == 0, kc == KC - 1,
                           R=[hfB, wrtB], W=[psB[1]])
                    copy("dve", rl[:sn, tmi, :], psum[1][:sn, 0:8], R=[psB[1]], W=[rlB])
                    S.op("dve", lambda e, sn=sn, tmi=tmi: e.max(out=mx8[:sn, :], in_=rl[:sn, tmi, :]), R=[rlB], W=[rlB])
                    ts("dve", nb1[:sn, 0:1], mx8[:sn, 0:1], -1.0, None, ALU.mult, None, R=[rlB], W=[rlB])
                    act(ex8[:sn, :], rl[:sn, tmi, :], AF.Exp, R=[rlB], W=[rlB], bias=nb1[:sn, 0:1])
                    stt("dve", ex8[:sn, :], rl[:sn, tmi, :], mx8[:sn, 1:2], ex8[:sn, :], ALU.is_ge, ALU.mult,
                        R=[rlB], W=[rlB])
                    S.op("dve", lambda e, sn=sn: e.reduce_sum(out=nb1[:sn, 1:2], in_=ex8[:sn, :], axis=AX.X), R=[rlB], W=[rlB])
                    recip(nb1[:sn, 1:2], nb1[:sn, 1:2], R=[rlB], W=[rlB])
                    ts("dve", comb[:sn, tmi, :], ex8[:sn, :], nb1[:sn, 1:2], None, ALU.mult, None, R=[rlB], W=[combB])
            new_phase()
            cw_all = bufB_flat.bitcast(F32).rearrange("p (a b) -> p a b", a=4)
            cwr = Ring(4, lambda i: (cw_all[:, i, :], Buf(f"cw{i}")))
            dgr = Ring(2, lambda i: (AR.take([128, 128], F32), Buf(f"dg{i}")))
            rings = ffn_rings(512, 2)
            jobs = []
            for ex_i in range(NEXP):
                cw, cwb = cwr.next()

                def pre(ex_i=ex_i, cw=cw, cwb=cwb):
                    for (t0, n) in TM:
                        if last and t0 == 0:
                            continue
                        tmi = 0 if t0 == 0 else 1 + (t0 - 64) // 128
                        dg, dgb = dgr.next()
                        ts("pool", dg[:n, :n], ID_F[:n, :n], comb[:n, tmi, ex_i:ex_i + 1], None, ALU.mult, None,
                           R=[cmB, combB], W=[dgb])
                        mm(psum[6][:, :n], ONES_F[:n, :], dg[:n, :n], True, True, R=[dgb, cmB], W=[psB[6]])
                        copy("act", cw[:, t0:t0 + n], psum[6][:, :n], R=[psB[6]], W=[cwb])

                for j in range(EXP // 512):
                    jobs.append((moe_w1_d[i2, ex_i], moe_w3_d[i2, ex_i], moe_w2_d[i2, ex_i], j, cw, cwb,
                                 pre if j == 0 else None))
            swiglu_jobs(L, jobs, 512, ffn_tiles, rings)
        else:
            X = ex[L]
            XE, XKc = X["E"], X["Kc"]
            lgc = lambda d_, hd, i2=i2: smallp[:, SP_LG + i2 * 8 + d_ * 4 + hd:SP_LG + i2 * 8 + d_ * 4 + hd + 1]
            g128c = lambda d_, hd, i2=i2: smallp[:, SP_G128 + i2 * 8 + d_ * 4 + hd:SP_G128 + i2 * 8 + d_ * 4 + hd + 1]
            new_phase()
            modulate(L, 0, 1, all_tiles)
            new_phase()
            QrT = AR.take([128, 2, NT], BF16)
            KrT = AR.take([128, 2, NT], BF16)
            qrB = [Buf(f"qr{t}") for t in range(5)]
            krB = [Buf(f"kr{t}") for t in range(5)]
            keep_off = AR.off
            load_rope()
            wr = Ring(2, lambda i: (AR.take([128, KC, 512], BF16), Buf(f"we{i}")))
            cs128 = AR.take([128, 256], BF16)
            csB = Buf("cs128")
            dma("pool", cs128, cs128_d[:, :], R=[], W=[csB])
            qnr = Ring(2, lambda i: (AR.take([128, 512], BF16), Buf(f"qn{i}")))
            tmpr = Ring(4, lambda i: (AR.take([128, 512], F32), Buf(f"rt{i}")))
            stg = Ring(3, lambda i: (AR.take([128, 512], BF16), Buf(f"stg{i}")))
            fT = bufB

            def ld_even(blk):
                wt, wb = wr.next()
                wload(wt, wb, w_in_even_d[i2], 0, 2048, blk * 512, 512)
                return wt, wb

            nxt = ld_even(0)
            for blk in range(4):
                wt, wb = nxt
                if blk + 1 < 4:
                    nxt = ld_even(blk + 1)
                if blk == 2:
                    v_project(wt, wb, XE, 1280, stg)
                    continue
                for ti in all_tiles:
                    t0, n = TT[ti]
                    for m in range(4):
                        p0 = m % 2
                        for kc in range(KC):
                            mm(psum[p0][:, :n], wt[:, kc, m * 128:(m + 1) * 128], bufA[:, kc, t0:t0 + n],
                               kc == 0, kc == KC - 1, R=[wb, aB[ti]], W=[psB[p0]])
                        if blk == 0:
                            copy("act", fT[:, m, t0:t0 + n], psum[p0][:, :n], R=[psB[p0]], W=[bB[ti]])
                        elif blk == 3:
                            act(bufB[:, 4 + m, t0:t0 + n], psum[p0][:, :n], AF.Silu, R=[psB[p0]], W=[bB[ti]])
                        else:
                            qt, qb = qnr.next()
                            if m < 2:
                                copy("act", qt[:, :n], psum[p0][:, :n], R=[psB[p0]], W=[qb])
                                rope(qt, qb, n, t0, QrT[:, m, t0:t0 + n], qrB[ti], 4 + m % 2, tmpr)
                            else:
                                S.op("act", lambda e, qt=qt, p0=p0, n=n: e.mul(out=qt[:, :n], in_=psum[p0][:, :n], mul=0.125),
                                     R=[psB[p0]], W=[qb])
                                rope(qt, qb, n, t0, KrT[:, m - 2, t0:t0 + n], krB[ti], 4 + m % 2, tmpr)
            for (t0, n) in TM:
                ti = 0 if t0 == 0 else 1 + (t0 - 64) // 512
                for gp in range(2):
                    p0 = 6 + gp
                    for g2 in range(2):
                        g = gp * 2 + g2
                        mm(psum[p0][:n, g2 * 256:(g2 + 1) * 256], fT[:, g, t0:t0 + n], cs128, True, True,
                           R=[bB[ti], csB], W=[psB[p0]])
                    at, ab = stg.next()
                    copy("act" if gp == 0 else "dve", at[:n, :], psum[p0][:n, :], R=[psB[p0]], W=[ab])
                    xstore(XE, t0, n, gp * 512, 512, at, ab)
                p0 = 5
                for c2 in range(2):
                    mm(psum[p0][:n, c2 * 128:(c2 + 1) * 128], KrT[:, c2, t0:t0 + n], ID_B, True, True,
                       R=[krB[ti], cmB], W=[psB[p0]])
                kt, kb = stg.next()
                copy("dve", kt[:n, 0:256], psum[p0][:n, 0:256], R=[psB[p0]], W=[kb])
                xstore(XE, t0, n, 1024, 256, kt, kb)
            for c2 in range(2):
                xstore(XKc, c2 * 128, 128, 0, 64, KrT[:, c2, :], krB[0])
            allgather(XE)
            allgather(XKc)
            S.phase += 1
            if S.phase_limit is not None and S.phase > S.phase_limit:
                S.enabled = False
            S.barrier()
            AR.off = keep_off
            AR2.reset()
            AR3.reset()
            rtab = AR.take([128, 4, 128], F32)
            rpos = AR.take([128, 4, 128], F32)
            rdist = AR.take([128, 4, 68], F32)
            rctx = AR.take([64, 4, 4, 64], F32)
            rcB = Buf("rconst")
            dma("sp", rtab, rtab_d[:, :, :], R=[], W=[rcB])
            dma("sp", rpos, rpos_d[:, :, :], R=[], W=[rcB])
            dma("sp", rdist, rdist_d[:, :, :], R=[], W=[rcB])
            dma("sp", rctx, rctx_d[:, :, :, :], R=[], W=[rcB])
            Dc = AR.take([128, 4, 128], F32)
            dq = AR.take([128, 2, 4, 128], BF16)
            dk = AR.take([128, 2, 4], F32)
            wk = AR.take([128, 2, 4, 68], F32)
            Dx = AR.take([64, 4, 4, 64], F32)
            dcB = Buf("dconst")
            t1 = AR.take([128, 128], F32)
            for hd in range(4):
                act(Dc[:, hd, :], rtab[:, 0, :], AF.Exp, R=[rcB, smB], W=[dcB], scale=lgc(0, hd))
                tt("dve", Dc[:, hd, :], Dc[:, hd, :], rtab[:, 1, :], ALU.mult, R=[dcB, rcB], W=[dcB])
                act(t1[:, :], rtab[:, 2, :], AF.Exp, R=[rcB, smB, dcB], W=[dcB], scale=lgc(1, hd))
                tt("dve", t1[:, :], t1[:, :], rtab[:, 3, :], ALU.mult, R=[dcB, rcB], W=[dcB])
                tt("dve", Dc[:, hd, :], Dc[:, hd, :], t1[:, :], ALU.add, R=[dcB], W=[dcB])
                for d_ in range(2):
                    act(dq[:, d_, hd, :], rpos[:, d_, :], AF.Exp, R=[rcB, smB], W=[dcB], scale=lgc(d_, hd))
                    act(dk[:, d_, hd:hd + 1], rpos[:, 2, d_:d_ + 1], AF.Exp, R=[rcB, smB], W=[dcB], scale=lgc(d_, hd))
                    act(wk[:, d_, hd, :], rdist[:, 2 * d_, :], AF.Exp, R=[rcB, smB], W=[dcB], scale=lgc(d_, hd))
                    tt("dve", wk[:, d_, hd, :], wk[:, d_, hd, :], rdist[:, 2 * d_ + 1, :], ALU.mult, R=[dcB, rcB], W=[dcB])
                for r in range(4):
                    act(Dx[:, hd, r, :], rctx[:, 0, r, :], AF.Exp, R=[rcB, smB], W=[dcB], scale=lgc(0, hd)[0:64, :])
                    tt("dve", Dx[:, hd, r, :], Dx[:, hd, r, :], rctx[:, 1, r, :], ALU.mult, R=[dcB, rcB], W=[dcB])
                    act(t1[0:64, 0:64], rctx[:, 2, r, :], AF.Exp, R=[rcB, smB, dcB], W=[dcB], scale=lgc(1, hd)[0:64, :])
                    tt("dve", t1[0:64, 0:64], t1[0:64, 0:64], rctx[:, 3, r, :], ALU.mult, R=[dcB, rcB], W=[dcB])
                    tt("dve", Dx[:, hd, r, :], Dx[:, hd, r, :], t1[0:64, 0:64], ALU.add, R=[dcB], W=[dcB])
            kvr = Ring(2, lambda i: (AR.take([128, 4, 768], BF16), Buf(f"kv{i}")))
            k2r = Ring(4, lambda i: (AR.take([128, 128], BF16), Buf(f"k2{i}")))

            def scaled_pair(src_ap, pr_, scal, R_, eng0="dve", eng1="pool"):
                k2, k2b = k2r.next()
                n_ = src_ap.shape[0]
                for hh in range(2):
                    hd = pr_ * 2 + hh
                    ts(eng0 if hh == 0 else eng1, k2[:n_, hh * 64:(hh + 1) * 64], src_ap[:, hd * 64:(hd + 1) * 64],
                       scal(hd), None, ALU.mult, None, R=R_, W=[k2b])
                return k2, k2b

            gi = 0
            for r in range(4):
                groups = [(0, [0])] + [(1 + 4 * q, [1 + 4 * q + u for u in range(4)]) for q in range(4)]
                for (tl0, tls) in groups:
                    kv, kvb = kvr.next()
                    if tl0 == 0:
                        xload_rows(XE, r, 0, 64, 1024, 768, kv[:, 0, :], kvb)
                    else:
                        xload_tiles(XE, r, 64 + (tl0 - 1) * 128, 512, 1024, 768, kv[:, 0:4, :], kvb)
                    for u, tl in enumerate(tls):
                        n = 64 if tl == 0 else 128
                        gi = r * 17 + tl
                        for d_ in range(2):
                            for pr_ in range(2):
                                k2, k2b = scaled_pair(kv[:n, u, :], pr_, lambda hd, d_=d_, gi=gi, n=n: wk[:n, d_, hd, gi:gi + 1], [kvb, dcB])
                                for hh in range(2):
                                    hd = pr_ * 2 + hh
                                    mm(psum[4 + d_][:, hd * 128:(hd + 1) * 128], k2[:n, :], kv[:n, u, 256 + hd * 128:256 + (hd + 1) * 128],
                                       gi == 0, gi == 67, R=[k2b, kvb], W=[psB[4 + d_]])
            Sf = AR2.take([128, 4, 128], F32)
            Sfb = AR2.take([128, 4, 128], BF16)
            Sbf = AR2.take([128, 4, 128], F32)
            okv = AR2.take([128, 16, 768], BF16)
            Sb_all = AR3.take([128, 16, 4, 128], BF16)
            stB = Buf("states")
            ps4v = psum[4][:, :].rearrange("p (h e) -> p h e", h=4)
            ps5v = psum[5][:, :].rearrange("p (h e) -> p h e", h=4)
            copy("dve", Sf[:, :, :], ps4v, R=[psB[4]], W=[stB])
            copy("act", Sfb[:, :, :], ps4v, R=[psB[4]], W=[stB])
            copy("dve", Sbf[:, :, :], ps5v, R=[psB[5]], W=[stB])
            okB = Buf("okv")
            xload_tiles(XE, 0, 64, NLAT, 1024, 768, okv, okB, own=True)
            for c in range(15, -1, -1):
                copy("act", Sb_all[:, c, :, :], Sbf[:, :, :], R=[stB], W=[stB])
                if c == 0:
                    break
                for pr_ in range(2):
                    k2, k2b = scaled_pair(okv[:, c, :], pr_, lambda hd: dk[:, 1, hd:hd + 1], [okB, dcB], "pool", "pool")
                    for hh in range(2):
                        hd = pr_ * 2 + hh
                        mm(psum[5][:, hd * 128:(hd + 1) * 128], k2[:, :], okv[:, c, 256 + hd * 128:256 + (hd + 1) * 128],
                           True, True, R=[k2b, okB], W=[psB[5]])
                for hd in range(4):
                    stt("dve", Sbf[:, hd, :], Sbf[:, hd, :], g128c(1, hd), ps5v[:, hd, :],
                        ALU.mult, ALU.add, R=[stB, psB[5], smB], W=[stB])
            qd = Ring(4, lambda i: (AR.take([128, 128], BF16), Buf(f"qd{i}")))
            sdr = Ring(3, lambda i: (AR.take([128, 128], BF16), Buf(f"sd{i}")))
            osq = Ring(2, lambda i: (AR.take([128, 512], BF16), Buf(f"osq{i}")))
            ors = Ring(2, lambda i: (AR.take([128, 512], F32), Buf(f"ors{i}")))
            oo = Ring(2, lambda i: (AR.take([128, 512], F32), Buf(f"oo{i}")))
            scr = Ring(2, lambda i: i)

            def finish_out(pso, hd, t0, n, ti):
                o_, ob = oo.next()
                copy("act", o_[:, :n], psum[pso][:, :n], R=[psB[pso]], W=[ob])
                sq_, sqb_ = osq.next()
                act(sq_[:, :n], psum[pso][:, :n], AF.Square, R=[psB[pso]], W=[sqb_])
                mm(psum[6][:, :n], ONES_B, sq_[:, :n], True, True, R=[sqb_, cmB], W=[psB[6]])
                rs_, rsb_ = ors.next()
                rstd_from_ps(psum[6][:, :n], psB[6], rs_[:, :n], rsb_, 1.0 / 128)
                tt("dve", o_[:, :n], o_[:, :n], rs_[:, :n], ALU.mult, R=[ob, rsb_], W=[ob])
                tt("dve", bufB[:, 4 + hd, t0:t0 + n], o_[:, :n], bufB[:, 4 + hd, t0:t0 + n], ALU.mult, R=[ob, bB[ti]], W=[bB[ti]])

            for c4 in range(4):
                ti = 1 + c4
                t0t, _ = TT[ti]
                for pr_ in range(2):
                    for cc in range(4):
                        c = c4 * 4 + cc
                        t0 = 64 + c * 128
                        k2, k2b = scaled_pair(okv[:, c, :], pr_, lambda hd: dk[:, 0, hd:hd + 1], [okB, dcB], "pool", "pool")
                        for hh in range(2):
                            hd = pr_ * 2 + hh
                            pso = 2 + hh
                            hp = hh * 64
                            cq = pr_
                            si = scr.next()
                            mm(psum[si][:, 0:128], KrT[hp:hp + 64, cq, t0:t0 + 128], QrT[hp:hp + 64, cq, t0:t0 + 128], True, True,
                               R=[krB[ti], qrB[ti]], W=[psB[si]])
                            sd, sdb = sdr.next()
                            tt("dve", sd[:, :], psum[si][:, 0:128], Dc[:, hd, :], ALU.mult, R=[psB[si], dcB], W=[sdb])
                            qf, qfb = qd.next()
                            qbk, qbb = qd.next()
                            tt("pool", qf[hp:hp + 64, :], QrT[hp:hp + 64, cq, t0:t0 + 128], dq[hp:hp + 64, 0, hd, :], ALU.mult,
                               R=[qrB[ti], dcB], W=[qfb])
                            tt("pool", qbk[hp:hp + 64, :], QrT[hp:hp + 64, cq, t0:t0 + 128], dq[hp:hp + 64, 1, hd, :], ALU.mult,
                               R=[qrB[ti], dcB], W=[qbb])
                            oc = psum[pso][:, cc * 128:(cc + 1) * 128]
                            mm(oc, okv[:, c, 256 + hd * 128:256 + (hd + 1) * 128], sd[:, :], True, False, R=[okB, sdb], W=[psB[pso]])
                            mm(oc, Sfb[hp:hp + 64, hd, :], qf[hp:hp + 64, :], False, False, R=[stB, qfb], W=[psB[pso]])
                            mm(oc, Sb_all[hp:hp + 64, c, hd, :], qbk[hp:hp + 64, :], False, True, R=[stB, qbb], W=[psB[pso]])
                            mm(psum[4][:, hd * 128:(hd + 1) * 128], k2[:, :], okv[:, c, 256 + hd * 128:256 + (hd + 1) * 128],
                               True, True, R=[k2b, okB], W=[psB[4]])
                            stt("dve", Sf[:, hd, :], Sf[:, hd, :], g128c(0, hd), ps4v[:, hd, :],
                                ALU.mult, ALU.add, R=[stB, psB[4], smB], W=[stB])
                            copy("act", Sfb[:, hd, :], Sf[:, hd, :], R=[stB], W=[stB])
                    for hh in range(2):
                        finish_out(2 + hh, pr_ * 2 + hh, t0t, 512, ti)
            kcs = AR.take([128, 2, 4, 64], BF16)
            kcB = Buf("kcs")
            for r in range(4):
                for c2 in range(2):
                    xload_rows(XKc, r, c2 * 128, 128, 0, 64, kcs[:, c2, r, :], kcB)
            vcs = AR2.take([64, 4, 512], BF16)
            vcB = Buf("vcs")
            for r in range(4):
                xload_rows(XE, r, 0, 64, 1280, 512, vcs[:, r, :], vcB)
            for hd in range(4):
                pso = 2 + hd % 2
                hp = (hd % 2) * 64
                cq = hd // 2
                for r in range(4):
                    si = scr.next()
                    mm(psum[si][0:64, 0:64], kcs[hp:hp + 64, cq, r, :], QrT[hp:hp + 64, cq, 0:64], True, True,
                       R=[kcB, qrB[0]], W=[psB[si]])
                    sd, sdb = sdr.next()
                    tt("dve", sd[0:64, 0:64], psum[si][0:64, 0:64], Dx[:, hd, r, :], ALU.mult, R=[psB[si], dcB], W=[sdb])
                    mm(psum[pso][:, 0:64], vcs[:, r, hd * 128:(hd + 1) * 128], sd[0:64, 0:64], r == 0, r == 3,
                       R=[vcB, sdb], W=[psB[pso]])
                finish_out(pso, hd, 0, 64, 0)
            new_phase()
            AR2.reset()
            AgA = AR2.take([128, 64, 256], BF16)
            dctx = AR2.take([64, 4, 2, 64], BF16)
            AgB = AR.take([128, 64, 256], BF16)
            agB = [Buf("AgA"), Buf("AgB")]
            Ags = [AgA, AgB]
            tbr = Ring(3, lambda i: (AR.take([128, 2, 4, 512], BF16), Buf(f"tb{i}")))
            dcxB = Buf("dctx")
            dma("pool", dctx, dftctx_d[:, :, :, :], R=[], W=[dcxB])
            actx = AR.take([64, 4, 1024], BF16)
            acxB = Buf("actx")
            for r in range(4):
                xload_rows(XE, r, 0, 64, 0, 1024, actx[:, r, :], acxB)

            def ld_tab(kt, tc):
                tb, tbb = tbr.next()
                row0 = (kt * 16 + tc) * 128
                dma("sp", tb[:, :, :, :].rearrange("p a b c -> p (a b c)"), dft_d[row0:row0 + 128, :], R=[], W=[tbb])
                return tb, tbb

            for gp in range(2):
                for g2 in range(2):
                    for r in range(4):
                        xload_tiles(XE, r, 64, NLAT, (gp * 2 + g2) * 256, 256, Ags[g2][:, r * 16:(r + 1) * 16, :], agB[g2])
                seq = [(kt, tc) for kt in range(4) for tc in range(16)]
                nxt = ld_tab(*seq[0])
                for qi, (kt, tc) in enumerate(seq):
                    tb, tbb = nxt
                    if qi + 1 < len(seq):
                        nxt = ld_tab(*seq[qi + 1])
                    for g2 in range(2):
                        pi = g2 * 2 + kt % 2
                        for j in range(4):
                            tl = tc * 4 + j
                            for cs_ in range(2):
                                mm(psum[pi][:, :], Ags[g2][:, tl, cs_ * 128:(cs_ + 1) * 128], tb[:, cs_, j, :],
                                   tc == 0 and j == 0 and cs_ == 0, tc == 15 and j == 3 and cs_ == 1,
                                   R=[agB[g2], tbb], W=[psB[pi]])
                        if tc == 15:
                            t0 = 64 + kt * 512
                            copy("act" if g2 == 0 else "dve", bufB[:, gp * 2 + g2, t0:t0 + 512], psum[pi][:, :],
                                 R=[psB[pi]], W=[bB[1 + kt]])
                for g2 in range(2):
                    g = gp * 2 + g2
                    for r in range(4):
                        for cs_ in range(2):
                            mm(psum[4 + g2][:, 0:64], actx[:, r, g * 256 + cs_ * 128:g * 256 + (cs_ + 1) * 128], dctx[:, r, cs_, :],
                               r == 0 and cs_ == 0, r == 3 and cs_ == 1, R=[acxB, dcxB], W=[psB[4 + g2]])
                    copy("act", bufB[:, g, 0:64], psum[4 + g2][:, 0:64], R=[psB[4 + g2]], W=[bB[0]])
            new_phase()
            out_proj_residual(L, w_out_even_d[i2], all_tiles)
            new_phase()
            modulate(L, 3, 4, all_tiles)
            rings = ffn_rings(256, 3)
            jobs = [(ffn_w1_d[i2], ffn_w3_d[i2], ffn_w2_d[i2], j, None, None) for j in range(FFN // 256)]
            swiglu_jobs(L, jobs, 256, all_tiles, rings)

    S.enabled = True
    S.phase_limit = None
    new_phase()
    yB = Buf("yT", keep=True)
    for kc in range(KC):
        dma("sp", yT_d[kc * 128:(kc + 1) * 128, :], xT[:, kc, 64:NT], R=xB, W=[yB])
    fin = Ins("sp", None, "c")
    fin.deps = set([yB.lw])
    S.q["sp"].append(fin)
    S.finalize()
    with nc.Block() as block:
        @block.tensor
        def _(e):
            S.emit(e, "pe")

        @block.scalar
        def _(e):
            S.emit(e, "act")

        @block.vector
        def _(e):
            S.emit(e, "dve")

        @block.gpsimd
        def _(e):
            S.emit(e, "pool")

        @block.sync
        def _(e):
            S.emit(e, "sp")
    return nc


def _rope_tables(core):
    r = core % 4
    quarter = 16
    inv_freq = 10000.0 ** (-np.arange(quarter, dtype=np.float32) / quarter)
    cos = np.ones((128, NT), np.float32)
    sin = np.zeros((128, NT), np.float32)
    tok = np.arange(r * NLAT, (r + 1) * NLAT)
    row = (tok // 64).astype(np.float32)
    colv = (tok % 64).astype(np.float32)
    ang = np.concatenate([row[:, None] * inv_freq, colv[:, None] * inv_freq], axis=-1).astype(np.float32)
    c = np.cos(ang).T
    s = np.sin(ang).T
    for p in range(128):
        d = p % 64
        fi = d % 32
        cos[p, 64:] = c[fi]
        sin[p, 64:] = -s[fi] if d < 32 else s[fi]
    return cos, sin


def _const_mats():
    cm = np.zeros((128, 6, 128), np.float32)
    cm[:, 0, :] = 1.0
    cm[0:64, 1, 0:64] = 1.0
    cm[64:128, 1, 64:128] = 1.0
    cm[:, 2, :] = np.eye(128, dtype=np.float32)
    for p in range(128):
        d = p % 64
        partner = (p - d) + ((d + 32) % 64)
        cm[p, 3, partner] = 1.0
    return cm


def _dft_tables(core):
    r = core % 4
    n = 8192
    tab = np.zeros((4, 16, 128, 2, 4, 512), ml_dtypes.bfloat16)
    sc = 1.0 / np.sqrt(n)
    p = np.arange(128, dtype=np.int64)[:, None, None]
    j = np.arange(4, dtype=np.int64)[None, :, None]
    k = np.arange(512, dtype=np.int64)[None, None, :]
    for kt in range(4):
        kk = r * NLAT + kt * 512 + k
        for tc in range(16):
            t = (tc * 4 + j) * 128 + p
            ph = ((t * kk) % n).astype(np.float64) * (2.0 * np.pi / n)
            tab[kt, tc, :, 0] = (np.cos(ph) * sc).astype(np.float32).astype(ml_dtypes.bfloat16)
            tab[kt, tc, :, 1] = (-np.sin(ph) * sc).astype(np.float32).astype(ml_dtypes.bfloat16)
    tab = tab.reshape(8192, 4096)
    n2 = 256
    dctx = np.zeros((64, 4, 2, 64), np.float32)
    for rr in range(4):
        tt_ = (np.arange(64) + rr * 64)[:, None]
        kk = (np.arange(64) + r * 64)[None, :]
        ph2 = ((tt_ * kk) % n2).astype(np.float64) * (2.0 * np.pi / n2)
        dctx[:, rr, 0, :] = np.cos(ph2) / np.sqrt(n2)
        dctx[:, rr, 1, :] = -np.sin(ph2) / np.sqrt(n2)
    return tab, dctx


def _cs128():
    c = np.arange(128)[:, None] * np.arange(128)[None, :]
    ph = (c % 128).astype(np.float64) * (2.0 * np.pi / 128)
    out = np.zeros((128, 256), np.float32)
    out[:, :128] = np.cos(ph) / np.sqrt(128.0)
    out[:, 128:] = np.sin(ph) / np.sqrt(128.0)
    return out


def _ret_tables(core):
    r = core % 4
    j = np.arange(128)[:, None].astype(np.float32)
    i = np.arange(128)[None, :].astype(np.float32)
    rtab = np.zeros((128, 4, 128), np.float32)
    rtab[:, 0, :] = np.maximum(i - j, 0)
    rtab[:, 1, :] = (j <= i)
    rtab[:, 2, :] = np.maximum(j - i, 0)
    rtab[:, 3, :] = (j > i)
    rpos = np.zeros((128, 4, 128), np.float32)
    rpos[:, 0, :] = np.arange(128)[None, :] + 1.0
    rpos[:, 1, :] = 128.0 - np.arange(128)[None, :]
    rpos[:, 2, 0] = 127.0 - np.arange(128)
    rpos[:, 2, 1] = np.arange(128)
    I0 = r * NLAT
    P0f = 256 + I0
    i_last = I0 + NLAT - 1
    P0b = 256 + 8191 - i_last
    rdist = np.zeros((128, 4, 68), np.float32)
    for rr in range(4):
        for tl in range(17):
            gi = rr * 17 + tl
            for p in range(128):
                if tl == 0:
                    if p >= 64:
                        continue
                    m = rr * 64 + p
                    pf = m
                    pb = 255 - m
                else:
                    li = rr * NLAT + (tl - 1) * 128 + p
                    pf = 256 + li
                    pb = 256 + 8191 - li
                if pf < P0f:
                    rdist[p, 0, gi] = P0f - 1 - pf
                    rdist[p, 1, gi] = 1.0
                if pb < P0b:
                    rdist[p, 2, gi] = P0b - 1 - pb
                    rdist[p, 3, gi] = 1.0
    rctx = np.zeros((64, 4, 4, 64), np.float32)
    for rr in range(4):
        mj = (rr * 64 + np.arange(64))[:, None].astype(np.float32)
        mi = (r * 64 + np.arange(64))[None, :].astype(np.float32)
        rctx[:, 0, rr, :] = np.maximum(mi - mj, 0)
        rctx[:, 1, rr, :] = (mj <= mi)
        rctx[:, 2, rr, :] = np.maximum(mj - mi, 0)
        rctx[:, 3, rr, :] = (mj > mi)
    return rtab, rpos, rdist, rctx


_CACHE = {}


def _get_program(n_layers=DEPTH):
    key = ("nc", n_layers)
    if key not in _CACHE:
        _CACHE[key] = build_program(n_layers)
    return _CACHE[key]


def _host_inputs(inp, n_layers=DEPTH):
    f32 = np.float32
    x = np.asarray(inp["x"], f32)
    ctx = np.asarray(inp["ctx"], f32)
    c = np.asarray(inp["c"], f32)
    c_ctx = np.asarray(inp["c_ctx"], f32)
    shared = {}
    n_even = (n_layers + 1) // 2
    n_odd = max(n_layers // 2, 1)
    shared["w_mod"] = np.ascontiguousarray(np.asarray(inp["w_mod"], f32)[:n_layers])
    for k in ("w_in_even", "w_out_even", "ffn_w1", "ffn_w3", "ffn_w2"):
        shared[k] = np.ascontiguousarray(np.asarray(inp[k], f32)[:n_even])
    for k in ("w_in_odd", "w_out_odd", "w_router", "moe_w1", "moe_w3", "moe_w2"):
        if n_layers >= 2:
            shared[k] = np.ascontiguousarray(np.asarray(inp[k], f32)[:n_odd])
    b_mod = np.asarray(inp["b_mod"], f32)
    shared["b_modT"] = np.ascontiguousarray(b_mod.reshape(4, 48, 128).transpose(2, 0, 1))
    sp = np.zeros((128, 1024), f32)
    dec = np.stack([np.asarray(inp["ret_decay_fwd"], f32), np.asarray(inp["ret_decay_bwd"], f32)], axis=1)
    sp[:, 0:16] = dec.reshape(1, 16)
    p64 = np.arange(128) % 64
    for i in range(2):
        sp[:, 16 + i] = np.asarray(inp["q_norm_gain"], f32)[i][p64]
        sp[:, 18 + i] = np.asarray(inp["k_norm_gain"], f32)[i][p64]
        sp[:, 20 + i] = np.asarray(inp["subln_gain"], f32)[i]
        lam = np.concatenate([np.asarray(inp[k], f32)[i] for k in ("lambda_q1", "lambda_k1", "lambda_q2", "lambda_k2")])
        sp[:, 32 + i * 256:32 + (i + 1) * 256] = lam[None, :]
    shared["smallp"] = sp
    shared["cmat"] = _const_mats()
    shared["cs128"] = _cs128()
    in_maps = []
    for core in range(NCORES):
        b, r = core // 4, core % 4
        m = dict(shared)
        xt = np.concatenate([ctx[b, r * 64:(r + 1) * 64, :], x[b, r * NLAT:(r + 1) * NLAT, :]], axis=0)
        m["xT"] = np.ascontiguousarray(xt.T)
        cT = np.stack([c[b], c_ctx], axis=-1)
        m["cT"] = np.ascontiguousarray(cT.reshape(8, 128, 2).transpose(1, 0, 2))
        key = ("tabs", r)
        if key not in _CACHE:
            cos, sin = _rope_tables(core)
            dtab, dctx = _dft_tables(core)
            rtab, rpos, rdist, rctx = _ret_tables(core)
            _CACHE[key] = dict(cosT=cos, sinT=sin, dft=dtab, dftctx=dctx, rtab=rtab, rpos=rpos, rdist=rdist, rctx=rctx)
        m.update(_CACHE[key])
        in_maps.append(m)
    return in_maps


def kernel(**inputs):
    nc = _get_program(DEPTH)
    in_maps = _host_inputs(inputs)
    res = run_bass_kernel_spmd(nc, in_maps, core_ids=list(range(NCORES)))
    out = np.zeros((2, 8192, D), np.float32)
    for core in range(NCORES):
        b, r = core // 4, core % 4
        out[b, r * NLAT:(r + 1) * NLAT, :] = np.asarray(res.results[core]["yT"], np.float32).T
    return out
```

```python
import math
import numpy as np
import ml_dtypes
import concourse.bass as bass
import concourse.mybir as mybir
from concourse.bass_utils import run_bass_kernel_spmd

F32 = mybir.dt.float32
BF16 = mybir.dt.bfloat16
AF = mybir.ActivationFunctionType
ALU = mybir.AluOpType
AX = mybir.AxisListType

NCORES = 8
D = 1024
KC = 8
NCTX = 64
NLAT = 2048
NT = NCTX + NLAT
TT = [(0, 64), (64, 512), (576, 512), (1088, 512), (1600, 512)]
TM = [(0, 64)] + [(64 + 128 * i, 128) for i in range(16)]
EPS = 1e-6
FFN = 2816
EXP = 3584
NEXP = 8
DEPTH = 4
ENGS = ("pe", "act", "dve", "pool", "sp")


PHASE_BUFS = []


class Buf:
    def __init__(self, name, keep=False):
        self.name = name
        if not keep:
            PHASE_BUFS.append(self)
        self.lw = None
        self.readers = {}
        self.dma_readers = []
        self.dsem = None
        self.dcount = 0


class Ins:
    __slots__ = ("eng", "fn", "deps", "kind", "dsem", "dcount", "sig", "need", "waits", "idx")

    def __init__(self, eng, fn, kind):
        self.eng = eng
        self.fn = fn
        self.kind = kind
        self.deps = set()
        self.dsem = None
        self.dcount = 0
        self.sig = None
        self.need = False
        self.waits = None


class Sched:
    def __init__(self, nc):
        self.nc = nc
        self.q = {e: [] for e in ENGS}
        self.last = {e: None for e in ENGS}
        self.dmas_since_bar = []
        self.nsem = 0
        self.enabled = True
        self.free_dsems = {}
        self.phase = 0
        self.phase_limit = None

    def newsem(self):
        self.nsem += 1
        return self.nc.alloc_semaphore(name=f"s{self.nsem}")

    def op(self, eng, fn, R=(), W=(), kind="c"):
        if not self.enabled:
            return None
        ins = Ins(eng, fn, kind)
        for b in R:
            if b.lw is not None:
                ins.deps.add(b.lw)
        for b in W:
            if b.lw is not None:
                ins.deps.add(b.lw)
            for r in b.readers.values():
                ins.deps.add(r)
            for r in b.dma_readers:
                ins.deps.add(r)
        ins.deps.discard(ins)
        for b in R:
            if kind == "d":
                b.dma_readers.append(ins)
            else:
                b.readers[eng] = ins
        for b in W:
            b.lw = ins
            b.readers = {}
            b.dma_readers = []
        if kind == "d":
            tgt = W[0]
            if tgt.dsem is None:
                tgt.dq = eng
                fl = self.free_dsems.setdefault(eng, [])
                if fl:
                    tgt.dsem, tgt.dcount = fl.pop()
                else:
                    tgt.dsem = self.newsem()
            assert tgt.dq == eng, (tgt.name, tgt.dq, eng)
            tgt.dcount += 16
            ins.dsem = tgt.dsem
            ins.dcount = tgt.dcount
            self.dmas_since_bar.append(ins)
        elif kind == "cc":
            tgt = getattr(W[0], "owner", W[0])
            if tgt.dsem is None:
                tgt.dsem = self.newsem()
            tgt.dcount += 1
            ins.dsem = tgt.dsem
            ins.dcount = tgt.dcount
        self.q[eng].append(ins)
        if kind == "c":
            self.last[eng] = ins
        return ins

    def barrier(self):
        if not self.enabled:
            return
        deps = set(x for x in self.last.values() if x is not None) | set(self.dmas_since_bar)
        self.dmas_since_bar = []
        for e in ENGS:
            ins = Ins(e, None, "c")
            ins.deps = set(deps)
            self.q[e].append(ins)

    def finalize(self):
        for e in ENGS:
            for ins in self.q[e]:
                for d in ins.deps:
                    if d.kind == "c":
                        if d.eng == "pe" and ins.eng == "pe" and ins.kind == "c":
                            continue
                        d.need = True
        self.engsem = {}
        for e in ENGS:
            cur = None
            cnt = 0
            for ins in self.q[e]:
                if ins.kind == "c" and ins.need and ins.fn is not None:
                    if cur is None or cnt >= 20000:
                        cur = self.newsem()
                        cnt = 0
                    cnt += 1
                    ins.sig = (cur, cnt)
        for e in ENGS:
            waited = {}
            for ins in self.q[e]:
                w = {}
                for d in ins.deps:
                    if d.kind == "c":
                        if d.eng == "pe" and ins.eng == "pe" and ins.kind == "c":
                            continue
                        if d.sig is None:
                            continue
                        sem, cnt = d.sig
                    else:
                        sem, cnt = d.dsem, d.dcount
                    k = id(sem)
                    if waited.get(k, (None, 0))[1] >= cnt:
                        continue
                    if k not in w or w[k][1] < cnt:
                        w[k] = (sem, cnt)
                for k, v in w.items():
                    waited[k] = v
                ins.waits = list(w.values())

    def emit(self, e, eng):
        for ins in self.q[eng]:
            for sem, cnt in ins.waits:
                e.wait_ge(sem, cnt)
            if ins.fn is None:
                continue
            r = ins.fn(e)
            if ins.kind == "d":
                r.then_inc(ins.dsem, 16)
            elif ins.kind == "cc":
                r.then_inc(ins.dsem)
            elif ins.sig is not None:
                r.then_inc(ins.sig[0], 1)


def build_program(n_layers=DEPTH, stop_after=None):
    del PHASE_BUFS[:]
    nc = bass.Bass("TRN2", target_bir_lowering=False)
    S = Sched(nc)
    S.phase_limit = stop_after

    ODD = ("w_in_odd", "w_out_odd", "w_router", "moe_w1", "moe_w3", "moe_w2")

    def din(name, shape, dt=F32):
        if name in ODD and n_layers < 2:
            return None
        return nc.dram_tensor(name, list(shape), dt, kind="ExternalInput").ap()

    xT_d = din("xT", [D, NT])
    cT_d = din("cT", [128, KC, 2])
    n_even = (n_layers + 1) // 2
    n_odd = max(n_layers // 2, 1)
    wmod_d = din("w_mod", [n_layers, D, 6 * D])
    bmod_d = din("b_modT", [128, 4, 48])
    w_in_even_d = din("w_in_even", [n_even, D, 2048])
    w_out_even_d = din("w_out_even", [n_even, D, D])
    ffn_w1_d = din("ffn_w1", [n_even, D, FFN])
    ffn_w3_d = din("ffn_w3", [n_even, D, FFN])
    ffn_w2_d = din("ffn_w2", [n_even, FFN, D])
    w_in_odd_d = din("w_in_odd", [n_odd, D, 3072])
    w_out_odd_d = din("w_out_odd", [n_odd, D, D])
    w_router_d = din("w_router", [n_odd, D, NEXP])
    moe_w1_d = din("moe_w1", [n_odd, NEXP, D, EXP])
    moe_w3_d = din("moe_w3", [n_odd, NEXP, D, EXP])
    moe_w2_d = din("moe_w2", [n_odd, NEXP, EXP, D])
    smallp_d = din("smallp", [128, 1024])
    cosT_d = din("cosT", [128, NT])
    sinT_d = din("sinT", [128, NT])
    cmat_d = din("cmat", [128, 6, 128])
    cs128_d = din("cs128", [128, 256])
    dft_d = din("dft", [8192, 4096], BF16)
    dftctx_d = din("dftctx", [64, 4, 2, 64])
    rtab_d = din("rtab", [128, 4, 128])
    rpos_d = din("rpos", [128, 4, 128])
    rdist_d = din("rdist", [128, 4, 68])
    rctx_d = din("rctx", [64, 4, 4, 64])
    yT_d = nc.dram_tensor("yT", [D, NLAT], F32, kind="ExternalOutput").ap()

    class XT:
        def __init__(self, name, rblocks, cblocks):
            self.rb = rblocks
            self.cb = cblocks
            self.ch = {}
            self.xb = Buf(name + "xb", keep=True)
            self.gb = Buf(name + "gb", keep=True)
            for i, (r0, rn) in enumerate(rblocks):
                for j, (c0, cw) in enumerate(cblocks):
                    xt_ = nc.dram_tensor(f"{name}x{i}_{j}", [rn, cw], BF16)
                    gt_ = nc.dram_tensor(f"{name}g{i}_{j}", [4 * rn, cw], BF16)
                    gbc = Buf(f"{name}gb{i}_{j}", keep=True)
                    gbc.owner = self.gb
                    self.ch[(i, j)] = (xt_, gt_, self.xb, gbc)

        def pieces(self, r0, n, c0, w):
            out = []
            for i, (b0, bn) in enumerate(self.rb):
                lo, hi = max(r0, b0), min(r0 + n, b0 + bn)
                if lo >= hi:
                    continue
                for j, (d0, dw) in enumerate(self.cb):
                    cl, chh = max(c0, d0), min(c0 + w, d0 + dw)
                    if cl >= chh:
                        continue
                    out.append((i, j, lo - b0, hi - lo, cl - d0, chh - cl, lo - r0, cl - c0))
            return out

        def wr(self, r0, n, c0, w):
            res = []
            for (i, j, br, nn, bc, ww, dr, dc) in self.pieces(r0, n, c0, w):
                xt_, gt_, xb_, gb_ = self.ch[(i, j)]
                res.append((xt_.ap()[br:br + nn, bc:bc + ww], xb_, dr, nn, dc, ww))
            return res

        def rd(self, rank, r0, n, c0, w, own=False):
            res = []
            for (i, j, br, nn, bc, ww, dr, dc) in self.pieces(r0, n, c0, w):
                xt_, gt_, xb_, gb_ = self.ch[(i, j)]
                if own:
                    res.append((xt_.ap()[br:br + nn, bc:bc + ww], xb_, dr, nn, dc, ww))
                else:
                    rn = self.rb[i][1]
                    res.append((gt_.ap()[rank * rn + br:rank * rn + br + nn, bc:bc + ww], gb_, dr, nn, dc, ww))
            return res

    TTB = [(t0, n) for (t0, n) in [(0, 64), (64, 512), (576, 512), (1088, 512), (1600, 512)]]
    ex = {}
    for L in range(n_layers):
        if L % 2 == 1:
            ex[L] = dict(KT=XT(f"KT{L}", [(i * 256, 256) for i in range(4)], TTB),
                         V=XT(f"V{L}", TTB, [(i * 256, 256) for i in range(4)]))
        else:
            ex[L] = dict(E=XT(f"E{L}", TTB, [(0, 512), (512, 512), (1024, 256), (1280, 512)]),
                         Kc=XT(f"Kc{L}", [(0, 256)], [(0, 64)]))

    def sb(name, shape, dt):
        return nc.alloc_sbuf_tensor(name, list(shape), dt)

    xT = sb("xT_s", [128, KC, NT], F32)
    bufA = sb("bufA", [128, KC, NT], BF16)
    bufB = sb("bufB", [128, KC, NT], BF16)
    xB = [Buf(f"x{t}", keep=True) for t in range(5)]
    aB = [Buf(f"a{t}", keep=True) for t in range(5)]
    bB = [Buf(f"b{t}", keep=True) for t in range(5)]
    modt = sb("modt", [128, 4, 48, 2], F32)
    modB = Buf("mod", keep=True)
    smallp = sb("smallp_s", [128, 1024], F32)
    smB = Buf("smallp", keep=True)
    cmat_f = sb("cmat_f", [128, 6, 128], F32)
    cmat_b = sb("cmat_b", [128, 6, 128], BF16)
    cmB = Buf("cmat", keep=True)
    ONES_B = cmat_b[:, 0, :]
    BD64_B = cmat_b[:, 1, :]
    ID_B = cmat_b[:, 2, :]
    PERM_B = cmat_b[:, 3, :]
    ONES_F = cmat_f[:, 0, :]
    ID_F = cmat_f[:, 2, :]
    ARENA = 66000
    comb = sb("comb_s", [128, 17, 8], F32)
    combB = Buf("comb", keep=True)
    arena = sb("arena", [128, ARENA // 2], BF16)

    class Arena:
        def __init__(self, backing=None, cap=None):
            self.off = 0
            self.backing = backing
            self.cap = cap

        def reset(self):
            self.off = 0

        def take(self, shape, dt):
            n = int(np.prod(shape[1:]))
            nb = n * (2 if dt == BF16 else 4)
            nb_al = (nb + 63) // 64 * 64
            bk = arena if self.backing is None else self.backing
            cap = ARENA if self.cap is None else self.cap
            assert self.off + nb_al <= cap, (self.off, nb_al, cap)
            v = bk[0:shape[0], self.off // 2:(self.off + nb) // 2]
            if dt == F32:
                v = v.bitcast(F32)
            self.off += nb_al
            if len(shape) == 3:
                v = v.rearrange("p (a b) -> p a b", a=shape[1])
            elif len(shape) == 4:
                v = v.rearrange("p (a b c) -> p a b c", a=shape[1], b=shape[2])
            return v

    AR = Arena()
    bufA_flat = bufA[:, :, :].rearrange("p a b -> p (a b)")
    bufB_flat = bufB[:, :, :].rearrange("p a b -> p (a b)")
    AR2 = Arena(bufA_flat, KC * NT * 2)
    AR3 = Arena(bufB_flat, 4 * NT * 2)
    rope_state = {}

    def load_rope():
        c_ = AR.take([128, NT], F32)
        s_ = AR.take([128, NT], F32)
        b_ = Buf("rope")
        dma("sp", c_, cosT_d[:, :], R=[], W=[b_])
        dma("sp", s_, sinT_d[:, :], R=[], W=[b_])
        rope_state["c"], rope_state["s"], rope_state["b"] = c_, s_, b_
    psum = [nc.alloc_psum_tensor(f"ps{i}", [128, 512], F32) for i in range(8)]
    psB = [Buf(f"ps{i}", keep=True) for i in range(8)]

    class Ring:
        def __init__(self, n, mk):
            self.items = [mk(i) for i in range(n)]
            self.i = 0

        def next(self):
            it = self.items[self.i % len(self.items)]
            self.i += 1
            return it

    def new_phase():
        S.phase += 1
        if S.phase_limit is not None and S.phase > S.phase_limit:
            S.enabled = False
        S.barrier()
        for b_ in PHASE_BUFS:
            if b_.dsem is not None and getattr(b_, "dq", None) is not None:
                S.free_dsems.setdefault(b_.dq, []).append((b_.dsem, b_.dcount))
        del PHASE_BUFS[:]
        AR.reset()

    def mm(out, lhsT, rhs, start, stop, R, W):
        S.op("pe", lambda e: e.matmul(out, lhsT, rhs, start=start, stop=stop), R=R, W=W)

    def act(out, in_, func, R, W, bias=None, scale=None):
        kw = {}
        if bias is not None:
            kw["bias"] = bias
        if scale is not None:
            kw["scale"] = scale
        S.op("act", lambda e: e.activation(out=out, in_=in_, func=func, **kw), R=R, W=W)

    def tt(eng, out, in0, in1, op, R, W):
        S.op(eng, lambda e: e.tensor_tensor(out=out, in0=in0, in1=in1, op=op), R=R, W=W)

    def ts(eng, out, in0, s1, s2, op0, op1, R, W):
        if s2 is None:
            S.op(eng, lambda e: e.tensor_scalar(out=out, in0=in0, scalar1=s1, scalar2=None, op0=op0), R=R, W=W)
        else:
            S.op(eng, lambda e: e.tensor_scalar(out=out, in0=in0, scalar1=s1, scalar2=s2, op0=op0, op1=op1), R=R, W=W)

    def stt(eng, out, in0, scalar, in1, op0, op1, R, W):
        S.op(eng, lambda e: e.scalar_tensor_tensor(out=out, in0=in0, scalar=scalar, in1=in1, op0=op0, op1=op1), R=R, W=W)

    def recip(out, in_, R, W):
        S.op("dve", lambda e: e.reciprocal(out=out, in_=in_), R=R, W=W)

    def copy(eng, out, in_, R, W):
        if eng == "act":
            S.op("act", lambda e: e.copy(out=out, in_=in_), R=R, W=W)
        else:
            S.op(eng, lambda e: e.tensor_copy(out=out, in_=in_), R=R, W=W)

    def dma(q, out, in_, R, W):
        S.op(q, lambda e: e.dma_start(out=out, in_=in_), R=R, W=W, kind="d")

    def rstd_from_ps(ps_ap, psbuf, out_ap, outbuf, n_inv):
        act(out_ap, ps_ap, AF.Ln, R=[psbuf], W=[outbuf], bias=epsb[:, 0:1], scale=n_inv)
        act(out_ap, out_ap, AF.Exp, R=[outbuf], W=[outbuf], scale=-0.5)

    epsb = sb("epsb", [128, 2], F32)
    epsB = Buf("eps", keep=True)
    S.op("dve", lambda e: e.memset(epsb[:, 0:1], EPS), W=[epsB])
    S.op("dve", lambda e: e.memset(epsb[:, 1:2], 1.0), W=[epsB])
    for kc in range(KC):
        dma("sp", xT[:, kc, :], xT_d[kc * 128:(kc + 1) * 128, :], R=[], W=xB)
    dma("sp", smallp[:], smallp_d[:, :], R=[], W=[smB])
    dma("sp", cmat_f[:], cmat_d[:, :, :], R=[], W=[cmB])
    cmB2 = Buf("cmat_b", keep=True)
    dma("pool", cmat_b[:], cmat_d[:, :, :], R=[], W=[cmB2])
    S.op("dve", lambda e: e.memset(epsb[:, 1:2], 1.0), R=[cmB2], W=[epsB])

    SP_DEC = 0
    SP_QG = 16
    SP_KG = 18
    SP_SUB = 20
    SP_LAM = 32
    SP_LG = 600
    SP_LAMNEG = 620
    SP_SUBG = 624
    SP_G128 = 630
    SP_KG8 = 650
    SP_TMP = 700

    act(smallp[:, SP_LG:SP_LG + 16], smallp[:, SP_DEC:SP_DEC + 16], AF.Exp, R=[smB], W=[smB], scale=-1.0)
    act(smallp[:, SP_LG:SP_LG + 16], smallp[:, SP_LG:SP_LG + 16], AF.Ln, R=[smB, epsB], W=[smB], bias=epsb[:, 1:2])
    ts("dve", smallp[:, SP_LG:SP_LG + 16], smallp[:, SP_LG:SP_LG + 16], -1.0, None, ALU.mult, None, R=[smB], W=[smB])
    act(smallp[:, SP_G128:SP_G128 + 16], smallp[:, SP_LG:SP_LG + 16], AF.Exp, R=[smB], W=[smB], scale=128.0)
    for i in range(2):
        layer = 2 * i + 1
        lam_init = 0.8 - 0.6 * float(np.exp(-0.3 * layer))
        base = SP_LAM + i * 256
        for j in range(2):
            tt("dve", smallp[:, SP_TMP:SP_TMP + 64], smallp[:, base + j * 128:base + j * 128 + 64],
               smallp[:, base + j * 128 + 64:base + j * 128 + 128], ALU.mult, R=[smB], W=[smB])
            S.op("dve", lambda e, j=j: e.reduce_sum(out=smallp[:, SP_TMP + 64 + j:SP_TMP + 65 + j],
                                                   in_=smallp[:, SP_TMP:SP_TMP + 64], axis=AX.X), R=[smB], W=[smB])
        act(smallp[:, SP_TMP + 64:SP_TMP + 66], smallp[:, SP_TMP + 64:SP_TMP + 66], AF.Exp, R=[smB], W=[smB])
        tt("dve", smallp[:, SP_LAMNEG + i:SP_LAMNEG + i + 1], smallp[:, SP_TMP + 65:SP_TMP + 66],
           smallp[:, SP_TMP + 64:SP_TMP + 65], ALU.subtract, R=[smB], W=[smB])
        ts("dve", smallp[:, SP_LAMNEG + i:SP_LAMNEG + i + 1], smallp[:, SP_LAMNEG + i:SP_LAMNEG + i + 1],
           -lam_init, None, ALU.add, None, R=[smB], W=[smB])
        ts("dve", smallp[:, SP_SUBG + i:SP_SUBG + i + 1], smallp[:, SP_SUB + i:SP_SUB + i + 1],
           1.0 - lam_init, None, ALU.mult, None, R=[smB], W=[smB])

    AR.reset()
    csil_f = AR.take([128, KC, 2], F32)
    csil = AR.take([128, KC, 2], BF16)
    bmod_s = AR.take([128, 4, 48], F32)
    cB = Buf("csil")
    dma("sp", csil_f, cT_d[:, :, :], R=[], W=[cB])
    dma("sp", bmod_s, bmod_d[:, :, :], R=[], W=[cB])
    act(csil, csil_f, AF.Silu, R=[cB], W=[cB])
    wring = Ring(3, lambda i: (AR.take([128, KC, 1024], BF16), Buf(f"wm{i}")))
    pr = Ring(2, lambda i: i)
    for L in range(n_layers):
        for blk in range(6):
            wt, wb = wring.next()
            dma("pool", wt, wmod_d[L, :, blk * 1024:(blk + 1) * 1024].rearrange("(k p) n -> p k n", p=128), R=[], W=[wb])
            pi = pr.next()
            for m in range(8):
                for kc in range(KC):
                    mm(psum[pi][:, 2 * m:2 * m + 2], wt[:, kc, m * 128:(m + 1) * 128], csil[:, kc, :],
                       kc == 0, kc == KC - 1, R=[wb, cB], W=[psB[pi]])
            for col in range(2):
                tt("dve", modt[:, L, blk * 8:(blk + 1) * 8, col],
                   psum[pi][:, 0:16].rearrange("p (m c) -> p m c", c=2)[:, :, col],
                   bmod_s[:, L, blk * 8:(blk + 1) * 8], ALU.add, R=[psB[pi], cB], W=[modB])
        for which in (1, 4):
            ts("dve", modt[:, L, which * 8:(which + 1) * 8, :], modt[:, L, which * 8:(which + 1) * 8, :],
               1.0, None, ALU.add, None, R=[modB], W=[modB])

    def modap(L, which, kc, col):
        return modt[:, L, which * 8 + kc, col:col + 1]

    def modulate(L, w_shift, w_scale, tiles, hook=None):
        sq = Ring(2, lambda i: (AR.take([128, 512], BF16), Buf(f"msq{i}")))
        rs = Ring(2, lambda i: (AR.take([128, 512], F32), Buf(f"mrs{i}")))
        tmp = Ring(2, lambda i: (AR.take([128, 512], F32), Buf(f"mtmp{i}")))
        pring = Ring(2, lambda i: i)
        for ti in tiles:
            t0, n = TT[ti]
            col = 1 if ti == 0 else 0
            pi = pring.next()
            for kc in range(KC):
                sqt, sqb = sq.next()
                act(sqt[:, :n], xT[:, kc, t0:t0 + n], AF.Square, R=[xB[ti]], W=[sqb])
                mm(psum[pi][:, :n], ONES_B, sqt[:, :n], kc == 0, kc == KC - 1, R=[sqb, cmB], W=[psB[pi]])
            rst, rsb = rs.next()
            rstd_from_ps(psum[pi][:, :n], psB[pi], rst[:, :n], rsb, 1.0 / D)
            for kc in range(KC):
                tmt, tmb = tmp.next()
                tt("dve", tmt[:, :n], xT[:, kc, t0:t0 + n], rst[:, :n], ALU.mult, R=[xB[ti], rsb], W=[tmb])
                if hook is not None:
                    hook(ti, kc, tmt, tmb, n, col)
                act(bufA[:, kc, t0:t0 + n], tmt[:, :n], AF.Identity, R=[tmb, modB], W=[aB[ti]],
                    bias=modap(L, w_shift, kc, col), scale=modap(L, w_scale, kc, col))

    def wload(dst, dstb, src2d, k0, ncols_total, c0, ncols):
        nk = dst.shape[1]
        dma("pool", dst, src2d[k0 * 128:(k0 + nk) * 128, c0:c0 + ncols].rearrange("(k p) n -> p k n", p=128), R=[], W=[dstb])

    def rope(src, srcb, n, t0, out_ap, outb, ps_i, tmpring):
        mm(psum[ps_i][:, :n], PERM_B, src[:, :n], True, True, R=[srcb, cmB], W=[psB[ps_i]])
        t1, t1b = tmpring.next()
        t2, t2b = tmpring.next()
        cosT, sinT, ropeB = rope_state["c"], rope_state["s"], rope_state["b"]
        tt("pool", t1[:, :n], src[:, :n], cosT[:, t0:t0 + n], ALU.mult, R=[srcb, ropeB], W=[t1b])
        tt("dve", t2[:, :n], psum[ps_i][:, :n], sinT[:, t0:t0 + n], ALU.mult, R=[psB[ps_i], ropeB], W=[t2b])
        tt("dve", out_ap, t1[:, :n], t2[:, :n], ALU.add, R=[t1b, t2b], W=[outb])

    def out_proj_residual(L, wsrc, tiles):
        wr = Ring(2, lambda i: (AR.take([128, KC, 512], BF16), Buf(f"wo{i}")))
        pring = Ring(4, lambda i: i)
        slots = []
        for blk in range(2):
            wt, wb = wr.next()
            wload(wt, wb, wsrc, 0, D, blk * 512, 512)
            slots.append((wt, wb))
        for blk in range(2):
            wt, wb = slots[blk]
            for ti in tiles:
                t0, n = TT[ti]
                col = 1 if ti == 0 else 0
                for m in range(4):
                    o = blk * 4 + m
                    pi = pring.next()
                    for kc in range(KC):
                        mm(psum[pi][:, :n], wt[:, kc, m * 128:(m + 1) * 128], bufB[:, kc, t0:t0 + n],
                           kc == 0, kc == KC - 1, R=[wb, bB[ti]], W=[psB[pi]])
                    stt("dve", xT[:, o, t0:t0 + n], psum[pi][:, :n], modap(L, 2, o, col), xT[:, o, t0:t0 + n],
                        ALU.mult, ALU.add, R=[psB[pi], modB, xB[ti]], W=[xB[ti]])

    def swiglu_jobs(L, jobs, blkw, tiles, rings):
        wr, gr, sr = rings
        nm = blkw // 128

        def load(job):
            w1src, w3src, w2src, j, cw, cwb = job[:6]
            if len(job) > 6 and job[6] is not None:
                job[6]()
            (w1t, w3t, w2t), wb = wr.next()
            wload(w1t, wb, w1src, 0, 0, j * blkw, blkw)
            wload(w3t, wb, w3src, 0, 0, j * blkw, blkw)
            dma("pool", w2t, w2src[j * blkw:(j + 1) * blkw, :].rearrange("(k p) n -> p k n", p=128), R=[], W=[wb])
            return (w1t, w3t, w2t, wb)

        def stage_ab(w, ti, cw, cwb):
            w1t, w3t, w2t, wb = w
            t0, n = TT[ti]
            gt, gb = gr.next()
            for m in range(nm):
                pa = m % 2
                pb = 2 + m % 2
                for kc in range(KC):
                    mm(psum[pa][:, :n], w1t[:, kc, m * 128:(m + 1) * 128], bufA[:, kc, t0:t0 + n],
                       kc == 0, kc == KC - 1, R=[wb, aB[ti]], W=[psB[pa]])
                for kc in range(KC):
                    mm(psum[pb][:, :n], w3t[:, kc, m * 128:(m + 1) * 128], bufA[:, kc, t0:t0 + n],
                       kc == 0, kc == KC - 1, R=[wb, aB[ti]], W=[psB[pb]])
                st, sbf = sr.next()
                act(st[:, :n], psum[pa][:, :n], AF.Silu, R=[psB[pa]], W=[sbf])
                if cw is None:
                    tt("dve", gt[:, m, :n], st[:, :n], psum[pb][:, :n], ALU.mult, R=[sbf, psB[pb]], W=[gb])
                else:
                    tt("dve", st[:, :n], st[:, :n], psum[pb][:, :n], ALU.mult, R=[sbf, psB[pb]], W=[sbf])
                    tt("dve", gt[:, m, :n], st[:, :n], cw[:, t0:t0 + n], ALU.mult, R=[sbf, cwb], W=[gb])
            return (gt, gb)

        def stage_w2(w, ti, g):
            w1t, w3t, w2t, wb = w
            gt, gb = g
            t0, n = TT[ti]
            col = 1 if ti == 0 else 0
            for o in range(KC):
                py = 4 + o % 4
                for m in range(nm):
                    mm(psum[py][:, :n], w2t[:, m, o * 128:(o + 1) * 128], gt[:, m, :n],
                       m == 0, m == nm - 1, R=[wb, gb], W=[psB[py]])
                stt("dve", xT[:, o, t0:t0 + n], psum[py][:, :n], modap(L, 5, o, col), xT[:, o, t0:t0 + n],
                    ALU.mult, ALU.add, R=[psB[py], modB, xB[ti]], W=[xB[ti]])

        nxt = load(jobs[0])
        pend = None
        for ji, job in enumerate(jobs):
            w = nxt
            if pend is not None:
                stage_w2(*pend)
                pend = None
            if ji + 1 < len(jobs):
                nxt = load(jobs[ji + 1])
            for ti in tiles:
                g = stage_ab(w, ti, job[4], job[5])
                if pend is not None:
                    stage_w2(*pend)
                pend = (w, ti, g)
        if pend is not None:
            stage_w2(*pend)

    def ffn_rings(blkw, nslots):
        nm = blkw // 128
        wr = Ring(nslots, lambda i: ((AR.take([128, KC, blkw], BF16), AR.take([128, KC, blkw], BF16),
                                      AR.take([128, nm, D], BF16)), Buf(f"fw{i}")))
        gr = Ring(2, lambda i: (AR.take([128, nm, 512], BF16), Buf(f"g{i}")))
        sr = Ring(3, lambda i: (AR.take([128, 512], F32), Buf(f"s{i}")))
        return wr, gr, sr

    AG = [[0, 1, 2, 3], [4, 5, 6, 7]]

    def allgather(xt, keys=None):
        for key in (keys if keys is not None else list(xt.ch.keys())):
            xt_, gt_, xb_, gb_ = xt.ch[key]
            S.op("pool", lambda e, xt_=xt_, gt_=gt_: e.collective_compute(
                "AllGather", ALU.bypass, replica_groups=AG, ins=[xt_.ap().opt()], outs=[gt_.ap().opt()]),
                R=[xb_], W=[gb_], kind="cc")

    def xstore(xt, r0, n, c0, w, src, srcb):
        for (ap_, buf_, dr, nn, dc, ww) in xt.wr(r0, n, c0, w):
            dma("sp", ap_, src[dr:dr + nn, dc:dc + ww], R=[srcb], W=[buf_])

    def xload_rows(xt, rank, r0, n, c0, w, dst, dstb, own=False):
        for (ap_, buf_, dr, nn, dc, ww) in xt.rd(rank, r0, n, c0, w, own):
            dma("sp", dst[dr:dr + nn, dc:dc + ww], ap_, R=[buf_], W=[dstb])

    def xload_tiles(xt, rank, r0, n, c0, w, dst, dstb, own=False):
        for (ap_, buf_, dr, nn, dc, ww) in xt.rd(rank, r0, n, c0, w, own):
            assert dr % 128 == 0 and nn % 128 == 0
            dma("sp", dst[:, dr // 128:(dr + nn) // 128, dc:dc + ww], ap_.rearrange("(t p) c -> p t c", p=128),
                R=[buf_], W=[dstb])

    def v_project(wt, wb, dst, c0, stg):
        for (t0, n) in TM:
            ti = 0 if t0 == 0 else 1 + (t0 - 64) // 512
            p0 = 6 + (t0 // 128) % 2
            for kc in range(KC):
                mm(psum[p0][:n, :], bufA[:, kc, t0:t0 + n], wt[:, kc, :], kc == 0, kc == KC - 1,
                   R=[wb, aB[ti]], W=[psB[p0]])
            vt, vb2 = stg.next()
            copy("act", vt[:n, :], psum[p0][:n, :], R=[psB[p0]], W=[vb2])
            xstore(dst, t0, n, c0, 512, vt, vb2)

    for L in range(n_layers):
        i2 = L // 2
        last = L == DEPTH - 1
        all_tiles = [0, 1, 2, 3, 4]
        lat_tiles = [1, 2, 3, 4]
        if L % 2 == 1:
            X = ex[L]
            XKT, XV = X["KT"], X["V"]
            new_phase()
            modulate(L, 0, 1, all_tiles)
            new_phase()
            load_rope()
            wr = Ring(2, lambda i: (AR.take([128, KC, 512], BF16), Buf(f"wq{i}")))
            raw = Ring(2, lambda i: (AR.take([128, 512], F32), Buf(f"raw{i}")))
            sqr = Ring(2, lambda i: (AR.take([128, 512], BF16), Buf(f"sq{i}")))
            rsr = Ring(2, lambda i: (AR.take([128, 512], F32), Buf(f"rs{i}")))
            qnr = Ring(2, lambda i: (AR.take([128, 512], BF16), Buf(f"qn{i}")))
            tmpr = Ring(4, lambda i: (AR.take([128, 512], F32), Buf(f"rt{i}")))
            kst = Ring(3, lambda i: (AR.take([128, 512], BF16), Buf(f"kst{i}")))

            def ld_odd(blk):
                wt, wb = wr.next()
                wload(wt, wb, w_in_odd_d[i2], 0, 3072, blk * 512, 512)
                return wt, wb

            nxt = ld_odd(0)
            for blk in range(6):
                wt, wb = nxt
                if blk + 1 < 6:
                    nxt = ld_odd(blk + 1)
                if blk < 4:
                    isq = blk < 2
                    gcol = (SP_QG if isq else SP_KG) + i2
                    for ti in all_tiles:
                        t0, n = TT[ti]
                        for m in range(4):
                            ch = (blk % 2) * 4 + m
                            p0 = m % 2
                            for kc in range(KC):
                                mm(psum[p0][:, :n], wt[:, kc, m * 128:(m + 1) * 128], bufA[:, kc, t0:t0 + n],
                                   kc == 0, kc == KC - 1, R=[wb, aB[ti]], W=[psB[p0]])
                            rt, rb = raw.next()
                            copy("act", rt[:, :n], psum[p0][:, :n], R=[psB[p0]], W=[rb])
                            st, sbf = sqr.next()
                            act(st[:, :n], rt[:, :n], AF.Square, R=[rb], W=[sbf])
                            p1 = 2 + m % 2
                            mm(psum[p1][:, :n], BD64_B, st[:, :n], True, True, R=[sbf, cmB], W=[psB[p1]])
                            rst, rsb = rsr.next()
                            rstd_from_ps(psum[p1][:, :n], psB[p1], rst[:, :n], rsb, 1.0 / 64)
                            qt, qb = qnr.next()
                            stt("dve", qt[:, :n], rt[:, :n], smallp[:, gcol:gcol + 1], rst[:, :n], ALU.mult, ALU.mult,
                                R=[rb, rsb, smB], W=[qb])
                            p2 = 4 + m % 2
                            if isq:
                                rope(qt, qb, n, t0, bufB[:, ch, t0:t0 + n], bB[ti], p2, tmpr)
                            else:
                                kt, kb = kst.next()
                                rope(qt, qb, n, t0, kt[:, :n], kb, p2, tmpr)
                                xstore(XKT, ch * 128, 128, t0, n, kt, kb)
                else:
                    v_project(wt, wb, XV, (blk - 4) * 512, kst)
            for hp_ in range(4):
                allgather(XKT, [(hp_, j) for j in range(5)])
                allgather(XV, [(i, hp_) for i in range(5)])
            new_phase()
            AR2.reset()
            kslots = [(AR.take([128, 4, NT], BF16), Buf("kslot0")), (AR2.take([128, 4, NT], BF16), Buf("kslot1"))]
            vslots = [(AR.take([128, 4, 17, 128], BF16), Buf("vslot0")), (AR.take([128, 4, 17, 128], BF16), Buf("vslot1"))]
            ering = Ring(9, lambda i: (AR2.take([128, 512], BF16), Buf(f"e{i}")))
            ftmp = Ring(5, lambda i: (AR.take([128, 512], F32), Buf(f"ft{i}")))
            fsq = Ring(2, lambda i: (AR2.take([128, 512], BF16), Buf(f"fsq{i}")))
            accs = [(AR2.take([128, 512], F32), Buf(f"acc{i}")) for i in range(2)]
            qtiles = lat_tiles if last else all_tiles
            G = 2

            def load_head(h):
                (kslot, ksB), (vslot, vsB) = kslots[h % 2], vslots[h % 2]
                for r in range(4):
                    xload_rows(XKT, r, h * 128, 128, 0, NT, kslot[:, r, :], ksB)
                    xload_rows(XV, r, 0, 64, h * 128, 128, vslot[:, r, 0, :], vsB)
                    xload_tiles(XV, r, 64, NLAT, h * 128, 128, vslot[:, r, 1:17, :], vsB)
                return kslot, ksB, vslot, vsB

            nxt_head = load_head(0)
            for h in range(8):
                kslot, ksB, vslot, vsB = nxt_head
                if h + 1 < 8:
                    nxt_head = load_head(h + 1)
                for ti in qtiles:
                    t0, n = TT[ti]
                    if ti == 0:
                        ktl = [(r, 0, 0, 64) for r in range(4)]
                    else:
                        ktl = []
                        for r in range(4):
                            ktl.append((r, 0, 0, 64))
                            for j in range(16):
                                ktl.append((r, 1 + j, 64 + 128 * j, 128))
                    steps = [(ki, m) for ki in range(len(ktl)) for m in range(2)]
                    groups = [steps[i:i + G] for i in range(0, len(steps), G)]
                    orders = []
                    for gi_, grp in enumerate(groups):
                        fwd = (gi_ // 2) % 2 == 0
                        orders.append(list(range(len(grp))) if fwd else list(range(len(grp) - 1, -1, -1)))
                    av_order = []
                    for gi_, grp in enumerate(groups):
                        av_order += [grp[i] for i in reversed(orders[gi_])]
                    first_av, last_av = {}, {}
                    l2_list = [idx for idx, (ki, m) in enumerate(av_order) if m == 1 and ki % 2 == 1]
                    for idx, (ki, m) in enumerate(av_order):
                        first_av.setdefault(m, idx)
                        last_av[m] = idx
                    av_idx = 0
                    for (at_, ab_) in accs:
                        S.op("dve", lambda e, at_=at_: e.memset(at_[:, :], 0.0), W=[ab_])
                    prev = None
                    for gi_ in range(len(groups) + 1):
                        cur = None
                        if gi_ < len(groups):
                            grp = groups[gi_]
                            cur = []
                            for i in orders[gi_]:
                                ki, m = grp[i]
                                r, vt_i, k0, kn = ktl[ki]
                                si = (gi_ % 2) * 2 + i
                                mm(psum[si][:kn, :n], kslot[m * 64:(m + 1) * 64, r, k0:k0 + kn],
                                   bufB[m * 64:(m + 1) * 64, h, t0:t0 + n], True, True, R=[ksB, bB[ti]], W=[psB[si]])
                                et, eb = ering.next()
                                act(et[:kn, :n], psum[si][:kn, :n], AF.Exp, R=[psB[si]], W=[eb], scale=0.125)
                                if m == 0 or ki % 2 == 0:
                                    at_, ab_ = accs[m]
                                    tt("dve", at_[:kn, :n], at_[:kn, :n], et[:kn, :n], ALU.add, R=[ab_, eb], W=[ab_])
                                cur.append((ki, m, et, eb))
                        if prev is not None:
                            for (ki, m, et, eb) in reversed(prev):
                                r, vt_i, k0, kn = ktl[ki]
                                mm(psum[6 + m][:, :n], vslot[:kn, r, vt_i, :], et[:kn, :n],
                                   av_idx == first_av[m], av_idx == last_av[m], R=[vsB, eb], W=[psB[6 + m]])
                                if m == 1 and ki % 2 == 1:
                                    mm(psum[4][:, :n], ONES_B[:kn, :], et[:kn, :n],
                                       av_idx == l2_list[0], av_idx == l2_list[-1], R=[cmB2, eb], W=[psB[4]])
                                av_idx += 1
                        prev = cur
                    mm(psum[0][:, :n], ONES_F, accs[0][0][:, :n], True, True, R=[cmB, accs[0][1]], W=[psB[0]])
                    mm(psum[5][:, :n], ONES_F, accs[1][0][:, :n], True, True, R=[cmB, accs[1][1]], W=[psB[5]])
                    o1s, o1b = ftmp.next()
                    o2s, o2b = ftmp.next()
                    l1s, l1b = ftmp.next()
                    l2s, l2b = ftmp.next()
                    rs_, rsb_ = ftmp.next()
                    copy("dve", o1s[:, :n], psum[6][:, :n], R=[psB[6]], W=[o1b])
                    copy("dve", o2s[:, :n], psum[7][:, :n], R=[psB[7]], W=[o2b])
                    copy("act", l1s[:, :n], psum[0][:, :n], R=[psB[0]], W=[l1b])
                    copy("act", l2s[:, :n], psum[5][:, :n], R=[psB[5]], W=[l2b])
                    tt("dve", l2s[:, :n], l2s[:, :n], psum[4][:, :n], ALU.add, R=[l2b, psB[4]], W=[l2b])
                    recip(l1s[:, :n], l1s[:, :n], R=[l1b], W=[l1b])
                    recip(l2s[:, :n], l2s[:, :n], R=[l2b], W=[l2b])
                    tt("pool", o1s[:, :n], o1s[:, :n], l1s[:, :n], ALU.mult, R=[o1b, l1b], W=[o1b])
                    tt("pool", o2s[:, :n], o2s[:, :n], l2s[:, :n], ALU.mult, R=[o2b, l2b], W=[o2b])
                    ts("pool", o2s[:, :n], o2s[:, :n], smallp[:, SP_LAMNEG + i2:SP_LAMNEG + i2 + 1], None,
                       ALU.mult, None, R=[o2b, smB], W=[o2b])
                    tt("pool", o1s[:, :n], o1s[:, :n], o2s[:, :n], ALU.add, R=[o1b, o2b], W=[o1b])
                    sq_, sqb_ = fsq.next()
                    act(sq_[:, :n], o1s[:, :n], AF.Square, R=[o1b], W=[sqb_])
                    mm(psum[5][:, :n], ONES_B, sq_[:, :n], True, True, R=[sqb_, cmB2], W=[psB[5]])
                    rstd_from_ps(psum[5][:, :n], psB[5], rs_[:, :n], rsb_, 1.0 / 128)
                    tt("pool", o1s[:, :n], o1s[:, :n], rs_[:, :n], ALU.mult, R=[o1b, rsb_], W=[o1b])
                    ts("pool", bufB[:, h, t0:t0 + n], o1s[:, :n], smallp[:, SP_SUBG + i2:SP_SUBG + i2 + 1], None,
                       ALU.mult, None, R=[o1b, smB], W=[bB[ti]])
            new_phase()
            out_proj_residual(L, w_out_odd_d[i2], qtiles)
            new_phase()
            wrt = AR.take([128, KC, 8], F32)
            wrtB = Buf("wrt")
            dma("sp", wrt, w_router_d[i2].rearrange("(k p) n -> p k n", p=128), R=[], W=[wrtB])
            hf32 = AR.take([128, KC, 512], F32)
            hfB = Buf("hf32")
            rl = AR.take([128, 17, 8], F32)
            rlB = Buf("rl")
            mx8 = AR.take([128, 8], F32)
            ex8 = AR.take([128, 8], F32)
            nb1 = AR.take([128, 2], F32)
            ffn_tiles = lat_tiles if last else all_tiles
            sqm = Ring(2, lambda i: (AR.take([128, 512], BF16), Buf(f"msq{i}")))
            rsm = Ring(2, lambda i: (AR.take([128, 512], F32), Buf(f"mrs{i}")))
            tmpm = Ring(2, lambda i: (AR.take([128, 512], F32), Buf(f"mtmp{i}")))
            for ti in ffn_tiles:
                t0, n = TT[ti]
                col = 1 if ti == 0 else 0
                pi = 0
                for kc in range(KC):
                    sqt, sqb = sqm.next()
                    act(sqt[:, :n], xT[:, kc, t0:t0 + n], AF.Square, R=[xB[ti]], W=[sqb])
                    mm(psum[pi][:, :n], ONES_B, sqt[:, :n], kc == 0, kc == KC - 1, R=[sqb, cmB], W=[psB[pi]])
                rst, rsb = rsm.next()
                rstd_from_ps(psum[pi][:, :n], psB[pi], rst[:, :n], rsb, 1.0 / D)
                for kc in range(KC):
                    tmt, tmb = tmpm.next()
                    tt("dve", tmt[:, :n], xT[:, kc, t0:t0 + n], rst[:, :n], ALU.mult, R=[xB[ti], rsb], W=[tmb])
                    act(hf32[:, kc, :n], tmt[:, :n], AF.Identity, R=[tmb, modB], W=[hfB],
                        bias=modap(L, 3, kc, col), scale=modap(L, 4, kc, col))
                    copy("pool", bufA[:, kc, t0:t0 + n], hf32[:, kc, :n], R=[hfB], W=[aB[ti]])
                for s0 in range(0, n, 128):
                    sn = min(128, n - s0)
                    tmi = 0 if ti == 0 else 1 + (t0 + s0 - 64) // 128
                    for kc in range(KC):
                        mm(psum[1][:sn, 0:8], hf32[:, kc, s0:s0 + sn], wrt[:, kc, :], kc == 0, kc == KC - 1,
                           R=[hfB, wrtB], W=[psB[1]])
                    copy("dve", rl[:sn, tmi, :], psum[1][:sn, 0:8], R=[psB[1]], W=[rlB])
                    S.op("dve", lambda e, sn=sn, tmi=tmi: e.max(out=mx8[:sn, :], in_=rl[:sn, tmi, :]), R=[rlB], W=[rlB])
                    ts("dve", nb1[:sn, 0:1], mx8[:sn, 0:1], -1.0, None, ALU.mult, None, R=[rlB], W=[rlB])
                    act(ex8[:sn, :], rl[:sn, tmi, :], AF.Exp, R=[rlB], W=[rlB], bias=nb1[:sn, 0:1])
                    stt("dve", ex8[:sn, :], rl[:sn, tmi, :], mx8[:sn, 1:2], ex8[:sn, :], ALU.is_ge, ALU.mult,
                        R=[rlB], W=[rlB])
                    S.op("dve", lambda e, sn=sn: e.reduce_sum(out=nb1[:sn, 1:2], in_=ex8[:sn, :], axis=AX.X), R=[rlB], W=[rlB])
                    recip(nb1[:sn, 1:2], nb1[:sn, 1:2], R=[rlB], W=[rlB])
                    ts("dve", comb[:sn, tmi, :], ex8[:sn, :], nb1[:sn, 1:2], None, ALU.mult, None, R=[rlB], W=[combB])
            new_phase()
            cw_all = bufB_flat.bitcast(F32).rearrange("p (a b) -> p a b", a=4)
            cwr = Ring(4, lambda i: (cw_all[:, i, :], Buf(f"cw{i}")))
            dgr = Ring(2, lambda i: (AR.take([128, 128], F32), Buf(f"dg{i}")))
            rings = ffn_rings(512, 2)
            jobs = []
            for ex_i in range(NEXP):
                cw, cwb = cwr.next()

                def pre(ex_i=ex_i, cw=cw, cwb=cwb):
                    for (t0, n) in TM:
                        if last and t0 == 0:
                            continue
                        tmi = 0 if t0 == 0 else 1 + (t0 - 64) // 128
                        dg, dgb = dgr.next()
                        ts("pool", dg[:n, :n], ID_F[:n, :n], comb[:n, tmi, ex_i:ex_i + 1], None, ALU.mult, None,
                           R=[cmB, combB], W=[dgb])
                        mm(psum[6][:, :n], ONES_F[:n, :], dg[:n, :n], True, True, R=[dgb, cmB], W=[psB[6]])
                        copy("act", cw[:, t0:t0 + n], psum[6][:, :n], R=[psB[6]], W=[cwb])

                for j in range(EXP // 512):
                    jobs.append((moe_w1_d[i2, ex_i], moe_w3_d[i2, ex_i], moe_w2_d[i2, ex_i], j, cw, cwb,
                                 pre if j == 0 else None))
            swiglu_jobs(L, jobs, 512, ffn_tiles, rings)
        else:
            X = ex[L]
            XE, XKc = X["E"], X["Kc"]
            lgc = lambda d_, hd, i2=i2: smallp[:, SP_LG + i2 * 8 + d_ * 4 + hd:SP_LG + i2 * 8 + d_ * 4 + hd + 1]
            g128c = lambda d_, hd, i2=i2: smallp[:, SP_G128 + i2 * 8 + d_ * 4 + hd:SP_G128 + i2 * 8 + d_ * 4 + hd + 1]
            new_phase()
            modulate(L, 0, 1, all_tiles)
            new_phase()
            QrT = AR.take([128, 2, NT], BF16)
            KrT = AR.take([128, 2, NT], BF16)
            qrB = [Buf(f"qr{t}") for t in range(5)]
            krB = [Buf(f"kr{t}") for t in range(5)]
            keep_off = AR.off
            load_rope()
            wr = Ring(2, lambda i: (AR.take([128, KC, 512], BF16), Buf(f"we{i}")))
            cs128 = AR.take([128, 256], BF16)
            csB = Buf("cs128")
            dma("pool", cs128, cs128_d[:, :], R=[], W=[csB])
            qnr = Ring(2, lambda i: (AR.take([128, 512], BF16), Buf(f"qn{i}")))
            tmpr = Ring(4, lambda i: (AR.take([128, 512], F32), Buf(f"rt{i}")))
            stg = Ring(3, lambda i: (AR.take([128, 512], BF16), Buf(f"stg{i}")))
            fT = bufB

            def ld_even(blk):
                wt, wb = wr.next()
                wload(wt, wb, w_in_even_d[i2], 0, 2048, blk * 512, 512)
                return wt, wb

            nxt = ld_even(0)
            for blk in range(4):
                wt, wb = nxt
                if blk + 1 < 4:
                    nxt = ld_even(blk + 1)
                if blk == 2:
                    v_project(wt, wb, XE, 1280, stg)
                    continue
                for ti in all_tiles:
                    t0, n = TT[ti]
                    for m in range(4):
                        p0 = m % 2
                        for kc in range(KC):
                            mm(psum[p0][:, :n], wt[:, kc, m * 128:(m + 1) * 128], bufA[:, kc, t0:t0 + n],
                               kc == 0, kc == KC - 1, R=[wb, aB[ti]], W=[psB[p0]])
                        if blk == 0:
                            copy("act", fT[:, m, t0:t0 + n], psum[p0][:, :n], R=[psB[p0]], W=[bB[ti]])
                        elif blk == 3:
                            act(bufB[:, 4 + m, t0:t0 + n], psum[p0][:, :n], AF.Silu, R=[psB[p0]], W=[bB[ti]])
                        else:
                            qt, qb = qnr.next()
                            if m < 2:
                                copy("act", qt[:, :n], psum[p0][:, :n], R=[psB[p0]], W=[qb])
                                rope(qt, qb, n, t0, QrT[:, m, t0:t0 + n], qrB[ti], 4 + m % 2, tmpr)
                            else:
                                S.op("act", lambda e, qt=qt, p0=p0, n=n: e.mul(out=qt[:, :n], in_=psum[p0][:, :n], mul=0.125),
                                     R=[psB[p0]], W=[qb])
                                rope(qt, qb, n, t0, KrT[:, m - 2, t0:t0 + n], krB[ti], 4 + m % 2, tmpr)
            for (t0, n) in TM:
                ti = 0 if t0 == 0 else 1 + (t0 - 64) // 512
                for gp in range(2):
                    p0 = 6 + gp
                    for g2 in range(2):
                        g = gp * 2 + g2
                        mm(psum[p0][:n, g2 * 256:(g2 + 1) * 256], fT[:, g, t0:t0 + n], cs128, True, True,
                           R=[bB[ti], csB], W=[psB[p0]])
                    at, ab = stg.next()
                    copy("act" if gp == 0 else "dve", at[:n, :], psum[p0][:n, :], R=[psB[p0]], W=[ab])
                    xstore(XE, t0, n, gp * 512, 512, at, ab)
                p0 = 5
                for c2 in range(2):
                    mm(psum[p0][:n, c2 * 128:(c2 + 1) * 128], KrT[:, c2, t0:t0 + n], ID_B, True, True,
                       R=[krB[ti], cmB], W=[psB[p0]])
                kt, kb = stg.next()
                copy("dve", kt[:n, 0:256], psum[p0][:n, 0:256], R=[psB[p0]], W=[kb])
                xstore(XE, t0, n, 1024, 256, kt, kb)
            for c2 in range(2):
                xstore(XKc, c2 * 128, 128, 0, 64, KrT[:, c2, :], krB[0])
            allgather(XKc)
            allgather(XE, [(i, j) for j in (2, 3) for i in range(5)])
            allgather(XE, [(i, j) for j in (0, 1) for i in range(5)])
            S.phase += 1
            if S.phase_limit is not None and S.phase > S.phase_limit:
                S.enabled = False
            S.barrier()
            AR.off = keep_off
            AR2.reset()
            AR3.reset()
            rtab = AR.take([128, 4, 128], F32)
            rpos = AR.take([128, 4, 128], F32)
            rdist = AR.take([128, 4, 68], F32)
            rctx = AR.take([64, 4, 4, 64], F32)
            rcB = Buf("rconst")
            dma("sp", rtab, rtab_d[:, :, :], R=[], W=[rcB])
            dma("sp", rpos, rpos_d[:, :, :], R=[], W=[rcB])
            dma("sp", rdist, rdist_d[:, :, :], R=[], W=[rcB])
            dma("sp", rctx, rctx_d[:, :, :, :], R=[], W=[rcB])
            Dc = AR.take([128, 4, 128], F32)
            dq = AR.take([128, 2, 4, 128], BF16)
            dk = AR.take([128, 2, 4], F32)
            wk = AR.take([128, 2, 4, 68], F32)
            Dx = AR.take([64, 4, 4, 64], F32)
            dcB = Buf("dconst")
            t1 = AR.take([128, 128], F32)
            for hd in range(4):
                act(Dc[:, hd, :], rtab[:, 0, :], AF.Exp, R=[rcB, smB], W=[dcB], scale=lgc(0, hd))
                tt("dve", Dc[:, hd, :], Dc[:, hd, :], rtab[:, 1, :], ALU.mult, R=[dcB, rcB], W=[dcB])
                act(t1[:, :], rtab[:, 2, :], AF.Exp, R=[rcB, smB, dcB], W=[dcB], scale=lgc(1, hd))
                tt("dve", t1[:, :], t1[:, :], rtab[:, 3, :], ALU.mult, R=[dcB, rcB], W=[dcB])
                tt("dve", Dc[:, hd, :], Dc[:, hd, :], t1[:, :], ALU.add, R=[dcB], W=[dcB])
                for d_ in range(2):
                    act(dq[:, d_, hd, :], rpos[:, d_, :], AF.Exp, R=[rcB, smB], W=[dcB], scale=lgc(d_, hd))
                    act(dk[:, d_, hd:hd + 1], rpos[:, 2, d_:d_ + 1], AF.Exp, R=[rcB, smB], W=[dcB], scale=lgc(d_, hd))
                    act(wk[:, d_, hd, :], rdist[:, 2 * d_, :], AF.Exp, R=[rcB, smB], W=[dcB], scale=lgc(d_, hd))
                    tt("dve", wk[:, d_, hd, :], wk[:, d_, hd, :], rdist[:, 2 * d_ + 1, :], ALU.mult, R=[dcB, rcB], W=[dcB])
                for r in range(4):
                    act(Dx[:, hd, r, :], rctx[:, 0, r, :], AF.Exp, R=[rcB, smB], W=[dcB], scale=lgc(0, hd)[0:64, :])
                    tt("dve", Dx[:, hd, r, :], Dx[:, hd, r, :], rctx[:, 1, r, :], ALU.mult, R=[dcB, rcB], W=[dcB])
                    act(t1[0:64, 0:64], rctx[:, 2, r, :], AF.Exp, R=[rcB, smB, dcB], W=[dcB], scale=lgc(1, hd)[0:64, :])
                    tt("dve", t1[0:64, 0:64], t1[0:64, 0:64], rctx[:, 3, r, :], ALU.mult, R=[dcB, rcB], W=[dcB])
                    tt("dve", Dx[:, hd, r, :], Dx[:, hd, r, :], t1[0:64, 0:64], ALU.add, R=[dcB], W=[dcB])
            kvr = Ring(2, lambda i: (AR.take([128, 4, 768], BF16), Buf(f"kv{i}")))
            k2r = Ring(4, lambda i: (AR.take([128, 128], BF16), Buf(f"k2{i}")))

            def scaled_pair(src_ap, pr_, scal, R_, eng0="dve", eng1="pool"):
                k2, k2b = k2r.next()
                n_ = src_ap.shape[0]
                for hh in range(2):
                    hd = pr_ * 2 + hh
                    ts(eng0 if hh == 0 else eng1, k2[:n_, hh * 64:(hh + 1) * 64], src_ap[:, hd * 64:(hd + 1) * 64],
                       scal(hd), None, ALU.mult, None, R=R_, W=[k2b])
                return k2, k2b

            gi = 0
            for r in range(4):
                groups = [(0, [0])] + [(1 + 4 * q, [1 + 4 * q + u for u in range(4)]) for q in range(4)]
                for (tl0, tls) in groups:
                    kv, kvb = kvr.next()
                    if tl0 == 0:
                        xload_rows(XE, r, 0, 64, 1024, 768, kv[:, 0, :], kvb)
                    else:
                        xload_tiles(XE, r, 64 + (tl0 - 1) * 128, 512, 1024, 768, kv[:, 0:4, :], kvb)
                    for u, tl in enumerate(tls):
                        n = 64 if tl == 0 else 128
                        gi = r * 17 + tl
                        for d_ in range(2):
                            for pr_ in range(2):
                                k2, k2b = scaled_pair(kv[:n, u, :], pr_, lambda hd, d_=d_, gi=gi, n=n: wk[:n, d_, hd, gi:gi + 1], [kvb, dcB])
                                for hh in range(2):
                                    hd = pr_ * 2 + hh
                                    mm(psum[4 + d_][:, hd * 128:(hd + 1) * 128], k2[:n, :], kv[:n, u, 256 + hd * 128:256 + (hd + 1) * 128],
                                       gi == 0, gi == 67, R=[k2b, kvb], W=[psB[4 + d_]])
            Sf = AR2.take([128, 4, 128], F32)
            Sfb = AR2.take([128, 4, 128], BF16)
            Sbf = AR2.take([128, 4, 128], F32)
            okv = AR2.take([128, 16, 768], BF16)
            Sb_all = AR3.take([128, 16, 4, 128], BF16)
            stB = Buf("states")
            ps4v = psum[4][:, :].rearrange("p (h e) -> p h e", h=4)
            ps5v = psum[5][:, :].rearrange("p (h e) -> p h e", h=4)
            copy("dve", Sf[:, :, :], ps4v, R=[psB[4]], W=[stB])
            copy("act", Sfb[:, :, :], ps4v, R=[psB[4]], W=[stB])
            copy("dve", Sbf[:, :, :], ps5v, R=[psB[5]], W=[stB])
            okB = Buf("okv")
            xload_tiles(XE, 0, 64, NLAT, 1024, 768, okv, okB, own=True)
            for c in range(15, -1, -1):
                copy("act", Sb_all[:, c, :, :], Sbf[:, :, :], R=[stB], W=[stB])
                if c == 0:
                    break
                for pr_ in range(2):
                    k2, k2b = scaled_pair(okv[:, c, :], pr_, lambda hd: dk[:, 1, hd:hd + 1], [okB, dcB], "pool", "pool")
                    for hh in range(2):
                        hd = pr_ * 2 + hh
                        mm(psum[5][:, hd * 128:(hd + 1) * 128], k2[:, :], okv[:, c, 256 + hd * 128:256 + (hd + 1) * 128],
                           True, True, R=[k2b, okB], W=[psB[5]])
                for hd in range(4):
                    stt("dve", Sbf[:, hd, :], Sbf[:, hd, :], g128c(1, hd), ps5v[:, hd, :],
                        ALU.mult, ALU.add, R=[stB, psB[5], smB], W=[stB])
            qd = Ring(4, lambda i: (AR.take([128, 128], BF16), Buf(f"qd{i}")))
            sdr = Ring(3, lambda i: (AR.take([128, 128], BF16), Buf(f"sd{i}")))
            osq = Ring(2, lambda i: (AR.take([128, 512], BF16), Buf(f"osq{i}")))
            ors = Ring(2, lambda i: (AR.take([128, 512], F32), Buf(f"ors{i}")))
            oo = Ring(2, lambda i: (AR.take([128, 512], F32), Buf(f"oo{i}")))
            scr = Ring(2, lambda i: i)

            def finish_out(pso, hd, t0, n, ti):
                o_, ob = oo.next()
                copy("act", o_[:, :n], psum[pso][:, :n], R=[psB[pso]], W=[ob])
                sq_, sqb_ = osq.next()
                act(sq_[:, :n], psum[pso][:, :n], AF.Square, R=[psB[pso]], W=[sqb_])
                mm(psum[6][:, :n], ONES_B, sq_[:, :n], True, True, R=[sqb_, cmB], W=[psB[6]])
                rs_, rsb_ = ors.next()
                rstd_from_ps(psum[6][:, :n], psB[6], rs_[:, :n], rsb_, 1.0 / 128)
                tt("dve", o_[:, :n], o_[:, :n], rs_[:, :n], ALU.mult, R=[ob, rsb_], W=[ob])
                tt("dve", bufB[:, 4 + hd, t0:t0 + n], o_[:, :n], bufB[:, 4 + hd, t0:t0 + n], ALU.mult, R=[ob, bB[ti]], W=[bB[ti]])

            for c4 in range(4):
                ti = 1 + c4
                t0t, _ = TT[ti]
                for pr_ in range(2):
                    for cc in range(4):
                        c = c4 * 4 + cc
                        t0 = 64 + c * 128
                        k2, k2b = scaled_pair(okv[:, c, :], pr_, lambda hd: dk[:, 0, hd:hd + 1], [okB, dcB], "pool", "pool")
                        for hh in range(2):
                            hd = pr_ * 2 + hh
                            pso = 2 + hh
                            hp = hh * 64
                            cq = pr_
                            si = scr.next()
                            mm(psum[si][:, 0:128], KrT[hp:hp + 64, cq, t0:t0 + 128], QrT[hp:hp + 64, cq, t0:t0 + 128], True, True,
                               R=[krB[ti], qrB[ti]], W=[psB[si]])
                            sd, sdb = sdr.next()
                            tt("dve", sd[:, :], psum[si][:, 0:128], Dc[:, hd, :], ALU.mult, R=[psB[si], dcB], W=[sdb])
                            qf, qfb = qd.next()
                            qbk, qbb = qd.next()
                            tt("pool", qf[hp:hp + 64, :], QrT[hp:hp + 64, cq, t0:t0 + 128], dq[hp:hp + 64, 0, hd, :], ALU.mult,
                               R=[qrB[ti], dcB], W=[qfb])
                            tt("pool", qbk[hp:hp + 64, :], QrT[hp:hp + 64, cq, t0:t0 + 128], dq[hp:hp + 64, 1, hd, :], ALU.mult,
                               R=[qrB[ti], dcB], W=[qbb])
                            oc = psum[pso][:, cc * 128:(cc + 1) * 128]
                            mm(oc, okv[:, c, 256 + hd * 128:256 + (hd + 1) * 128], sd[:, :], True, False, R=[okB, sdb], W=[psB[pso]])
                            mm(oc, Sfb[hp:hp + 64, hd, :], qf[hp:hp + 64, :], False, False, R=[stB, qfb], W=[psB[pso]])
                            mm(oc, Sb_all[hp:hp + 64, c, hd, :], qbk[hp:hp + 64, :], False, True, R=[stB, qbb], W=[psB[pso]])
                            mm(psum[4][:, hd * 128:(hd + 1) * 128], k2[:, :], okv[:, c, 256 + hd * 128:256 + (hd + 1) * 128],
                               True, True, R=[k2b, okB], W=[psB[4]])
                            stt("dve", Sf[:, hd, :], Sf[:, hd, :], g128c(0, hd), ps4v[:, hd, :],
                                ALU.mult, ALU.add, R=[stB, psB[4], smB], W=[stB])
                            copy("act", Sfb[:, hd, :], Sf[:, hd, :], R=[stB], W=[stB])
                    for hh in range(2):
                        finish_out(2 + hh, pr_ * 2 + hh, t0t, 512, ti)
            kcs = AR.take([128, 2, 4, 64], BF16)
            kcB = Buf("kcs")
            for r in range(4):
                for c2 in range(2):
                    xload_rows(XKc, r, c2 * 128, 128, 0, 64, kcs[:, c2, r, :], kcB)
            vcs = AR2.take([64, 4, 512], BF16)
            vcB = Buf("vcs")
            for r in range(4):
                xload_rows(XE, r, 0, 64, 1280, 512, vcs[:, r, :], vcB)
            for hd in range(4):
                pso = 2 + hd % 2
                hp = (hd % 2) * 64
                cq = hd // 2
                for r in range(4):
                    si = scr.next()
                    mm(psum[si][0:64, 0:64], kcs[hp:hp + 64, cq, r, :], QrT[hp:hp + 64, cq, 0:64], True, True,
                       R=[kcB, qrB[0]], W=[psB[si]])
                    sd, sdb = sdr.next()
                    tt("dve", sd[0:64, 0:64], psum[si][0:64, 0:64], Dx[:, hd, r, :], ALU.mult, R=[psB[si], dcB], W=[sdb])
                    mm(psum[pso][:, 0:64], vcs[:, r, hd * 128:(hd + 1) * 128], sd[0:64, 0:64], r == 0, r == 3,
                       R=[vcB, sdb], W=[psB[pso]])
                finish_out(pso, hd, 0, 64, 0)
            new_phase()
            AR2.reset()
            AgA = AR2.take([128, 64, 256], BF16)
            dctx = AR2.take([64, 4, 2, 64], BF16)
            AgB = AR.take([128, 64, 256], BF16)
            agB = [Buf("AgA"), Buf("AgB")]
            Ags = [AgA, AgB]
            tbr = Ring(3, lambda i: (AR.take([128, 2, 4, 512], BF16), Buf(f"tb{i}")))
            dcxB = Buf("dctx")
            dma("pool", dctx, dftctx_d[:, :, :, :], R=[], W=[dcxB])
            actx = AR.take([64, 4, 1024], BF16)
            acxB = Buf("actx")
            for r in range(4):
                xload_rows(XE, r, 0, 64, 0, 1024, actx[:, r, :], acxB)

            def ld_tab(kt, tc):
                tb, tbb = tbr.next()
                row0 = (kt * 16 + tc) * 128
                dma("sp", tb[:, :, :, :].rearrange("p a b c -> p (a b c)"), dft_d[row0:row0 + 128, :], R=[], W=[tbb])
                return tb, tbb

            for gp in range(2):
                for g2 in range(2):
                    for r in range(4):
                        xload_tiles(XE, r, 64, NLAT, (gp * 2 + g2) * 256, 256, Ags[g2][:, r * 16:(r + 1) * 16, :], agB[g2])
                seq = [(kt, tc) for kt in range(4) for tc in range(16)]
                nxt = ld_tab(*seq[0])
                for qi, (kt, tc) in enumerate(seq):
                    tb, tbb = nxt
                    if qi + 1 < len(seq):
                        nxt = ld_tab(*seq[qi + 1])
                    for g2 in range(2):
                        pi = g2 * 2 + kt % 2
                        for j in range(4):
                            tl = tc * 4 + j
                            for cs_ in range(2):
                                mm(psum[pi][:, :], Ags[g2][:, tl, cs_ * 128:(cs_ + 1) * 128], tb[:, cs_, j, :],
                                   tc == 0 and j == 0 and cs_ == 0, tc == 15 and j == 3 and cs_ == 1,
                                   R=[agB[g2], tbb], W=[psB[pi]])
                        if tc == 15:
                            t0 = 64 + kt * 512
                            copy("act" if g2 == 0 else "dve", bufB[:, gp * 2 + g2, t0:t0 + 512], psum[pi][:, :],
                                 R=[psB[pi]], W=[bB[1 + kt]])
                for g2 in range(2):
                    g = gp * 2 + g2
                    for r in range(4):
                        for cs_ in range(2):
                            mm(psum[4 + g2][:, 0:64], actx[:, r, g * 256 + cs_ * 128:g * 256 + (cs_ + 1) * 128], dctx[:, r, cs_, :],
                               r == 0 and cs_ == 0, r == 3 and cs_ == 1, R=[acxB, dcxB], W=[psB[4 + g2]])
                    copy("act", bufB[:, g, 0:64], psum[4 + g2][:, 0:64], R=[psB[4 + g2]], W=[bB[0]])
            new_phase()
            out_proj_residual(L, w_out_even_d[i2], all_tiles)
            new_phase()
            modulate(L, 3, 4, all_tiles)
            rings = ffn_rings(256, 3)
            jobs = [(ffn_w1_d[i2], ffn_w3_d[i2], ffn_w2_d[i2], j, None, None) for j in range(FFN // 256)]
            swiglu_jobs(L, jobs, 256, all_tiles, rings)

    S.enabled = True
    S.phase_limit = None
    new_phase()
    yB = Buf("yT", keep=True)
    for kc in range(KC):
        dma("sp", yT_d[kc * 128:(kc + 1) * 128, :], xT[:, kc, 64:NT], R=xB, W=[yB])
    fin = Ins("sp", None, "c")
    fin.deps = set([yB.lw])
    S.q["sp"].append(fin)
    S.finalize()
    with nc.Block() as block:
        @block.tensor
        def _(e):
            S.emit(e, "pe")

        @block.scalar
        def _(e):
            S.emit(e, "act")

        @block.vector
        def _(e):
            S.emit(e, "dve")

        @block.gpsimd
        def _(e):
            S.emit(e, "pool")

        @block.sync
        def _(e):
            S.emit(e, "sp")
    return nc


def _rope_tables(core):
    r = core % 4
    quarter = 16
    inv_freq = 10000.0 ** (-np.arange(quarter, dtype=np.float32) / quarter)
    cos = np.ones((128, NT), np.float32)
    sin = np.zeros((128, NT), np.float32)
    tok = np.arange(r * NLAT, (r + 1) * NLAT)
    row = (tok // 64).astype(np.float32)
    colv = (tok % 64).astype(np.float32)
    ang = np.concatenate([row[:, None] * inv_freq, colv[:, None] * inv_freq], axis=-1).astype(np.float32)
    c = np.cos(ang).T
    s = np.sin(ang).T
    for p in range(128):
        d = p % 64
        fi = d % 32
        cos[p, 64:] = c[fi]
        sin[p, 64:] = -s[fi] if d < 32 else s[fi]
    return cos, sin


def _const_mats():
    cm = np.zeros((128, 6, 128), np.float32)
    cm[:, 0, :] = 1.0
    cm[0:64, 1, 0:64] = 1.0
    cm[64:128, 1, 64:128] = 1.0
    cm[:, 2, :] = np.eye(128, dtype=np.float32)
    for p in range(128):
        d = p % 64
        partner = (p - d) + ((d + 32) % 64)
        cm[p, 3, partner] = 1.0
    return cm


def _dft_tables(core):
    r = core % 4
    n = 8192
    tab = np.zeros((4, 16, 128, 2, 4, 512), ml_dtypes.bfloat16)
    sc = 1.0 / np.sqrt(n)
    p = np.arange(128, dtype=np.int64)[:, None, None]
    j = np.arange(4, dtype=np.int64)[None, :, None]
    k = np.arange(512, dtype=np.int64)[None, None, :]
    for kt in range(4):
        kk = r * NLAT + kt * 512 + k
        for tc in range(16):
            t = (tc * 4 + j) * 128 + p
            ph = ((t * kk) % n).astype(np.float64) * (2.0 * np.pi / n)
            tab[kt, tc, :, 0] = (np.cos(ph) * sc).astype(np.float32).astype(ml_dtypes.bfloat16)
            tab[kt, tc, :, 1] = (-np.sin(ph) * sc).astype(np.float32).astype(ml_dtypes.bfloat16)
    tab = tab.reshape(8192, 4096)
    n2 = 256
    dctx = np.zeros((64, 4, 2, 64), np.float32)
    for rr in range(4):
        tt_ = (np.arange(64) + rr * 64)[:, None]
        kk = (np.arange(64) + r * 64)[None, :]
        ph2 = ((tt_ * kk) % n2).astype(np.float64) * (2.0 * np.pi / n2)
        dctx[:, rr, 0, :] = np.cos(ph2) / np.sqrt(n2)
        dctx[:, rr, 1, :] = -np.sin(ph2) / np.sqrt(n2)
    return tab, dctx


def _cs128():
    c = np.arange(128)[:, None] * np.arange(128)[None, :]
    ph = (c % 128).astype(np.float64) * (2.0 * np.pi / 128)
    out = np.zeros((128, 256), np.float32)
    out[:, :128] = np.cos(ph) / np.sqrt(128.0)
    out[:, 128:] = np.sin(ph) / np.sqrt(128.0)
    return out


def _ret_tables(core):
    r = core % 4
    j = np.arange(128)[:, None].astype(np.float32)
    i = np.arange(128)[None, :].astype(np.float32)
    rtab = np.zeros((128, 4, 128), np.float32)
    rtab[:, 0, :] = np.maximum(i - j, 0)
    rtab[:, 1, :] = (j <= i)
    rtab[:, 2, :] = np.maximum(j - i, 0)
    rtab[:, 3, :] = (j > i)
    rpos = np.zeros((128, 4, 128), np.float32)
    rpos[:, 0, :] = np.arange(128)[None, :] + 1.0
    rpos[:, 1, :] = 128.0 - np.arange(128)[None, :]
    rpos[:, 2, 0] = 127.0 - np.arange(128)
    rpos[:, 2, 1] = np.arange(128)
    I0 = r * NLAT
    P0f = 256 + I0
    i_last = I0 + NLAT - 1
    P0b = 256 + 8191 - i_last
    rdist = np.zeros((128, 4, 68), np.float32)
    for rr in range(4):
        for tl in range(17):
            gi = rr * 17 + tl
            for p in range(128):
                if tl == 0:
                    if p >= 64:
                        continue
                    m = rr * 64 + p
                    pf = m
                    pb = 255 - m
                else:
                    li = rr * NLAT + (tl - 1) * 128 + p
                    pf = 256 + li
                    pb = 256 + 8191 - li
                if pf < P0f:
                    rdist[p, 0, gi] = P0f - 1 - pf
                    rdist[p, 1, gi] = 1.0
                if pb < P0b:
                    rdist[p, 2, gi] = P0b - 1 - pb
                    rdist[p, 3, gi] = 1.0
    rctx = np.zeros((64, 4, 4, 64), np.float32)
    for rr in range(4):
        mj = (rr * 64 + np.arange(64))[:, None].astype(np.float32)
        mi = (r * 64 + np.arange(64))[None, :].astype(np.float32)
        rctx[:, 0, rr, :] = np.maximum(mi - mj, 0)
        rctx[:, 1, rr, :] = (mj <= mi)
        rctx[:, 2, rr, :] = np.maximum(mj - mi, 0)
        rctx[:, 3, rr, :] = (mj > mi)
    return rtab, rpos, rdist, rctx


_CACHE = {}


def _get_program(n_layers=DEPTH):
    key = ("nc", n_layers)
    if key not in _CACHE:
        _CACHE[key] = build_program(n_layers)
    return _CACHE[key]


def _host_inputs(inp, n_layers=DEPTH):
    f32 = np.float32
    x = np.asarray(inp["x"], f32)
    ctx = np.asarray(inp["ctx"], f32)
    c = np.asarray(inp["c"], f32)
    c_ctx = np.asarray(inp["c_ctx"], f32)
    shared = {}
    n_even = (n_layers + 1) // 2
    n_odd = max(n_layers // 2, 1)
    shared["w_mod"] = np.ascontiguousarray(np.asarray(inp["w_mod"], f32)[:n_layers])
    for k in ("w_in_even", "w_out_even", "ffn_w1", "ffn_w3", "ffn_w2"):
        shared[k] = np.ascontiguousarray(np.asarray(inp[k], f32)[:n_even])
    for k in ("w_in_odd", "w_out_odd", "w_router", "moe_w1", "moe_w3", "moe_w2"):
        if n_layers >= 2:
            shared[k] = np.ascontiguousarray(np.asarray(inp[k], f32)[:n_odd])
    b_mod = np.asarray(inp["b_mod"], f32)
    shared["b_modT"] = np.ascontiguousarray(b_mod.reshape(4, 48, 128).transpose(2, 0, 1))
    sp = np.zeros((128, 1024), f32)
    dec = np.stack([np.asarray(inp["ret_decay_fwd"], f32), np.asarray(inp["ret_decay_bwd"], f32)], axis=1)
    sp[:, 0:16] = dec.reshape(1, 16)
    p64 = np.arange(128) % 64
    for i in range(2):
        sp[:, 16 + i] = np.asarray(inp["q_norm_gain"], f32)[i][p64]
        sp[:, 18 + i] = np.asarray(inp["k_norm_gain"], f32)[i][p64]
        sp[:, 20 + i] = np.asarray(inp["subln_gain"], f32)[i]
        lam = np.concatenate([np.asarray(inp[k], f32)[i] for k in ("lambda_q1", "lambda_k1", "lambda_q2", "lambda_k2")])
        sp[:, 32 + i * 256:32 + (i + 1) * 256] = lam[None, :]
    shared["smallp"] = sp
    shared["cmat"] = _const_mats()
    shared["cs128"] = _cs128()
    in_maps = []
    for core in range(NCORES):
        b, r = core // 4, core % 4
        m = dict(shared)
        xt = np.concatenate([ctx[b, r * 64:(r + 1) * 64, :], x[b, r * NLAT:(r + 1) * NLAT, :]], axis=0)
        m["xT"] = np.ascontiguousarray(xt.T)
        cT = np.stack([c[b], c_ctx], axis=-1)
        m["cT"] = np.ascontiguousarray(cT.reshape(8, 128, 2).transpose(1, 0, 2))
        key = ("tabs", r)
        if key not in _CACHE:
            cos, sin = _rope_tables(core)
            dtab, dctx = _dft_tables(core)
            rtab, rpos, rdist, rctx = _ret_tables(core)
            _CACHE[key] = dict(cosT=cos, sinT=sin, dft=dtab, dftctx=dctx, rtab=rtab, rpos=rpos, rdist=rdist, rctx=rctx)
        m.update(_CACHE[key])
        in_maps.append(m)
    return in_maps


def kernel(**inputs):
    nc = _get_program(DEPTH)
    in_maps = _host_inputs(inputs)
    res = run_bass_kernel_spmd(nc, in_maps, core_ids=list(range(NCORES)))
    out = np.zeros((2, 8192, D), np.float32)
    for core in range(NCORES):
        b, r = core // 4, core % 4
        out[b, r * NLAT:(r + 1) * NLAT, :] = np.asarray(res.results[core]["yT"], np.float32).T
    return out
```
